# Optimizing a Trainium2 kernel written in Bass

```python
import math
import jax, jax.numpy as jnp
from jax import lax
import numpy as np

D_MODEL = 1024
BATCH = 8
SEQ = 2048
DEPTH = 1

ATTN_HEADS = 8
ATTN_HEAD_DIM = 64
ATTN_WIDTH = ATTN_HEADS * ATTN_HEAD_DIM
MOBA_BLOCK = 256
MOBA_TOPK = 3
MOBA_QUERY_BLOCK = 16
ROPE_THETA = 10000.0
MLSTM_HEADS = 4
MLSTM_HEAD_DIM = 128
MLSTM_WIDTH = MLSTM_HEADS * MLSTM_HEAD_DIM
MLSTM_CONV = 4
MLSTM_CHUNK = 64
MOE_GROUPS = 8
MOE_EXPERTS_PER_GROUP = 8
MOE_EXPERTS = MOE_GROUPS * MOE_EXPERTS_PER_GROUP
MOE_TOPK = 2
MOE_D_FF = 512
MOE_BLOCK = 128
LN_EPS = 1e-5
GN_EPS = 1e-6
DEEPNORM_ALPHA = (2 * DEPTH) ** 0.25
DEEPNORM_BETA = (8 * DEPTH) ** -0.25
IN_SPLIT_SIZES = (ATTN_WIDTH, ATTN_WIDTH, ATTN_WIDTH, MLSTM_WIDTH, MLSTM_WIDTH, MLSTM_WIDTH, MLSTM_HEADS, MLSTM_HEADS, D_MODEL, D_MODEL)
IN_WIDTH = 3 * ATTN_WIDTH + 3 * MLSTM_WIDTH + 2 * MLSTM_HEADS + 2 * D_MODEL

kernel_name = 'hybrid_moba_mlstm_hmoe_deepnorm'


def layer_norm(x, g, b):
    xf = x.astype(jnp.float32)
    mu = jnp.mean(xf, axis=-1, keepdims=True)
    var = jnp.mean(jnp.square(xf - mu), axis=-1, keepdims=True)
    return ((xf - mu) * lax.rsqrt(var + LN_EPS) * g + b).astype(x.dtype)


def split_cols(z):
    outs = []
    off = 0
    for s in IN_SPLIT_SIZES:
        outs.append(z[..., off:off + s])
        off += s
    return outs


def apply_rope(x, pos):
    half = x.shape[-1] // 2
    inv_freq = ROPE_THETA ** (-jnp.arange(half, dtype=jnp.float32) / half)
    ang = pos.astype(jnp.float32)[:, None] * inv_freq[None, :]
    cos = jnp.cos(ang)[None, :, None, :]
    sin = jnp.sin(ang)[None, :, None, :]
    xf = x.astype(jnp.float32)
    x1, x2 = xf[..., :half], xf[..., half:]
    return jnp.concatenate([x1 * cos - x2 * sin, x2 * cos + x1 * sin], axis=-1).astype(x.dtype)


def moba_attention(q, k, v):
    B, S, H, hd = q.shape
    nb = -(-S // MOBA_BLOCK)
    sp = nb * MOBA_BLOCK
    G = B * H
    qb_len = MOBA_QUERY_BLOCK

    def to_groups(t):
        t = jnp.transpose(t, (0, 2, 1, 3)).reshape(G, S, hd)
        return jnp.pad(t, ((0, 0), (0, sp - S), (0, 0)))

    qg = to_groups(q) * (hd ** -0.5)
    kg = to_groups(k)
    vg = to_groups(v)
    kb = kg.reshape(G, nb, MOBA_BLOCK, hd)
    vb = vg.reshape(G, nb, MOBA_BLOCK, hd)
    kmean = jnp.mean(kb.astype(jnp.float32), axis=2)
    gate = jnp.einsum('gtd,gnd->gtn', qg.astype(jnp.float32), kmean)
    q_blk = jnp.arange(sp) // MOBA_BLOCK
    fully_past = jnp.arange(nb)[None, :] < q_blk[:, None]
    gate = jnp.where(fully_past[None], gate, -jnp.inf)
    n_sel = min(MOBA_TOPK, nb)
    g_val, g_idx = lax.top_k(gate, n_sel)
    g_valid = jnp.isfinite(g_val)

    nq = sp // qb_len

    def chunks(t):
        return jnp.moveaxis(t.reshape((G, nq, qb_len) + t.shape[2:]), 1, 0)

    xs = (chunks(qg), chunks(g_idx), chunks(g_valid), jnp.arange(nq, dtype=jnp.int32) * qb_len)
    g_ar = jnp.arange(G)[:, None, None]

    def query_block(args):
        qc, ic, mc, start = args
        qpos = start + jnp.arange(qb_len)
        own = (start // MOBA_BLOCK) * MOBA_BLOCK
        k_own = lax.dynamic_slice_in_dim(kg, own, MOBA_BLOCK, axis=1)
        v_own = lax.dynamic_slice_in_dim(vg, own, MOBA_BLOCK, axis=1)
        kpos = own + jnp.arange(MOBA_BLOCK)
        s_own = jnp.einsum('gqd,gkd->gqk', qc, k_own).astype(jnp.float32)
        s_own = jnp.where((kpos[None, :] <= qpos[:, None])[None], s_own, -jnp.inf)
        k_sel = kb[g_ar, ic]
        v_sel = vb[g_ar, ic]
        s_sel = jnp.einsum('gqd,gqnkd->gqnk', qc, k_sel).astype(jnp.float32)
        s_sel = jnp.where(mc[..., None], s_sel, -jnp.inf).reshape(G, qb_len, n_sel * MOBA_BLOCK)
        p = jax.nn.softmax(jnp.concatenate([s_sel, s_own], axis=-1), axis=-1)
        p_sel = p[..., :n_sel * MOBA_BLOCK].reshape(G, qb_len, n_sel, MOBA_BLOCK).astype(vg.dtype)
        p_own = p[..., n_sel * MOBA_BLOCK:].astype(vg.dtype)
        return jnp.einsum('gqnk,gqnkd->gqd', p_sel, v_sel) + jnp.einsum('gqk,gkd->gqd', p_own, v_own)

    out = lax.map(query_block, xs)
    out = jnp.moveaxis(out, 0, 1).reshape(G, sp, hd)[:, :S]
    return out.reshape(B, H, S, hd).transpose(0, 2, 1, 3).reshape(B, S, H * hd)


def causal_depthwise_conv(x, w, b):
    K, C = w.shape
    y = lax.conv_general_dilated(x, w[:, None, :], window_strides=(1,), padding=[(K - 1, 0)],
                                 dimension_numbers=('NWC', 'WIO', 'NWC'), feature_group_count=C)
    return y + b


def mlstm_chunk_step(carry, inp):
    c_state, n_state, m_state = carry
    q, k, v, ig, lf = inp
    L = q.shape[-2]
    causal = jnp.tril(jnp.ones((L, L), dtype=bool))
    b = jnp.cumsum(lf, axis=-1)
    d_log = b[..., :, None] - b[..., None, :] + ig[..., None, :]
    d_log = jnp.where(causal, d_log, -jnp.inf)
    inter_log = b + m_state[..., None]
    m_t = jnp.maximum(inter_log, jnp.max(d_log, axis=-1))
    w_intra = jnp.exp(d_log - m_t[..., None])
    w_inter = jnp.exp(inter_log - m_t)
    s = jnp.einsum('bhtd,bhsd->bhts', q, k) * w_intra
    num = w_inter[..., None] * jnp.einsum('bhtd,bhde->bhte', q, c_state) + jnp.einsum('bhts,bhse->bhte', s, v)
    den = w_inter * jnp.einsum('bhtd,bhd->bht', q, n_state) + jnp.sum(s, axis=-1)
    h = num / jnp.maximum(jnp.abs(den), jnp.exp(-m_t))[..., None]
    b_end = b[..., -1]
    w_log = b_end[..., None] - b + ig
    m_new = jnp.maximum(b_end + m_state, jnp.max(w_log, axis=-1))
    decay = jnp.exp(b_end + m_state - m_new)
    w_state = jnp.exp(w_log - m_new[..., None])
    c_new = decay[..., None, None] * c_state + jnp.einsum('bhs,bhsd,bhse->bhde', w_state, k, v)
    n_new = decay[..., None] * n_state + jnp.einsum('bhs,bhsd->bhd', w_state, k)
    return (c_new, n_new, m_new), h


def mlstm_chunkwise(q, k, v, ig, lf):
    B, H, S, d = q.shape
    nc = S // MLSTM_CHUNK

    def chunks(t):
        return jnp.moveaxis(t.reshape((B, H, nc, MLSTM_CHUNK) + t.shape[3:]), 2, 0)

    init = (jnp.zeros((B, H, d, d), q.dtype), jnp.zeros((B, H, d), q.dtype), jnp.zeros((B, H), q.dtype))
    _, hs = lax.scan(mlstm_chunk_step, init, (chunks(q), chunks(k), chunks(v), chunks(ig), chunks(lf)))
    return jnp.moveaxis(hs, 0, 2).reshape(B, H, S, d)


def mlstm_branch(u, v, o_pre, i_pre, f_pre, conv_w, conv_b, w_mq, w_mk, b_i, b_f, gn_g, skip):
    B, S, _ = u.shape
    f32 = jnp.float32
    u_c = jax.nn.silu(causal_depthwise_conv(u, conv_w, conv_b))
    uh = u_c.reshape(B, S, MLSTM_HEADS, MLSTM_HEAD_DIM)
    q = jnp.einsum('bshd,hde->bhse', uh, w_mq).astype(f32)
    k = (jnp.einsum('bshd,hde->bhse', uh, w_mk) * (MLSTM_HEAD_DIM ** -0.5)).astype(f32)
    vh = v.reshape(B, S, MLSTM_HEADS, MLSTM_HEAD_DIM).transpose(0, 2, 1, 3).astype(f32)
    ig = jnp.transpose((i_pre + b_i).astype(f32), (0, 2, 1))
    lf = jnp.transpose(jax.nn.log_sigmoid((f_pre + b_f).astype(f32)), (0, 2, 1))
    h = mlstm_chunkwise(q, k, vh, ig, lf).transpose(0, 2, 1, 3)
    o = jax.nn.sigmoid(o_pre.astype(f32)).reshape(B, S, MLSTM_HEADS, MLSTM_HEAD_DIM)
    h = o * h
    mu = jnp.mean(h, axis=-1, keepdims=True)
    var = jnp.mean(jnp.square(h - mu), axis=-1, keepdims=True)
    h = ((h - mu) * lax.rsqrt(var + GN_EPS)).reshape(B, S, MLSTM_WIDTH) * gn_g + skip * u_c
    return h.astype(u.dtype)


def hierarchical_moe(x, w_rg, b_rg, w_re, b_re, w_gate, w_up, w_down):
    B, S, D = x.shape
    N = B * S
    xf = x.reshape(N, D)
    g_prob = jax.nn.softmax((xf @ w_rg + b_rg).astype(jnp.float32), axis=-1)
    g_w, g_idx = lax.top_k(g_prob, 1)
    e_logits = (xf @ w_re + b_re).astype(jnp.float32).reshape(N, MOE_GROUPS, MOE_EXPERTS_PER_GROUP)
    e_logits = jnp.take_along_axis(e_logits, g_idx[:, :, None], axis=1)[:, 0]
    e_val, e_idx = lax.top_k(e_logits, MOE_TOPK)
    weights = g_w * jax.nn.softmax(e_val, axis=-1)
    expert = g_idx * MOE_EXPERTS_PER_GROUP + e_idx
    A = N * MOE_TOPK
    eid = expert.reshape(A)
    tok = jnp.repeat(jnp.arange(N, dtype=jnp.int32), MOE_TOPK)
    wt = weights.reshape(A)
    order = jnp.argsort(eid)
    e_s, t_s, w_s = eid[order], tok[order], wt[order]
    counts = jnp.bincount(eid, length=MOE_EXPERTS)
    padded = ((counts + MOE_BLOCK - 1) // MOE_BLOCK) * MOE_BLOCK
    pad_end = jnp.cumsum(padded)
    pad_start = pad_end - padded
    start = jnp.cumsum(counts) - counts
    dest = pad_start[e_s] + jnp.arange(A) - start[e_s]
    nblk = -(-A // MOE_BLOCK) + MOE_EXPERTS
    P = nblk * MOE_BLOCK
    tok_pad = jnp.zeros((P,), jnp.int32).at[dest].set(t_s)
    w_pad = jnp.zeros((P,), w_s.dtype).at[dest].set(w_s)
    blk_expert = jnp.minimum(jnp.searchsorted(pad_end, jnp.arange(nblk) * MOE_BLOCK, side='right'), MOE_EXPERTS - 1)

    def expert_block(args):
        tb, e = args
        xb = xf[tb]
        hb = jax.nn.silu(xb @ w_gate[e]) * (xb @ w_up[e])
        return hb @ w_down[e]

    yb = lax.map(expert_block, (tok_pad.reshape(nblk, MOE_BLOCK), blk_expert))
    y = jnp.zeros((N, D), yb.dtype).at[tok_pad].add(yb.reshape(P, D) * w_pad[:, None].astype(yb.dtype))
    return y.reshape(B, S, D)


def setup_inputs(seed: int = 0) -> dict:
    key = jax.random.key(seed)
    ks = jax.random.split(key, 26)
    f32 = jnp.float32
    L = DEPTH

    def nrm(k, shape, scale):
        return jax.random.normal(k, shape, f32) * scale

    return {
        'x': nrm(ks[0], (BATCH, SEQ, D_MODEL), 1.0),
        'ln0_g': 1.0 + nrm(ks[1], (D_MODEL,), 0.01),
        'ln0_b': nrm(ks[2], (D_MODEL,), 0.01),
        'w_in': nrm(ks[3], (L, D_MODEL, IN_WIDTH), D_MODEL ** -0.5),
        'conv_w': nrm(ks[4], (L, MLSTM_CONV, MLSTM_WIDTH), MLSTM_CONV ** -0.5),
        'conv_b': nrm(ks[5], (L, MLSTM_WIDTH), 0.01),
        'w_mq': nrm(ks[6], (L, MLSTM_HEADS, MLSTM_HEAD_DIM, MLSTM_HEAD_DIM), MLSTM_HEAD_DIM ** -0.5),
        'w_mk': nrm(ks[7], (L, MLSTM_HEADS, MLSTM_HEAD_DIM, MLSTM_HEAD_DIM), MLSTM_HEAD_DIM ** -0.5),
        'b_i': nrm(ks[8], (L, MLSTM_HEADS), 0.1),
        'b_f': jnp.linspace(3.0, 6.0, MLSTM_HEADS, dtype=f32)[None, :] + nrm(ks[9], (L, MLSTM_HEADS), 0.01),
        'gn_g': 1.0 + nrm(ks[10], (L, MLSTM_WIDTH), 0.01),
        'skip': 1.0 + nrm(ks[11], (L, MLSTM_WIDTH), 0.01),
        'w_attn_up': nrm(ks[12], (L, ATTN_WIDTH, D_MODEL), ATTN_WIDTH ** -0.5),
        'w_mlstm_up': nrm(ks[13], (L, MLSTM_WIDTH, D_MODEL), MLSTM_WIDTH ** -0.5),
        'w_out': nrm(ks[14], (L, D_MODEL, D_MODEL), DEEPNORM_BETA * D_MODEL ** -0.5),
        'ln1_g': 1.0 + nrm(ks[15], (L, D_MODEL), 0.01),
        'ln1_b': nrm(ks[16], (L, D_MODEL), 0.01),
        'w_router_group': nrm(ks[17], (L, D_MODEL, MOE_GROUPS), D_MODEL ** -0.5),
        'b_router_group': nrm(ks[18], (L, MOE_GROUPS), 0.01),
        'w_router_expert': nrm(ks[19], (L, D_MODEL, MOE_EXPERTS), D_MODEL ** -0.5),
        'b_router_expert': nrm(ks[20], (L, MOE_EXPERTS), 0.01),
        'w_gate': nrm(ks[21], (L, MOE_EXPERTS, D_MODEL, MOE_D_FF), D_MODEL ** -0.5),
        'w_up': nrm(ks[22], (L, MOE_EXPERTS, D_MODEL, MOE_D_FF), D_MODEL ** -0.5),
        'w_down': nrm(ks[23], (L, MOE_EXPERTS, MOE_D_FF, D_MODEL), DEEPNORM_BETA * MOE_D_FF ** -0.5),
        'ln2_g': 1.0 + nrm(ks[24], (L, D_MODEL), 0.01),
        'ln2_b': nrm(ks[25], (L, D_MODEL), 0.01),
    }


def reference(x, ln0_g, ln0_b, w_in, conv_w, conv_b, w_mq, w_mk, b_i, b_f, gn_g, skip,
              w_attn_up, w_mlstm_up, w_out, ln1_g, ln1_b, w_router_group, b_router_group,
              w_router_expert, b_router_expert, w_gate, w_up, w_down, ln2_g, ln2_b):
    B, S, _ = x.shape
    pos = jnp.arange(S, dtype=jnp.int32)
    x = layer_norm(x, ln0_g, ln0_b)
    for l in range(DEPTH):
        z = x @ w_in[l]
        q_a, k_a, v_a, u_m, v_m, o_m, i_m, f_m, g_a, g_m = split_cols(z)
        q_a = apply_rope(q_a.reshape(B, S, ATTN_HEADS, ATTN_HEAD_DIM), pos)
        k_a = apply_rope(k_a.reshape(B, S, ATTN_HEADS, ATTN_HEAD_DIM), pos)
        v_a = v_a.reshape(B, S, ATTN_HEADS, ATTN_HEAD_DIM)
        y_a = moba_attention(q_a, k_a, v_a)
        y_m = mlstm_branch(u_m, v_m, o_m, i_m, f_m, conv_w[l], conv_b[l], w_mq[l], w_mk[l],
                           b_i[l], b_f[l], gn_g[l], skip[l])
        mix = jax.nn.sigmoid(g_a) * (y_a @ w_attn_up[l]) + jax.nn.sigmoid(g_m) * (y_m @ w_mlstm_up[l])
        x = layer_norm(DEEPNORM_ALPHA * x + mix @ w_out[l], ln1_g[l], ln1_b[l])
        ffn = hierarchical_moe(x, w_router_group[l], b_router_group[l], w_router_expert[l],
                               b_router_expert[l], w_gate[l], w_up[l], w_down[l])
        x = layer_norm(DEEPNORM_ALPHA * x + ffn, ln2_g[l], ln2_b[l])
    return x
```

```python
import contextlib
import numpy as np
import ml_dtypes
import concourse.bass as bass
import concourse.mybir as mybir
from concourse.bass_utils import run_bass_kernel_spmd

F32 = mybir.dt.float32
BF16 = mybir.dt.bfloat16
I32 = mybir.dt.int32
U32 = mybir.dt.uint32
AF = mybir.ActivationFunctionType
ALU = mybir.AluOpType
AX = mybir.AxisListType

D = 1024
S = 2048
NT = 16
INW = 5128
LN_EPS = 1e-5
GN_EPS = 1e-6
ALPHA = 2.0 ** 0.25
NEG = -30000.0


class Sched:
    def __init__(self, nc, stack, n_dma_sems=40):
        self.nc = nc
        self.E = {'pe': nc.tensor, 'act': nc.scalar, 'dve': nc.vector, 'pool': nc.gpsimd, 'sp': nc.sync}
        self.csem = {e: stack.enter_context(nc.semaphore("c_" + e)) for e in ('pe', 'act', 'dve', 'pool')}
        self.cnt = {e: 0 for e in self.csem}
        self.seen = {e: {} for e in self.E}
        self.lastw = {}
        self.readers = {}
        self.dsems = [stack.enter_context(nc.semaphore("d%d" % i)) for i in range(n_dma_sems)]
        self.dval = [0] * n_dma_sems
        self.dnext = 0
        self.nwait = 0

    def _deps(self, reads, writes):
        toks = []
        for k in reads:
            if k in self.lastw:
                toks.append(self.lastw[k] + (False,))
            if isinstance(k, tuple) and k[0] == 'bank':
                for t in self.readers.get(k, ()):
                    toks.append(t + (True,))
        for k in writes:
            if k in self.lastw:
                toks.append(self.lastw[k] + (False,))
            for t in self.readers.get(k, ()):
                toks.append(t + (True,))
        return toks

    def _wait(self, e, toks):
        eng = self.E[e]
        need = {}
        for (sem, val, owner, war) in toks:
            if owner == e and (e == 'pe' or war):
                continue
            key = id(sem)
            if self.seen[e].get(key, 0) >= val:
                continue
            if key not in need or need[key][1] < val:
                need[key] = (sem, val)
        for key, (sem, val) in need.items():
            eng.wait_ge(sem, val)
            self.seen[e][key] = val
            self.nwait += 1

    def _record(self, tok, reads, writes):
        for k in reads:
            self.readers.setdefault(k, []).append(tok)
        for k in writes:
            self.lastw[k] = tok
            self.readers[k] = []

    def op(self, e, fn, reads=(), writes=(), inc=True, ww=()):
        self._wait(e, self._deps(reads, list(writes) + list(ww)))
        ins = fn(self.E[e])
        n = self.cnt[e] + 1
        tok = (self.csem[e], n, e)
        if inc:
            ins.then_inc(self.csem[e], 1)
            self.cnt[e] = n
        self._record(tok, reads, writes)
        return ins

    def dma(self, q, out, in_, reads=(), writes=(), indirect=None, **kw):
        i = self.dnext % len(self.dsems)
        self.dnext += 1
        sem, val = self.dsems[i], self.dval[i]
        toks = self._deps(reads, writes)
        if val > 0:
            toks.append((sem, val, None, False))
        self._wait(q, toks)
        if indirect is None:
            ins = self.E[q].dma_start(out=out, in_=in_, **kw)
        else:
            ins = self.E[q].indirect_dma_start(out=out, in_=in_, **indirect)
        ins.then_inc(sem, 16)
        self.dval[i] = val + 16
        tok = (sem, val + 16, None)
        self._record(tok, reads, writes)
        return ins

    def barrier(self):
        toks = []
        for i, sem in enumerate(self.dsems):
            if self.dval[i] > 0:
                toks.append((sem, self.dval[i], None, False))
        for e, sem in self.csem.items():
            if self.cnt[e] > 0:
                toks.append((sem, self.cnt[e], None, False))
        for e in self.E:
            self._wait(e, [t for t in toks])

    def finish(self):
        toks = []
        for i, sem in enumerate(self.dsems):
            if self.dval[i] > 0:
                toks.append((sem, self.dval[i], None, False))
        for e, sem in self.csem.items():
            if self.cnt[e] > 0:
                toks.append((sem, self.cnt[e], None, False))
        self._wait('sp', toks)


def build_program(dbg=None):
    dbg = dbg or set()
    nc = bass.Bass("TRN2", target_bir_lowering=False)
    stack = contextlib.ExitStack()
    with stack:
        _emit(nc, stack, dbg)
    return nc


def _emit(nc, stack, dbg):
    def din(name, shape, dt=F32):
        return nc.dram_tensor(name, list(shape), dt, kind="ExternalInput").ap()

    def dout(name, shape, dt=F32):
        return nc.dram_tensor(name, list(shape), dt, kind="ExternalOutput").ap()

    def sb(name, shape, dt):
        return stack.enter_context(nc.sbuf_tensor(name, list(shape), dt))

    x_d = din("x", [S, D])
    ln0g_d = din("ln0_g", [D])
    ln0b_d = din("ln0_b", [D])
    win_d = din("w_in", [D, INW])
    identb_d = din("ident_bf", [128, 128], BF16)
    cos_d = din("cos_t", [S, 32])
    sin_d = din("sin_t", [S, 32])
    tri_d = din("tri_bf", [128, 128], BF16)
    blk_d = din("blkind", [8, S], BF16)
    out_d = dout("out", [S, D])

    sc = Sched(nc, stack)

    ps = stack.enter_context(nc.psum_tensor("ps", [128, 4096], F32))
    identb = sb("identb", [128, 128], BF16)
    x0T = sb("x0T", [128, 8, S], BF16)
    def bank(b, n=512, off=0):
        return ps[:, b * 512 + off: b * 512 + off + n]

    def bankbf(b):
        return ps[:, b * 512:(b + 1) * 512].bitcast(BF16)

    bank_rr = [0]

    def next_bank():
        b = bank_rr[0] % 8
        bank_rr[0] += 1
        return b

    sc.dma('sp', identb[:], identb_d[:, :], writes=['identb'])
    x0_d = nc.dram_tensor("x0_scr", [S, D], F32, kind="Internal").ap()

    st6 = [sb("st6_%d" % i, [128, 2, 6], F32) for i in range(2)]
    mv = [sb("mv%d" % i, [128, 2], F32) for i in range(2)]
    rs = [sb("rs%d" % i, [128, 2], F32) for i in range(2)]
    eps_t = sb("eps_t", [128, 2], F32)
    ph1 = contextlib.ExitStack()

    def sb1(name, shape, dt):
        return ph1.enter_context(nc.sbuf_tensor(name, list(shape), dt))

    g0B = sb1("g0B", [128, D], F32)
    b0B = sb1("b0B", [128, D], F32)
    sc.dma('sp', g0B[:], ln0g_d.partition_broadcast(128), writes=['g0B'])
    sc.dma('sp', b0B[:], ln0b_d.partition_broadcast(128), writes=['b0B'])
    xt = [sb1("xt%d" % i, [128, D], F32) for i in range(2)]
    xn = [sb1("xn%d" % i, [128, D], F32) for i in range(2)]
    xb = [sb1("xb%d" % i, [128, D], BF16) for i in range(2)]

    def layernorm_stats(src, p, tag):
        for hh in range(2):
            sc.op('dve', lambda e, hh=hh: e.bn_stats(out=st6[p][:, hh, :], in_=src[:, hh * 512:(hh + 1) * 512]),
                  reads=[tag], writes=[('st6', p, hh)])
        sc.op('dve', lambda e: e.bn_aggr(out=mv[p][:], in_=st6[p][:].rearrange("p a b -> p (a b)")),
              reads=[('st6', p, 0), ('st6', p, 1)], writes=[('mv', p)])
        sc.op('act', lambda e: e.activation(out=rs[p][:, 0:1], in_=mv[p][:, 1:2], func=AF.Sqrt, bias=eps_t[:, 0:1], scale=1.0),
              reads=[('mv', p), 'eps'], writes=[('rs0', p)])
        sc.op('dve', lambda e: e.reciprocal(out=rs[p][:, 1:2], in_=rs[p][:, 0:1]),
              reads=[('rs0', p)], writes=[('rs', p)])

    sc.op('dve', lambda e: e.memset(eps_t[:, 0:1], LN_EPS), writes=['eps'])
    sc.op('dve', lambda e: e.memset(eps_t[:, 1:2], GN_EPS), writes=['eps'])

    for i in range(NT):
        p = i % 2
        sc.dma('sp', xt[p][:], x_d[i * 128:(i + 1) * 128, :], writes=[('xt', p)])
        layernorm_stats(xt[p], p, ('xt', p))
        sc.op('dve', lambda e: e.tensor_scalar(out=xn[p][:], in0=xt[p][:], scalar1=mv[p][:, 0:1], scalar2=rs[p][:, 1:2],
                                               op0=ALU.subtract, op1=ALU.mult),
              reads=[('xt', p), ('mv', p), ('rs', p)], writes=[('xn', p)])
        sc.op('pool', lambda e: e.tensor_tensor(out=xn[p][:], in0=xn[p][:], in1=g0B[:], op=ALU.mult),
              reads=[('xn', p), 'g0B'], writes=[('xn', p)])
        sc.op('pool', lambda e: e.tensor_tensor(out=xn[p][:], in0=xn[p][:], in1=b0B[:], op=ALU.add),
              reads=[('xn', p), 'b0B'], writes=[('xn', p)])
        sc.dma('sp', x0_d[i * 128:(i + 1) * 128, :], xn[p][:], reads=[('xn', p)], writes=[('x0_d', i)])
        sc.op('act', lambda e: e.copy(out=xb[p][:], in_=xn[p][:]), reads=[('xn', p)], writes=[('xb', p)])
        b = next_bank()
        for kc in range(8):
            sc.op('pe', lambda e, kc=kc: e.transpose(out=bankbf(b)[:, kc * 128:(kc + 1) * 128],
                                                     in_=xb[p][:, kc * 128:(kc + 1) * 128], identity=identb[:]),
                  reads=[('xb', p), 'identb'], writes=[('bank', b)] if kc == 7 else [], ww=[('bank', b)], inc=(kc == 7))
        sc.op('dve', lambda e: e.tensor_copy(out=x0T[:, :, i * 128:(i + 1) * 128],
                                             in_=bankbf(b).rearrange("p (c t) -> p c t", c=8)),
              reads=[('bank', b)], writes=[('x0T', i)])

    if 'x0T' in dbg:
        d = dout("dbg_x0T", [128, 8, S], BF16)
        sc.dma('sp', d[:, :, :], x0T[:], reads=[('x0T', i) for i in range(NT)])

    if 'stage1_only' in dbg:
        for i in range(NT):
            p = i % 2
            sc.dma('sp', xt[p][:], x0_d[i * 128:(i + 1) * 128, :], reads=[('x0_d', i)], writes=[('xt', p)])
            sc.dma('sp', out_d[i * 128:(i + 1) * 128, :], xt[p][:], reads=[('xt', p)], writes=[('out', i)])
        sc.finish()
        return


    sc.barrier()
    ph1.close()
    y_aT = sb("y_aT", [128, 4, S], BF16)
    win_v = win_d.rearrange("(kc p) n -> p kc n", p=128)

    with contextlib.ExitStack() as ph:
        def sbp(name, shape, dt):
            return ph.enter_context(nc.sbuf_tensor(name, list(shape), dt))

        wqkv = sbp("wqkv", [128, 8, 1536], BF16)
        for g in range(3):
            sc.dma('pool', wqkv[:, :, g * 512:(g + 1) * 512], win_v[:, :, g * 512:(g + 1) * 512], writes=[('wqkv', g)])
        qT = sbp("qT", [72, 8, S], BF16)
        kT = sbp("kT", [72, 8, S], BF16)
        va = sbp("va", [128, NT, 8, 65], BF16)
        cosS = sbp("cosS", [128, NT, 32], F32)
        sinS = sbp("sinS", [128, NT, 32], F32)
        trib = sbp("trib", [128, 128], BF16)
        biasT = sbp("biasT", [64, S], BF16)
        sc.dma('sp', cosS[:], cos_d.rearrange("(i p) f -> p i f", p=128), writes=['cos'])
        sc.dma('sp', sinS[:], sin_d.rearrange("(i p) f -> p i f", p=128), writes=['sin'])
        sc.dma('sp', trib[:], tri_d[:, :], writes=['trib'])
        for h in range(8):
            sc.dma('sp', kT[64:72, h, :], blk_d[:, :], writes=[('kTaug', h)])
        sc.op('pool', lambda e: e.memset(va[:, :, :, 64:65], 1.0), writes=['va1'])
        tmp = [[sbp("rt%d_%d" % (a, b_), [128, 16, 32], F32) for b_ in range(4)] for a in range(2)]
        rot = [sbp("rot%d" % a, [128, 16, 64], BF16) for a in range(2)]

        for i in range(NT):
            p = i % 2
            for g in range(3):
                bk = (2 * p + g) if g < 2 else (4 + p)
                for kc in range(8):
                    sc.op('pe', lambda e, kc=kc: e.matmul(bank(bk), lhsT=x0T[:, kc, i * 128:(i + 1) * 128],
                                                         rhs=wqkv[:, kc, g * 512:(g + 1) * 512],
                                                         start=(kc == 0), stop=(kc == 7)),
                          reads=[('x0T', i), ('wqkv', g)], writes=[('bank', bk)] if kc == 7 else [], ww=[('bank', bk)], inc=(kc == 7))
            zz = ps[:, p * 1024:(p + 1) * 1024].rearrange("p (h d) -> p h d", d=64)
            zA, zB = zz[:, :, 0:32], zz[:, :, 32:64]
            cb = cosS[:, i, :].unsqueeze(1).broadcast_to([128, 16, 32])
            sb_ = sinS[:, i, :].unsqueeze(1).broadcast_to([128, 16, 32])
            bks = [('bank', 2 * p), ('bank', 2 * p + 1)]
            for t_i, (zin, tab, tk) in enumerate([(zA, cb, 'cos'), (zB, sb_, 'sin'), (zB, cb, 'cos'), (zA, sb_, 'sin')]):
                sc.op('dve', lambda e, zin=zin, tab=tab, t_i=t_i: e.tensor_tensor(out=tmp[p][t_i][:], in0=zin, in1=tab, op=ALU.mult),
                      reads=bks + [tk], writes=[('rt', p, t_i)])
            sc.op('pool', lambda e: e.tensor_tensor(out=rot[p][:, :, 0:32], in0=tmp[p][0][:], in1=tmp[p][1][:], op=ALU.subtract),
                  reads=[('rt', p, 0), ('rt', p, 1)], writes=[('rotA', p)])
            sc.op('pool', lambda e: e.tensor_tensor(out=rot[p][:, :, 32:64], in0=tmp[p][2][:], in1=tmp[p][3][:], op=ALU.add),
                  reads=[('rt', p, 2), ('rt', p, 3)], writes=[('rotB', p)])
            for qk in range(2):
                bt = 6 + qk
                for h in range(8):
                    sc.op('pe', lambda e, h=h: e.transpose(out=bankbf(bt)[0:64, h * 128:(h + 1) * 128],
                                                         in_=rot[p][:, qk * 8 + h, :], identity=identb[:]),
                          reads=[('rotA', p), ('rotB', p), 'identb'], writes=[('bank', bt)] if h == 7 else [], ww=[('bank', bt)], inc=(h == 7))
                src_v = bankbf(bt)[0:64, :].rearrange("p (h t) -> p h t", h=8)
                if qk == 0:
                    sc.op('act', lambda e: e.mul(out=qT[0:64, :, i * 128:(i + 1) * 128], in_=src_v, mul=0.125),
                          reads=[('bank', bt)], writes=[('qT', i)])
                else:
                    sc.op('act', lambda e: e.copy(out=kT[0:64, :, i * 128:(i + 1) * 128], in_=src_v),
                          reads=[('bank', bt)], writes=[('kT', i)])
            sc.op('act', lambda e: e.copy(out=va[:, i, :, 0:64], in_=bank(4 + p).rearrange("p (h d) -> p h d", d=64)),
                  reads=[('bank', 4 + p)], writes=[('va', i)])

        km32 = sbp("km32", [64, 8, 8], F32)
        kmb = sbp("kmb", [64, 8, 8], BF16)
        sc.op('dve', lambda e: e.tensor_reduce(out=km32[:], in_=kT[0:64, :, :].rearrange("p h (n k) -> p h n k", k=256),
                                               axis=AX.X, op=ALU.add),
              reads=[('kT', i) for i in range(NT)], writes=['km32'])
        sc.op('dve', lambda e: e.tensor_copy(out=kmb[:], in_=km32[:]), reads=['km32'], writes=['kmb'])
        G = [sbp("G%d" % a, [128, 8, 8], F32) for a in range(2)]
        cmpt = [sbp("cmp%d" % a, [128, 8, 8, 8], F32) for a in range(2)]
        rank = [sbp("rank%d" % a, [128, 8, 8], F32) for a in range(2)]
        biasb = [sbp("biasb%d" % a, [128, 8, 8], BF16) for a in range(2)]
        for i in range(NT):
            p = i % 2
            blk = i // 2
            bg = p
            for h in range(8):
                sc.op('pe', lambda e, h=h: e.matmul(bank(bg, 8, h * 8), lhsT=qT[0:64, h, i * 128:(i + 1) * 128], rhs=kmb[:, h, :],
                                                   start=True, stop=True),
                      reads=[('qT', i), 'kmb'], writes=[('bank', bg)] if h == 7 else [], ww=[('bank', bg)], inc=(h == 7))
            sc.op('act', lambda e: e.copy(out=G[p][:].rearrange("p h n -> p (h n)"), in_=bank(bg, 64)),
                  reads=[('bank', bg)], writes=[('G', p)])
            sc.op('pool', lambda e: e.memset(G[p][:, :, blk:8], -1e30), reads=[('G', p)], writes=[('G', p)])
            sc.op('dve', lambda e: e.tensor_tensor(out=cmpt[p][:], in0=G[p][:].unsqueeze(2).broadcast_to([128, 8, 8, 8]),
                                                   in1=G[p][:].unsqueeze(3).broadcast_to([128, 8, 8, 8]), op=ALU.is_gt),
                  reads=[('G', p)], writes=[('cmp', p)])
            sc.op('dve', lambda e: e.tensor_reduce(out=rank[p][:], in_=cmpt[p][:], axis=AX.X, op=ALU.add),
                  reads=[('cmp', p)], writes=[('rank', p)])
            sc.op('dve', lambda e: e.tensor_scalar(out=biasb[p][:], in0=rank[p][:], scalar1=3.0, scalar2=NEG,
                                                   op0=ALU.is_ge, op1=ALU.mult),
                  reads=[('rank', p)], writes=[('biasb', p)])
            sc.op('pool', lambda e: e.memset(biasb[p][:, :, blk:blk + 1], 0.0), reads=[('biasb', p)], writes=[('biasb', p)])
            bt = 2 + p
            sc.op('pe', lambda e: e.transpose(out=bankbf(bt)[0:64, 0:128], in_=biasb[p][:].rearrange("p h n -> p (h n)"),
                                              identity=identb[:]),
                  reads=[('biasb', p), 'identb'], writes=[('bank', bt)])
            sc.op('act', lambda e: e.copy(out=biasT[:, i * 128:(i + 1) * 128], in_=bankbf(bt)[0:64, 0:128]),
                  reads=[('bank', bt)], writes=[('biasT', i)])
        for h in range(8):
            sc.dma('sp', qT[64:72, h, :], biasT[h * 8:(h + 1) * 8, :], reads=[('biasT', i) for i in range(NT)],
                   writes=[('qTaug', h)])

        if 'qk' in dbg:
            dq = dout("dbg_qT", [72, 8, S], BF16)
            dk = dout("dbg_kT", [72, 8, S], BF16)
            sc.dma('sp', dq[:, :, :], qT[:], reads=[('qT', i) for i in range(NT)] + [('qTaug', h) for h in range(8)])
            sc.dma('sp', dk[:, :, :], kT[:], reads=[('kT', i) for i in range(NT)] + [('kTaug', h) for h in range(8)])

        PT = [sbp("PT%d" % a, [128, 4, 128], BF16) for a in range(3)]
        ya = [sbp("ya%d" % a, [128, 512], BF16) for a in range(2)]
        rden = [sbp("rden%d" % a, [128, 1], F32) for a in range(2)]
        st_rr = 0
        acc_rr = 0
        for qt in range(NT):
            yp = qt % 2
            for h in range(8):
                ab = 3 + (acc_rr % 2)
                ap_ = acc_rr % 2
                acc_rr += 1
                for g0 in range(0, qt + 1, 4):
                    js = list(range(g0, min(g0 + 4, qt + 1)))
                    sbk = st_rr % 3
                    st_rr += 1
                    for jj, j in enumerate(js):
                        last = (jj == len(js) - 1)
                        sc.op('pe', lambda e, jj=jj, j=j: e.matmul(bank(sbk, 128, jj * 128), lhsT=kT[0:72, h, j * 128:(j + 1) * 128],
                                                                 rhs=qT[0:72, h, qt * 128:(qt + 1) * 128], start=True, stop=True),
                              reads=[('kT', j), ('kTaug', h), ('qT', qt), ('qTaug', h)],
                              writes=[('bank', sbk)] if last else [], ww=[('bank', sbk)], inc=last)
                    n = len(js)
                    sc.op('act', lambda e: e.activation(out=PT[sbk][:, 0:n, :].rearrange("p a b -> p (a b)"), in_=bank(sbk, n * 128),
                                                        func=AF.Exp),
                          reads=[('bank', sbk)], writes=[('PT', sbk)])
                    if js[-1] == qt:
                        jj = len(js) - 1
                        sc.op('pool', lambda e: e.tensor_tensor(out=PT[sbk][:, jj, :], in0=PT[sbk][:, jj, :], in1=trib[:], op=ALU.mult),
                              reads=[('PT', sbk), 'trib'], writes=[('PT', sbk)])
                    for jj, j in enumerate(js):
                        last = (j == qt)
                        sc.op('pe', lambda e, jj=jj, j=j: e.matmul(bank(ab, 65), lhsT=PT[sbk][:, jj, :], rhs=va[:, j, h, :],
                                                                 start=(j == 0), stop=(j == qt)),
                              reads=[('PT', sbk), ('va', j), 'va1'], writes=[('bank', ab)] if last else [], ww=[('bank', ab)], inc=(last or jj == len(js) - 1))
                sc.op('dve', lambda e: e.reciprocal(out=rden[ap_][:], in_=bank(ab, 1, 64)), reads=[('bank', ab)], writes=[('rden', ap_)])
                sc.op('dve', lambda e: e.tensor_scalar(out=ya[yp][:, h * 64:(h + 1) * 64], in0=bank(ab, 64), scalar1=rden[ap_][:, 0:1],
                                                       scalar2=None, op0=ALU.mult),
                      reads=[('bank', ab), ('rden', ap_)], writes=[('ya', yp, h)])
            bt = 5
            for c in range(4):
                sc.op('pe', lambda e, c=c: e.transpose(out=bankbf(bt)[:, c * 128:(c + 1) * 128], in_=ya[yp][:, c * 128:(c + 1) * 128],
                                                     identity=identb[:]),
                      reads=[('ya', yp, h) for h in range(8)] + ['identb'], writes=[('bank', bt)] if c == 3 else [], ww=[('bank', bt)], inc=(c == 3))
            sc.op('act', lambda e: e.copy(out=y_aT[:, :, qt * 128:(qt + 1) * 128], in_=bankbf(bt)[:, 0:512].rearrange("p (c t) -> p c t", c=4)),
                  reads=[('bank', bt)], writes=[('y_aT', qt)])

    sc.barrier()
    if 'y_aT' in dbg:
        d = dout("dbg_y_aT", [128, 4, S], BF16)
        sc.dma('sp', d[:, :, :], y_aT[:], reads=[('y_aT', i) for i in range(NT)])

    y_mT = sb("y_mT", [128, 4, S], BF16)
    chanv_d = din("chanv", [128, 4, 7])
    wqk_d = din("wqk_m", [4, 128, 256])
    bif_d = din("b_if", [8])
    trif_d = din("tri_f32", [128, 128])
    onesf_d = din("ones_f32", [128, 128])
    with contextlib.ExitStack() as ph:
        def sbp(name, shape, dt):
            return ph.enter_context(nc.sbuf_tensor(name, list(shape), dt))

        chanv = sbp("chanv_s", [128, 4, 7], F32)
        wqk = sbp("wqk", [128, 4, 256], BF16)
        bifB = sbp("bifB", [128, 8], F32)
        trif = sbp("trif", [128, 128], F32)
        onesf = sbp("onesf", [128, 128], F32)
        trib = sbp("tribB", [128, 128], BF16)
        ones1 = sbp("ones1", [128, 1], F32)
        sc.dma('sp', chanv[:], chanv_d[:, :, :], writes=['chanv'])
        sc.dma('pool', wqk[:], wqk_d.rearrange("h p n -> p h n"), writes=['wqk'])
        sc.dma('sp', bifB[:], bif_d.partition_broadcast(128), writes=['bifB'])
        sc.dma('sp', trif[:], trif_d[:, :], writes=['trif'])
        sc.dma('sp', onesf[:], onesf_d[:, :], writes=['onesf'])
        sc.dma('sp', trib[:], tri_d[:, :], writes=['trib'])
        sc.op('dve', lambda e: e.memset(ones1[:], 1.0), writes=['ones1'])
        ucT = sbp("ucT", [128, 4, S], BF16)
        su = sbp("su", [128, 4, S], BF16)
        vm = sbp("vm", [128, NT, 4, 129], BF16)
        om = sbp("om", [128, NT, 512], BF16)
        pre = sbp("pre", [128, NT, 8], F32)
        sc.op('pool', lambda e: e.memset(vm[:, :, :, 128:129], 1.0), writes=['vm1'])

        with contextlib.ExitStack() as ph2:
            def sbq(name, shape, dt):
                return ph2.enter_context(nc.sbuf_tensor(name, list(shape), dt))
            wu = sbq("wu", [128, 8, 512], BF16)
            wvo = sbq("wvo", [128, 8, 1024], BF16)
            wif = sbq("wif", [128, 8, 8], BF16)
            sc.dma('pool', wu[:], win_v[:, :, 1536:2048], writes=['wu'])
            sc.dma('pool', wvo[:, :, 0:512], win_v[:, :, 2048:2560], writes=[('wvo', 0)])
            sc.dma('pool', wvo[:, :, 512:1024], win_v[:, :, 2560:3072], writes=[('wvo', 1)])
            sc.dma('pool', wif[:], win_v[:, :, 3072:3080], writes=['wif'])
            upad = [sbq("upad%d" % a, [128, S + 3], F32) for a in range(2)]
            cacc = [sbq("cacc%d" % a, [128, S], F32) for a in range(2)]
            for a in range(2):
                sc.op('pool', lambda e, a=a: e.memset(upad[a][:, 0:3], 0.0), writes=[('upadz', a)])
            for h in range(4):
                p = h % 2
                for tg in range(4):
                    b = next_bank()
                    for kc in range(8):
                        sc.op('pe', lambda e, kc=kc: e.matmul(bank(b), lhsT=wu[:, kc, h * 128:(h + 1) * 128],
                                                             rhs=x0T[:, kc, tg * 512:(tg + 1) * 512], start=(kc == 0), stop=(kc == 7)),
                              reads=['wu'] + [('x0T', tg * 4 + q) for q in range(4)], writes=[('bank', b)] if kc == 7 else [], ww=[('bank', b)], inc=(kc == 7))
                    sc.op('act', lambda e: e.copy(out=upad[p][:, 3 + tg * 512: 3 + (tg + 1) * 512], in_=bank(b)),
                          reads=[('bank', b)], writes=[('upad', p, tg)])
                ur = [('upad', p, tg) for tg in range(4)] + [('upadz', p)]
                sc.op('dve', lambda e: e.tensor_scalar(out=cacc[p][:], in0=upad[p][:, 0:S], scalar1=chanv[:, h, 0:1], scalar2=chanv[:, h, 4:5],
                                                       op0=ALU.mult, op1=ALU.add),
                      reads=ur + ['chanv'], writes=[('cacc', p)])
                for j in range(1, 4):
                    sc.op('dve', lambda e, j=j: e.scalar_tensor_tensor(out=cacc[p][:], in0=upad[p][:, j:S + j], scalar=chanv[:, h, j:j + 1],
                                                                       in1=cacc[p][:], op0=ALU.mult, op1=ALU.add),
                          reads=ur + ['chanv', ('cacc', p)], writes=[('cacc', p)])
                sc.op('act', lambda e: e.activation(out=ucT[:, h, :], in_=cacc[p][:], func=AF.Silu),
                      reads=[('cacc', p)], writes=[('ucT', h)])
                sc.op('pool', lambda e: e.tensor_scalar(out=su[:, h, :], in0=ucT[:, h, :], scalar1=chanv[:, h, 6:7], scalar2=None, op0=ALU.mult),
                      reads=[('ucT', h), 'chanv'], writes=[('su', h)])
            for i in range(NT):
                bs = []
                for g in range(3):
                    b = next_bank()
                    bs.append(b)
                    n = 512 if g < 2 else 8
                    for kc in range(8):
                        rhs = wvo[:, kc, g * 512:(g + 1) * 512] if g < 2 else wif[:, kc, :]
                        sc.op('pe', lambda e, kc=kc, rhs=rhs: e.matmul(bank(b, n), lhsT=x0T[:, kc, i * 128:(i + 1) * 128], rhs=rhs,
                                                                     start=(kc == 0), stop=(kc == 7)),
                              reads=[('x0T', i), ('wvo', g) if g < 2 else 'wif'], writes=[('bank', b)] if kc == 7 else [], ww=[('bank', b)], inc=(kc == 7))
                sc.op('act', lambda e: e.copy(out=vm[:, i, :, 0:128], in_=bank(bs[0]).rearrange("p (h d) -> p h d", d=128)),
                      reads=[('bank', bs[0])], writes=[('vm', i)])
                sc.op('act', lambda e: e.activation(out=om[:, i, :], in_=bank(bs[1]), func=AF.Sigmoid),
                      reads=[('bank', bs[1])], writes=[('om', i)])
                sc.op('dve', lambda e: e.tensor_tensor(out=pre[:, i, :], in0=bank(bs[2], 8), in1=bifB[:], op=ALU.add),
                      reads=[('bank', bs[2]), 'bifB'], writes=[('pre', i)])
        sc.barrier()
        if 'stopB1' in dbg:
            sc.finish()
            return

        Lall = sbp("Lall", [128, NT, 4], F32)
        eb = sbp("eb", [128, NT, 4], F32)
        e2 = sbp("e2", [128, NT, 4], F32)
        e3 = sbp("e3", [128, NT, 4], F32)
        dec = sbp("dec", [128, NT, 4], F32)
        a2 = sbp("a2", [128, NT, 4], F32)
        a3 = sbp("a3", [128, NT, 4], F32)
        prs = [('pre', i) for i in range(NT)]
        import os as _os
        _cut = int(_os.environ.get('GCUT', '99'))
        _real_op = sc.op
        _k = [0]

        def _cut_op(*a, **kw):
            _k[0] += 1
            if _k[0] <= _cut:
                return _real_op(*a, **kw)
            return None
        if 'stopB2' in dbg:
            sc.op = _cut_op
        sc.op('act', lambda e: e.activation(out=Lall[:], in_=pre[:, :, 4:8], func=AF.Exp, scale=-1.0), reads=prs, writes=['Lall'])
        sc.op('act', lambda e: e.activation(out=Lall[:], in_=Lall[:], func=AF.Ln, bias=ones1[:, 0:1], scale=1.0),
              reads=['Lall', 'ones1'], writes=['Lall'])
        bc, bg_ = next_bank(), next_bank()
        sc.op('pe', lambda e: e.matmul(bank(bc, 64), lhsT=trif[:], rhs=Lall[:].rearrange("p i h -> p (i h)"), start=True, stop=True),
              reads=['trif', 'Lall'], writes=[('bank', bc)])
        sc.op('pe', lambda e: e.matmul(bank(bg_, 64), lhsT=onesf[:], rhs=Lall[:].rearrange("p i h -> p (i h)"), start=True, stop=True),
              reads=['onesf', 'Lall'], writes=[('bank', bg_)])
        f64 = lambda t: t[:].rearrange("p i h -> p (i h)")
        sc.op('act', lambda e: e.activation(out=f64(eb), in_=bank(bc, 64), func=AF.Exp, scale=-1.0), reads=[('bank', bc)], writes=['eb'])
        sc.op('act', lambda e: e.activation(out=f64(dec), in_=bank(bg_, 64), func=AF.Exp, scale=-1.0), reads=[('bank', bg_)], writes=['dec'])
        sc.op('dve', lambda e: e.tensor_tensor(out=a2[:], in0=bank(bc, 64).rearrange("p (i h) -> p i h", h=4), in1=pre[:, :, 0:4], op=ALU.add),
              reads=[('bank', bc)] + prs, writes=['a2'])
        sc.op('dve', lambda e: e.tensor_tensor(out=f64(a3), in0=f64(a2), in1=bank(bg_, 64), op=ALU.subtract),
              reads=[('bank', bg_), 'a2'], writes=['a3'])
        sc.op('act', lambda e: e.activation(out=f64(e2), in_=f64(a2), func=AF.Exp), reads=['a2'], writes=['e2'])
        sc.op('act', lambda e: e.activation(out=f64(e3), in_=f64(a3), func=AF.Exp), reads=['a3'], writes=['e3'])
        KS = 128.0 ** -0.5
        sc.op('dve', lambda e: e.tensor_scalar(out=f64(e2), in0=f64(e2), scalar1=KS, scalar2=None, op0=ALU.mult), reads=['e2'], writes=['e2'])
        sc.op('dve', lambda e: e.tensor_scalar(out=f64(e3), in0=f64(e3), scalar1=KS, scalar2=None, op0=ALU.mult), reads=['e3'], writes=['e3'])

        if 'stopB2' in dbg:
            sc.op = _real_op
            sc.barrier()
            sc.finish()
            return
        S32 = sbp("S32", [128, 4, 129], F32)
        Sb = sbp("Sb", [128, 4, 129], BF16)
        sc.op('dve', lambda e: e.memset(S32[:], 0.0), writes=[('S32', h) for h in range(4)])
        sc.op('dve', lambda e: e.memset(Sb[:], 0.0), writes=[('Sb', h) for h in range(4)])
        NB = 3
        qs = [sbp("qs%d" % a, [128, 128], BF16) for a in range(NB)]
        k2 = [sbp("k2_%d" % a, [128, 128], BF16) for a in range(NB)]
        k3 = [sbp("k3_%d" % a, [128, 128], BF16) for a in range(NB)]
        qkT = [sbp("qkT%d" % a, [128, 2, 128], BF16) for a in range(NB)]
        PTm = [sbp("PTm%d" % a, [128, 128], BF16) for a in range(NB)]
        hh32 = [sbp("hh32_%d" % a, [128, 128], F32) for a in range(NB)]
        hn = [sbp("hn%d" % a, [128, 128], BF16) for a in range(NB)]
        gst = [sbp("gst%d" % a, [128, 6], F32) for a in range(NB)]
        gmv = [sbp("gmv%d" % a, [128, 2], F32) for a in range(NB)]
        grs = [sbp("grs%d" % a, [128, 4], F32) for a in range(NB)]
        it = 0
        for i in range(NT):
            for h in range(4):
                p = it % NB
                it += 1
                b_a = next_bank()
                sc.op('pe', lambda e: e.matmul(bank(b_a, 256), lhsT=ucT[:, h, i * 128:(i + 1) * 128], rhs=wqk[:, h, :], start=True, stop=True),
                      reads=[('ucT', h), 'wqk'], writes=[('bank', b_a)])
                sc.op('act', lambda e: e.activation(out=qs[p][:], in_=bank(b_a, 128, 0), func=AF.Copy, scale=eb[:, i, h:h + 1]),
                      reads=[('bank', b_a), 'eb'], writes=[('qs', p)])
                sc.op('act', lambda e: e.activation(out=k2[p][:], in_=bank(b_a, 128, 128), func=AF.Copy, scale=e2[:, i, h:h + 1]),
                      reads=[('bank', b_a), 'e2'], writes=[('k2', p)])
                sc.op('act', lambda e: e.activation(out=k3[p][:], in_=bank(b_a, 128, 128), func=AF.Copy, scale=e3[:, i, h:h + 1]),
                      reads=[('bank', b_a), 'e3'], writes=[('k3', p)])
                b_c = next_bank()
                sc.op('pe', lambda e: e.transpose(out=bankbf(b_c)[:, 0:128], in_=qs[p][:], identity=identb[:]),
                      reads=[('qs', p), 'identb'], inc=False, ww=[('bank', b_c)])
                sc.op('pe', lambda e: e.transpose(out=bankbf(b_c)[:, 128:256], in_=k2[p][:], identity=identb[:]),
                      reads=[('k2', p), 'identb'], writes=[('bank', b_c)])
                sc.op('dve', lambda e: e.tensor_copy(out=qkT[p][:].rearrange("p a t -> p (a t)"), in_=bankbf(b_c)[:, 0:256]),
                      reads=[('bank', b_c)], writes=[('qkT', p)])
                b_d = next_bank()
                sc.op('pe', lambda e: e.matmul(bank(b_d, 128), lhsT=qkT[p][:, 1, :], rhs=qkT[p][:, 0, :], start=True, stop=True),
                      reads=[('qkT', p)], writes=[('bank', b_d)])
                sc.op('dve', lambda e: e.tensor_tensor(out=PTm[p][:], in0=bank(b_d, 128), in1=trib[:], op=ALU.mult),
                      reads=[('bank', b_d), 'trib'], writes=[('PTm', p)])
                b_f = next_bank()
                sc.op('pe', lambda e: e.matmul(bank(b_f, 129), lhsT=qkT[p][:, 0, :], rhs=Sb[:, h, :], start=True, stop=False),
                      reads=[('qkT', p), ('Sb', h)], inc=False, ww=[('bank', b_f)])
                sc.op('pe', lambda e: e.matmul(bank(b_f, 129), lhsT=PTm[p][:], rhs=vm[:, i, h, :], start=False, stop=True),
                      reads=[('PTm', p), ('vm', i), 'vm1'], writes=[('bank', b_f)])
                sc.op('act', lambda e: e.activation(out=grs[p][:, 0:1], in_=bank(b_f, 1, 128), func=AF.Abs),
                      reads=[('bank', b_f)], writes=[('grs0', p)])
                sc.op('dve', lambda e: e.tensor_scalar(out=grs[p][:, 0:1], in0=grs[p][:, 0:1], scalar1=1.0, scalar2=None, op0=ALU.max),
                      reads=[('grs0', p)], writes=[('grs0', p)])
                sc.op('dve', lambda e: e.reciprocal(out=grs[p][:, 1:2], in_=grs[p][:, 0:1]), reads=[('grs0', p)], writes=[('grs1', p)])
                sc.op('dve', lambda e: e.scalar_tensor_tensor(out=hh32[p][:], in0=bank(b_f, 128), scalar=grs[p][:, 1:2],
                                                              in1=om[:, i, h * 128:(h + 1) * 128], op0=ALU.mult, op1=ALU.mult),
                      reads=[('bank', b_f), ('grs1', p), ('om', i)], writes=[('hh32', p)])
                sc.op('dve', lambda e: e.bn_stats(out=gst[p][:], in_=hh32[p][:]), reads=[('hh32', p)], writes=[('gst', p)])
                sc.op('dve', lambda e: e.bn_aggr(out=gmv[p][:], in_=gst[p][:]), reads=[('gst', p)], writes=[('gmv', p)])
                sc.op('act', lambda e: e.activation(out=grs[p][:, 2:3], in_=gmv[p][:, 1:2], func=AF.Sqrt, bias=eps_t[:, 1:2], scale=1.0),
                      reads=[('gmv', p), 'eps'], writes=[('grs2', p)])
                sc.op('dve', lambda e: e.reciprocal(out=grs[p][:, 3:4], in_=grs[p][:, 2:3]), reads=[('grs2', p)], writes=[('grs3', p)])
                sc.op('dve', lambda e: e.tensor_scalar(out=hn[p][:], in0=hh32[p][:], scalar1=gmv[p][:, 0:1], scalar2=grs[p][:, 3:4],
                                                       op0=ALU.subtract, op1=ALU.mult),
                      reads=[('hh32', p), ('gmv', p), ('grs3', p)], writes=[('hn', p)])
                b_i = next_bank()
                sc.op('pe', lambda e: e.transpose(out=bankbf(b_i)[:, 0:128], in_=hn[p][:], identity=identb[:]),
                      reads=[('hn', p), 'identb'], writes=[('bank', b_i)])
                sc.op('dve', lambda e: e.scalar_tensor_tensor(out=y_mT[:, h, i * 128:(i + 1) * 128], in0=bankbf(b_i)[:, 0:128],
                                                              scalar=chanv[:, h, 5:6], in1=su[:, h, i * 128:(i + 1) * 128],
                                                              op0=ALU.mult, op1=ALU.add),
                      reads=[('bank', b_i), 'chanv', ('su', h)], writes=[('y_mT', i, h)])
                b_j = next_bank()
                sc.op('pe', lambda e: e.matmul(bank(b_j, 129), lhsT=k3[p][:], rhs=vm[:, i, h, :], start=True, stop=True),
                      reads=[('k3', p), ('vm', i), 'vm1'], writes=[('bank', b_j)])
                sc.op('dve', lambda e: e.scalar_tensor_tensor(out=S32[:, h, :], in0=S32[:, h, :], scalar=dec[:, i, h:h + 1],
                                                              in1=bank(b_j, 129), op0=ALU.mult, op1=ALU.add),
                      reads=[('S32', h), 'dec', ('bank', b_j)], writes=[('S32', h)])
                sc.op('act', lambda e: e.copy(out=Sb[:, h, :], in_=S32[:, h, :]), reads=[('S32', h)], writes=[('Sb', h)])

    sc.barrier()
    if 'y_mT' in dbg:
        d = dout("dbg_y_mT", [128, 4, S], BF16)
        sc.dma('sp', d[:, :, :], y_mT[:], reads=[('y_mT', i, h) for i in range(NT) for h in range(4)])

    CAP = 128
    YROWS = 2 * S + 128
    x1_d = nc.dram_tensor("x1_scr", [S, D], F32, kind="Internal").ap()
    tab_d = nc.dram_tensor("tab_scr", [64 * CAP, 4], F32, kind="Internal").ap()
    ybuf_d = nc.dram_tensor("ybuf_scr", [YROWS, D], F32, kind="Internal").ap()
    wau_d = din("w_attn_up", [512, D])
    wmu_d = din("w_mlstm_up", [512, D])
    wout_d = din("w_out", [D, D])
    ln1g_d = din("ln1_g", [D])
    ln1b_d = din("ln1_b", [D])
    wr_d = din("w_router", [D, 72])
    br_d = din("b_router", [72])
    identf_d = din("ident_f32", [128, 128])
    stri_d = din("stri_bf", [128, 128], BF16)
    onesb_d = din("ones_bf", [128, 128], BF16)
    iota64_d = din("iota64", [128, 64])
    tokid_d = din("tokid", [128, NT])
    tabinit_d = din("tab_init", [64 * CAP, 4])
    sc.dma('sp', tab_d[:, :], tabinit_d[:, :], writes=['tab_d'])

    with contextlib.ExitStack() as ph:
        def sbp(name, shape, dt):
            return ph.enter_context(nc.sbuf_tensor(name, list(shape), dt))
        mixT = sbp("mixT", [128, 8, S], BF16)
        phc = contextlib.ExitStack()
        _sbp_outer = sbp

        def sbp(name, shape, dt):
            return phc.enter_context(nc.sbuf_tensor(name, list(shape), dt))
        wg = sbp("wg", [128, 8, 2048], BF16)
        wau = sbp("wau", [128, 4, D], BF16)
        wmu = sbp("wmu", [128, 4, D], BF16)
        for g in range(4):
            sc.dma('pool', wg[:, :, g * 512:(g + 1) * 512], win_v[:, :, 3080 + g * 512: 3080 + (g + 1) * 512], writes=[('wg', g)])
        for g in range(2):
            sc.dma('pool', wau[:, :, g * 512:(g + 1) * 512], wau_d.rearrange("(c p) n -> p c n", p=128)[:, :, g * 512:(g + 1) * 512], writes=[('wau', g)])
            sc.dma('pool', wmu[:, :, g * 512:(g + 1) * 512], wmu_d.rearrange("(c p) n -> p c n", p=128)[:, :, g * 512:(g + 1) * 512], writes=[('wmu', g)])
        sg = [[sbp("sg%d_%d" % (a, b_), [128, 512], F32) for b_ in range(2)] for a in range(2)]
        it = 0
        for fc in range(8):
            for tg in range(4):
                p = it % 2
                it += 1
                bA, bB, bC, bD = 4 * p, 4 * p + 1, 4 * p + 2, 4 * p + 3
                xr = [('x0T', tg * 4 + q) for q in range(4)]
                for kc in range(8):
                    sc.op('pe', lambda e, kc=kc: e.matmul(bank(bA), lhsT=wg[:, kc, fc * 128:(fc + 1) * 128], rhs=x0T[:, kc, tg * 512:(tg + 1) * 512],
                                                         start=(kc == 0), stop=(kc == 7)),
                          reads=xr + [('wg', fc // 4)], writes=[('bank', bA)] if kc == 7 else [], ww=[('bank', bA)], inc=(kc == 7))
                for kc in range(8):
                    sc.op('pe', lambda e, kc=kc: e.matmul(bank(bB), lhsT=wg[:, kc, 1024 + fc * 128: 1024 + (fc + 1) * 128],
                                                         rhs=x0T[:, kc, tg * 512:(tg + 1) * 512], start=(kc == 0), stop=(kc == 7)),
                          reads=xr + [('wg', 2 + fc // 4)], writes=[('bank', bB)] if kc == 7 else [], ww=[('bank', bB)], inc=(kc == 7))
                for c in range(4):
                    sc.op('pe', lambda e, c=c: e.matmul(bank(bC), lhsT=wau[:, c, fc * 128:(fc + 1) * 128], rhs=y_aT[:, c, tg * 512:(tg + 1) * 512],
                                                       start=(c == 0), stop=(c == 3)),
                          reads=[('y_aT', tg * 4 + q) for q in range(4)] + [('wau', fc // 4)], writes=[('bank', bC)] if c == 3 else [],
                          ww=[('bank', bC)], inc=(c == 3))
                for c in range(4):
                    sc.op('pe', lambda e, c=c: e.matmul(bank(bD), lhsT=wmu[:, c, fc * 128:(fc + 1) * 128], rhs=y_mT[:, c, tg * 512:(tg + 1) * 512],
                                                       start=(c == 0), stop=(c == 3)),
                          reads=[('y_mT', tg * 4 + q, h) for q in range(4) for h in range(4)] + [('wmu', fc // 4)],
                          writes=[('bank', bD)] if c == 3 else [], ww=[('bank', bD)], inc=(c == 3))
                sc.op('act', lambda e: e.activation(out=sg[p][0][:], in_=bank(bA), func=AF.Sigmoid), reads=[('bank', bA)], writes=[('sg', p, 0)])
                sc.op('act', lambda e: e.activation(out=sg[p][1][:], in_=bank(bB), func=AF.Sigmoid), reads=[('bank', bB)], writes=[('sg', p, 1)])
                sc.op('dve', lambda e: e.tensor_tensor(out=sg[p][0][:], in0=sg[p][0][:], in1=bank(bC), op=ALU.mult),
                      reads=[('sg', p, 0), ('bank', bC)], writes=[('sg', p, 0)])
                sc.op('dve', lambda e: e.tensor_tensor(out=sg[p][1][:], in0=sg[p][1][:], in1=bank(bD), op=ALU.mult),
                      reads=[('sg', p, 1), ('bank', bD)], writes=[('sg', p, 1)])
                sc.op('pool', lambda e: e.tensor_tensor(out=mixT[:, fc, tg * 512:(tg + 1) * 512], in0=sg[p][0][:], in1=sg[p][1][:], op=ALU.add),
                      reads=[('sg', p, 0), ('sg', p, 1)], writes=[('mixT', fc, tg)])
        sc.barrier()
        phc.close()
        sbp = _sbp_outer

        wout = sbp("wout", [128, 8, D], BF16)
        for g in range(2):
            sc.dma('pool', wout[:, :, g * 512:(g + 1) * 512], wout_d.rearrange("(c p) n -> p c n", p=128)[:, :, g * 512:(g + 1) * 512], writes=[('wout', g)])
        g1B = sbp("g1B", [128, D], F32)
        b1B = sbp("b1B", [128, D], F32)
        wr = sbp("wr", [128, 8, 72], F32)
        brB = sbp("brB", [128, 72], F32)
        identf = sbp("identf", [128, 128], F32)
        strib = sbp("strib", [128, 128], BF16)
        onesb = sbp("onesb", [128, 128], BF16)
        iota64 = sbp("iota64s", [128, 64], F32)
        tokid = sbp("tokids", [128, NT], F32)
        sc.dma('sp', g1B[:], ln1g_d.partition_broadcast(128), writes=['g1B'])
        sc.dma('sp', b1B[:], ln1b_d.partition_broadcast(128), writes=['b1B'])
        sc.dma('sp', wr[:], wr_d.rearrange("(c p) n -> p c n", p=128), writes=['wr'])
        sc.dma('sp', brB[:], br_d.partition_broadcast(128), writes=['brB'])
        sc.dma('sp', identf[:], identf_d[:, :], writes=['identf'])
        sc.dma('sp', strib[:], stri_d[:, :], writes=['strib'])
        sc.dma('sp', onesb[:], onesb_d[:, :], writes=['onesb'])
        sc.dma('sp', iota64[:], iota64_d[:, :], writes=['iota64'])
        sc.dma('sp', tokid[:], tokid_d[:, :], writes=['tokid'])
        x0t = [sbp("x0t%d" % a, [128, D], F32) for a in range(2)]
        xp = [sbp("xp%d" % a, [128, D], F32) for a in range(2)]
        x1T = [sbp("x1T%d" % a, [128, 8, 128], F32) for a in range(2)]
        run = sbp("run", [128, 64], F32)
        sc.op('dve', lambda e: e.memset(run[:], 0.0), writes=['run'])

        def small(name, n, dt=F32):
            return [sbp("%s%d" % (name, a), [128, n], dt) for a in range(2)]
        lg = small("lg", 72)
        mx8 = small("mx8", 8)
        ohg = small("ohg", 8)
        exg = small("exg", 8)
        sc1 = small("sc1", 8)
        tmp88 = small("tmp88", 64)
        el = small("el", 8)
        mxe = small("mxe", 8)
        oh = [small("oh1_", 8), small("oh2_", 8)]
        Ac = [small("A1_", 64), small("A2_", 64)]
        Ab = small("Ab", 64, BF16)
        pos = small("pos", 64)
        prod = small("prod", 64)
        rowt = [small("row1_", 4), small("row2_", 4)]
        ridx = [small("ridx1_", 1, I32), small("ridx2_", 1, I32)]
        ridf = small("ridf", 2)

        for i in range(NT):
            p = i % 2
            sc.dma('sp', x0t[p][:], x0_d[i * 128:(i + 1) * 128, :], reads=[('x0_d', i)], writes=[('x0t', p)])
            bp = 2 * p
            for half in range(2):
                for kc in range(8):
                    sc.op('pe', lambda e, kc=kc: e.matmul(bank(bp + half), lhsT=mixT[:, kc, i * 128:(i + 1) * 128],
                                                         rhs=wout[:, kc, half * 512:(half + 1) * 512], start=(kc == 0), stop=(kc == 7)),
                          reads=[('mixT', kc, i // 4), ('wout', half)], writes=[('bank', bp + half)] if kc == 7 else [],
                          ww=[('bank', bp + half)], inc=(kc == 7))
            sc.op('dve', lambda e: e.scalar_tensor_tensor(out=xp[p][:], in0=x0t[p][:], scalar=ALPHA, in1=ps[:, bp * 512:(bp + 2) * 512],
                                                          op0=ALU.mult, op1=ALU.add),
                  reads=[('x0t', p), ('bank', bp), ('bank', bp + 1)], writes=[('xp', p)])
            layernorm_stats(xp[p], p, ('xp', p))
            sc.op('dve', lambda e: e.tensor_scalar(out=xp[p][:], in0=xp[p][:], scalar1=mv[p][:, 0:1], scalar2=rs[p][:, 1:2],
                                                   op0=ALU.subtract, op1=ALU.mult),
                  reads=[('xp', p), ('mv', p), ('rs', p)], writes=[('xp', p)])
            sc.op('pool', lambda e: e.tensor_tensor(out=xp[p][:], in0=xp[p][:], in1=g1B[:], op=ALU.mult),
                  reads=[('xp', p), 'g1B'], writes=[('xp', p)])
            sc.op('pool', lambda e: e.tensor_tensor(out=xp[p][:], in0=xp[p][:], in1=b1B[:], op=ALU.add),
                  reads=[('xp', p), 'b1B'], writes=[('xp', p)])
            sc.dma('sp', x1_d[i * 128:(i + 1) * 128, :], xp[p][:], reads=[('xp', p)], writes=[('x1_d', i)])
            bt = 4
            for kc in range(8):
                sc.op('pe', lambda e, kc=kc: e.transpose(out=ps[:, bt * 512 + kc * 128: bt * 512 + (kc + 1) * 128],
                                                         in_=xp[p][:, kc * 128:(kc + 1) * 128], identity=identf[:]),
                      reads=[('xp', p), 'identf'], writes=[('bank', bt), ('bank', bt + 1)] if kc == 7 else [],
                      ww=[('bank', bt), ('bank', bt + 1)], inc=(kc == 7))
            sc.op('act', lambda e: e.copy(out=x1T[p][:].rearrange("p c t -> p (c t)"), in_=ps[:, bt * 512:(bt + 2) * 512]),
                  reads=[('bank', bt), ('bank', bt + 1)], writes=[('x1T', p)])
            bl = 6
            for kc in range(8):
                sc.op('pe', lambda e, kc=kc: e.matmul(bank(bl, 72), lhsT=x1T[p][:, kc, :], rhs=wr[:, kc, :], start=(kc == 0), stop=(kc == 7)),
                      reads=[('x1T', p), 'wr'], writes=[('bank', bl)] if kc == 7 else [], ww=[('bank', bl)], inc=(kc == 7))
            sc.op('dve', lambda e: e.tensor_tensor(out=lg[p][:], in0=bank(bl, 72), in1=brB[:], op=ALU.add),
                  reads=[('bank', bl), 'brB'], writes=[('lg', p)])
            sc.op('dve', lambda e: e.max(out=mx8[p][:], in_=lg[p][:, 0:8]), reads=[('lg', p)], writes=[('mx8', p)])
            sc.op('dve', lambda e: e.tensor_scalar(out=ohg[p][:], in0=lg[p][:, 0:8], scalar1=mx8[p][:, 0:1], scalar2=None, op0=ALU.is_equal),
                  reads=[('lg', p), ('mx8', p)], writes=[('ohg', p)])
            sc.op('dve', lambda e: e.tensor_scalar(out=exg[p][:], in0=lg[p][:, 0:8], scalar1=mx8[p][:, 0:1], scalar2=None, op0=ALU.subtract),
                  reads=[('lg', p), ('mx8', p)], writes=[('exg', p)])
            sc.op('act', lambda e: e.activation(out=exg[p][:], in_=exg[p][:], func=AF.Exp), reads=[('exg', p)], writes=[('exg', p)])
            sc.op('dve', lambda e: e.tensor_reduce(out=sc1[p][:, 0:1], in_=exg[p][:], axis=AX.X, op=ALU.add), reads=[('exg', p)], writes=[('sc1a', p)])
            sc.op('dve', lambda e: e.reciprocal(out=sc1[p][:, 1:2], in_=sc1[p][:, 0:1]), reads=[('sc1a', p)], writes=[('gw', p)])
            sc.op('dve', lambda e: e.tensor_tensor(out=tmp88[p][:].rearrange("p (g e) -> p g e", e=8),
                                                   in0=lg[p][:, 8:72].rearrange("p (g e) -> p g e", e=8),
                                                   in1=ohg[p][:].unsqueeze(2).broadcast_to([128, 8, 8]), op=ALU.mult),
                  reads=[('lg', p), ('ohg', p)], writes=[('tmp88', p)])
            sc.op('dve', lambda e: e.tensor_reduce(out=el[p][:], in_=tmp88[p][:].rearrange("p (g e) -> p e g", e=8), axis=AX.X, op=ALU.add),
                  reads=[('tmp88', p)], writes=[('el', p)])
            sc.op('dve', lambda e: e.max(out=mxe[p][:], in_=el[p][:]), reads=[('el', p)], writes=[('mxe', p)])
            for c in range(2):
                sc.op('dve', lambda e, c=c: e.tensor_scalar(out=oh[c][p][:], in0=el[p][:], scalar1=mxe[p][:, c:c + 1], scalar2=None, op0=ALU.is_equal),
                      reads=[('el', p), ('mxe', p)], writes=[('oh', c, p)])
            sc.op('dve', lambda e: e.tensor_tensor(out=sc1[p][:, 2:3], in0=mxe[p][:, 1:2], in1=mxe[p][:, 0:1], op=ALU.subtract),
                  reads=[('mxe', p)], writes=[('sc1c', p)])
            sc.op('act', lambda e: e.activation(out=sc1[p][:, 2:3], in_=sc1[p][:, 2:3], func=AF.Exp), reads=[('sc1c', p)], writes=[('sc1c', p)])
            sc.op('dve', lambda e: e.tensor_scalar(out=sc1[p][:, 3:4], in0=sc1[p][:, 2:3], scalar1=1.0, scalar2=None, op0=ALU.add),
                  reads=[('sc1c', p)], writes=[('sc1d', p)])
            sc.op('dve', lambda e: e.reciprocal(out=sc1[p][:, 4:5], in_=sc1[p][:, 3:4]), reads=[('sc1d', p)], writes=[('p1', p)])
            sc.op('dve', lambda e: e.tensor_scalar(out=sc1[p][:, 5:6], in0=sc1[p][:, 4:5], scalar1=-1.0, scalar2=1.0, op0=ALU.mult, op1=ALU.add),
                  reads=[('p1', p)], writes=[('p2', p)])
            for c in range(2):
                sc.op('dve', lambda e, c=c: e.tensor_tensor(out=Ac[c][p][:].rearrange("p (g e) -> p g e", e=8),
                                                            in0=ohg[p][:].unsqueeze(2).broadcast_to([128, 8, 8]),
                                                            in1=oh[c][p][:].unsqueeze(1).broadcast_to([128, 8, 8]), op=ALU.mult),
                      reads=[('ohg', p), ('oh', c, p)], writes=[('Ac', c, p)])
            sc.op('dve', lambda e: e.tensor_tensor(out=Ab[p][:], in0=Ac[0][p][:], in1=Ac[1][p][:], op=ALU.add),
                  reads=[('Ac', 0, p), ('Ac', 1, p)], writes=[('Ab', p)])
            bq = 7
            sc.op('pe', lambda e: e.matmul(bank(bq, 64, 0), lhsT=strib[:], rhs=Ab[p][:], start=True, stop=True),
                  reads=['strib', ('Ab', p)], ww=[('bank', bq)], inc=False)
            sc.op('pe', lambda e: e.matmul(bank(bq, 64, 64), lhsT=onesb[:], rhs=Ab[p][:], start=True, stop=True),
                  reads=['onesb', ('Ab', p)], writes=[('bank', bq)])
            sc.op('dve', lambda e: e.tensor_tensor(out=pos[p][:], in0=bank(bq, 64, 0), in1=run[:], op=ALU.add),
                  reads=[('bank', bq), 'run'], writes=[('pos', p)])
            sc.op('dve', lambda e: e.tensor_tensor(out=run[:], in0=bank(bq, 64, 64), in1=run[:], op=ALU.add),
                  reads=[('bank', bq), 'run'], writes=['run'])
            for c in range(2):
                sc.op('dve', lambda e, c=c: e.tensor_tensor(out=prod[p][:], in0=Ac[c][p][:], in1=pos[p][:], op=ALU.mult),
                      reads=[('Ac', c, p), ('pos', p)], writes=[('prod', p)])
                sc.op('dve', lambda e: e.tensor_reduce(out=ridf[p][:, 0:1], in_=prod[p][:], axis=AX.X, op=ALU.add),
                      reads=[('prod', p)], writes=[('ridf0', p)])
                sc.op('dve', lambda e, c=c: e.tensor_tensor(out=prod[p][:], in0=Ac[c][p][:], in1=iota64[:], op=ALU.mult),
                      reads=[('Ac', c, p), 'iota64'], writes=[('prod', p)])
                sc.op('dve', lambda e: e.tensor_reduce(out=ridf[p][:, 1:2], in_=prod[p][:], axis=AX.X, op=ALU.add),
                      reads=[('prod', p)], writes=[('ridf1', p)])
                sc.op('dve', lambda e: e.scalar_tensor_tensor(out=ridf[p][:, 0:1], in0=ridf[p][:, 1:2], scalar=float(CAP), in1=ridf[p][:, 0:1],
                                                              op0=ALU.mult, op1=ALU.add),
                      reads=[('ridf0', p), ('ridf1', p)], writes=[('ridf0', p)])
                sc.op('dve', lambda e, c=c: e.tensor_copy(out=ridx[c][p][:], in_=ridf[p][:, 0:1]), reads=[('ridf0', p)], writes=[('ridx', c, p)])
                sc.op('dve', lambda e, c=c: e.tensor_copy(out=rowt[c][p][:, 0:1], in_=tokid[:, i:i + 1]), reads=['tokid'], writes=[('row', c, p)])
                sc.op('dve', lambda e, c=c: e.tensor_scalar(out=rowt[c][p][:, 1:2], in0=tokid[:, i:i + 1], scalar1=float(c * S), scalar2=None, op0=ALU.add),
                      reads=['tokid', ('row', c, p)], writes=[('row', c, p)])
                sc.op('dve', lambda e, c=c: e.tensor_tensor(out=rowt[c][p][:, 2:3], in0=sc1[p][:, 1:2], in1=sc1[p][:, 4 + c:5 + c], op=ALU.mult),
                      reads=[('gw', p), ('p1', p), ('p2', p), ('row', c, p)], writes=[('row', c, p)])
                sc.op('dve', lambda e, c=c: e.memset(rowt[c][p][:, 3:4], 0.0), reads=[('row', c, p)], writes=[('row', c, p)])
                sc.dma('pool', tab_d[:, :], rowt[c][p][:], reads=[('row', c, p), ('ridx', c, p), 'tab_d'], writes=[('tabw', i, c)],
                       indirect=dict(out_offset=bass.IndirectOffsetOnAxis(ap=ridx[c][p][:, 0:1], axis=0), in_offset=None))
    sc.barrier()
    if 'x1' in dbg:
        d = dout("dbg_x1", [S, D])
        sc.dma('sp', d[:, :], x1_d[:, :], reads=[('x1_d', i) for i in range(NT)])
        d2 = dout("dbg_tab", [64 * CAP, 4])
        sc.dma('sp', d2[:, :], tab_d[:, :], reads=[('tabw', i, c) for i in range(NT) for c in range(2)])
    if 'stopC' in dbg:
        sc.barrier()
        sc.finish()
        return

    wgate_d = din("w_gate", [64, D, 512])
    wup_d = din("w_up", [64, D, 512])
    wdown_d = din("w_down", [64, 512, D])
    ln2g_d = din("ln2_g", [D])
    ln2b_d = din("ln2_b", [D])
    with contextlib.ExitStack() as ph:
        def sbp(name, shape, dt):
            return ph.enter_context(nc.sbuf_tensor(name, list(shape), dt))
        tabS = sbp("tabS", [128, 64, 4], F32)
        gidx = sbp("gidx", [128, 64], I32)
        sidx = sbp("sidx", [128, 64], I32)
        tabw = [('tabw', i, c) for i in range(NT) for c in range(2)]
        tab_v = tab_d.rearrange("(e s) c -> s e c", s=CAP)
        for q in range(4):
            sc.dma('sp', tabS[:, q * 16:(q + 1) * 16, :], tab_v[:, q * 16:(q + 1) * 16, :], reads=tabw, writes=[('tabS', q)])
        tq = [('tabS', q) for q in range(4)]
        sc.op('dve', lambda e: e.tensor_copy(out=gidx[:], in_=tabS[:, :, 0]), reads=tq, writes=['gidx'])
        sc.op('dve', lambda e: e.tensor_copy(out=sidx[:], in_=tabS[:, :, 1]), reads=tq, writes=['sidx'])
        wge = [sbp("wge%d" % a, [128, 8, 512], BF16) for a in range(2)]
        wue = [sbp("wue%d" % a, [128, 8, 512], BF16) for a in range(2)]
        wde = [sbp("wde%d" % a, [128, 4, D], BF16) for a in range(2)]
        xg = [sbp("xg%d" % a, [128, D], BF16) for a in range(2)]
        xgT = [sbp("xgT%d" % a, [128, 8, 128], BF16) for a in range(2)]
        sil = [sbp("sil%d" % a, [128, 512], F32) for a in range(2)]
        hT = [sbp("hT%d" % a, [128, 4, 128], BF16) for a in range(2)]
        yw = [sbp("yw%d" % a, [128, D], F32) for a in range(2)]

        def load_expert(ex):
            p = ex % 2
            sc.dma('pool', xg[p][:], x1_d[:, :], reads=[('x1_d', i) for i in range(NT)] + ['gidx'], writes=[('xg', p)],
                   indirect=dict(out_offset=None, in_offset=bass.IndirectOffsetOnAxis(ap=gidx[:, ex:ex + 1], axis=0)))
            sc.dma('pool', wge[p][:], wgate_d[ex].rearrange("(c p) n -> p c n", p=128), writes=[('wge', p)])
            sc.dma('pool', wue[p][:], wup_d[ex].rearrange("(c p) n -> p c n", p=128), writes=[('wue', p)])
            for half in range(2):
                sc.dma('pool', wde[p][:, :, half * 512:(half + 1) * 512],
                       wdown_d[ex].rearrange("(c p) n -> p c n", p=128)[:, :, half * 512:(half + 1) * 512], writes=[('wde', p, half)])

        load_expert(0)
        for ex in range(64):
            p = ex % 2
            if ex + 1 < 64:
                load_expert(ex + 1)
            bt = p
            for kc in range(8):
                sc.op('pe', lambda e, kc=kc: e.transpose(out=bankbf(bt)[:, kc * 128:(kc + 1) * 128], in_=xg[p][:, kc * 128:(kc + 1) * 128],
                                                         identity=identb[:]),
                      reads=[('xg', p), 'identb'], writes=[('bank', bt)] if kc == 7 else [], ww=[('bank', bt)], inc=(kc == 7))
            sc.op('act', lambda e: e.copy(out=xgT[p][:].rearrange("p c t -> p (c t)"), in_=bankbf(bt)), reads=[('bank', bt)], writes=[('xgT', p)])
            bG, bU = 2 + p, 4 + p
            for (bk, wt, wk) in ((bG, wge, 'wge'), (bU, wue, 'wue')):
                for fcn in range(4):
                    for kc in range(8):
                        last = (fcn == 3 and kc == 7)
                        sc.op('pe', lambda e, kc=kc, fcn=fcn, wt=wt, bk=bk: e.matmul(bank(bk, 128, fcn * 128), lhsT=wt[p][:, kc, fcn * 128:(fcn + 1) * 128],
                                                                                   rhs=xgT[p][:, kc, :], start=(kc == 0), stop=(kc == 7)),
                              reads=[('xgT', p), (wk, p)], writes=[('bank', bk)] if last else [], ww=[('bank', bk)], inc=last)
            sc.op('act', lambda e: e.activation(out=sil[p][:], in_=bank(bG), func=AF.Silu), reads=[('bank', bG)], writes=[('sil', p)])
            sc.op('dve', lambda e: e.tensor_tensor(out=hT[p][:].rearrange("p c t -> p (c t)"), in0=sil[p][:], in1=bank(bU), op=ALU.mult),
                  reads=[('sil', p), ('bank', bU)], writes=[('hT', p)])
            for half in range(2):
                for fcn in range(4):
                    sc.op('pe', lambda e, fcn=fcn: e.matmul(bank(6 + half), lhsT=hT[p][:, fcn, :], rhs=wde[p][:, fcn, half * 512:(half + 1) * 512],
                                                           start=(fcn == 0), stop=(fcn == 3)),
                          reads=[('hT', p), ('wde', p, half)], writes=[('bank', 6 + half)] if fcn == 3 else [], ww=[('bank', 6 + half)], inc=(fcn == 3))
            sc.op('act', lambda e: e.activation(out=yw[p][:], in_=ps[:, 6 * 512:8 * 512], func=AF.Copy, scale=tabS[:, ex, 2:3]),
                  reads=[('bank', 6), ('bank', 7)] + tq, writes=[('yw', p)])
            sc.dma('pool', ybuf_d[:, :], yw[p][:], reads=[('yw', p), 'sidx'], writes=[('ybuf', ex)],
                   indirect=dict(out_offset=bass.IndirectOffsetOnAxis(ap=sidx[:, ex:ex + 1], axis=0), in_offset=None))
        sc.barrier()

        g2B = sbp("g2B", [128, D], F32)
        b2B = sbp("b2B", [128, D], F32)
        sc.dma('sp', g2B[:], ln2g_d.partition_broadcast(128), writes=['g2B'])
        sc.dma('sp', b2B[:], ln2b_d.partition_broadcast(128), writes=['b2B'])
        xa = [sbp("xa%d" % a, [128, D], F32) for a in range(2)]
        ya_ = [sbp("yA%d" % a, [128, D], F32) for a in range(2)]
        yb_ = [sbp("yB%d" % a, [128, D], F32) for a in range(2)]
        for i in range(NT):
            p = i % 2
            sc.dma('sp', xa[p][:], x1_d[i * 128:(i + 1) * 128, :], writes=[('xa', p)])
            sc.dma('sp', ya_[p][:], ybuf_d[i * 128:(i + 1) * 128, :], writes=[('yA', p)])
            sc.dma('sp', yb_[p][:], ybuf_d[S + i * 128: S + (i + 1) * 128, :], writes=[('yB', p)])
            sc.op('pool', lambda e: e.tensor_tensor(out=ya_[p][:], in0=ya_[p][:], in1=yb_[p][:], op=ALU.add),
                  reads=[('yA', p), ('yB', p)], writes=[('yA', p)])
            sc.op('dve', lambda e: e.scalar_tensor_tensor(out=xa[p][:], in0=xa[p][:], scalar=ALPHA, in1=ya_[p][:], op0=ALU.mult, op1=ALU.add),
                  reads=[('xa', p), ('yA', p)], writes=[('xa', p)])
            layernorm_stats(xa[p], p, ('xa', p))
            sc.op('dve', lambda e: e.tensor_scalar(out=xa[p][:], in0=xa[p][:], scalar1=mv[p][:, 0:1], scalar2=rs[p][:, 1:2],
                                                   op0=ALU.subtract, op1=ALU.mult),
                  reads=[('xa', p), ('mv', p), ('rs', p)], writes=[('xa', p)])
            sc.op('pool', lambda e: e.tensor_tensor(out=xa[p][:], in0=xa[p][:], in1=g2B[:], op=ALU.mult),
                  reads=[('xa', p), 'g2B'], writes=[('xa', p)])
            sc.op('pool', lambda e: e.tensor_tensor(out=xa[p][:], in0=xa[p][:], in1=b2B[:], op=ALU.add),
                  reads=[('xa', p), 'b2B'], writes=[('xa', p)])
            sc.dma('sp', out_d[i * 128:(i + 1) * 128, :], xa[p][:], reads=[('xa', p)], writes=[('out', i)])

    sc.finish()


def host_consts():
    c = {}
    c["ident_bf"] = np.eye(128, dtype=np.float32).astype(ml_dtypes.bfloat16)
    half = 32
    inv_freq = (10000.0 ** (-np.arange(half, dtype=np.float32) / half)).astype(np.float32)
    ang = np.arange(S, dtype=np.float32)[:, None] * inv_freq[None, :]
    c["cos_t"] = np.cos(ang).astype(np.float32)
    c["sin_t"] = np.sin(ang).astype(np.float32)
    pp = np.arange(128)
    c["tri_bf"] = (pp[None, :] >= pp[:, None]).astype(np.float32).astype(ml_dtypes.bfloat16)
    c["blkind"] = (np.arange(S)[None, :] // 256 == np.arange(8)[:, None]).astype(np.float32).astype(ml_dtypes.bfloat16)
    c["tri_f32"] = (pp[None, :] >= pp[:, None]).astype(np.float32)
    c["ones_f32"] = np.ones((128, 128), np.float32)
    c["ident_f32"] = np.eye(128, dtype=np.float32)
    c["stri_bf"] = (pp[None, :] > pp[:, None]).astype(np.float32).astype(ml_dtypes.bfloat16)
    c["ones_bf"] = np.ones((128, 128), np.float32).astype(ml_dtypes.bfloat16)
    c["iota64"] = np.tile(np.arange(64, dtype=np.float32)[None, :], (128, 1))
    c["tokid"] = (np.arange(NT, dtype=np.float32)[None, :] * 128 + pp[:, None]).astype(np.float32)
    ti = np.zeros((64 * 128, 4), np.float32)
    ti[:, 1] = 2 * S + np.tile(np.arange(128, dtype=np.float32), 64)
    c["tab_init"] = ti
    return c


def make_in_maps(inputs, n_cores=8):
    c = host_consts()
    maps = []
    for b in range(n_cores):
        m = dict(c)
        m["x"] = np.ascontiguousarray(inputs["x"][b])
        m["ln0_g"] = np.ascontiguousarray(inputs["ln0_g"])
        m["ln0_b"] = np.ascontiguousarray(inputs["ln0_b"])
        m["w_in"] = np.ascontiguousarray(inputs["w_in"][0])
        cv = np.concatenate([inputs["conv_w"][0], inputs["conv_b"][0][None], inputs["gn_g"][0][None], inputs["skip"][0][None]], axis=0)
        m["chanv"] = np.ascontiguousarray(cv.reshape(7, 4, 128).transpose(2, 1, 0))
        m["wqk_m"] = np.ascontiguousarray(np.concatenate([inputs["w_mq"][0], inputs["w_mk"][0]], axis=2))
        m["b_if"] = np.ascontiguousarray(np.concatenate([inputs["b_i"][0], inputs["b_f"][0]]))
        m["w_attn_up"] = np.ascontiguousarray(inputs["w_attn_up"][0])
        m["w_mlstm_up"] = np.ascontiguousarray(inputs["w_mlstm_up"][0])
        m["w_out"] = np.ascontiguousarray(inputs["w_out"][0])
        m["ln1_g"] = np.ascontiguousarray(inputs["ln1_g"][0])
        m["w_gate"] = np.ascontiguousarray(inputs["w_gate"][0])
        m["w_up"] = np.ascontiguousarray(inputs["w_up"][0])
        m["w_down"] = np.ascontiguousarray(inputs["w_down"][0])
        m["ln2_g"] = np.ascontiguousarray(inputs["ln2_g"][0])
        m["ln2_b"] = np.ascontiguousarray(inputs["ln2_b"][0])
        m["ln1_b"] = np.ascontiguousarray(inputs["ln1_b"][0])
        m["w_router"] = np.ascontiguousarray(np.concatenate([inputs["w_router_group"][0], inputs["w_router_expert"][0]], axis=1))
        m["b_router"] = np.ascontiguousarray(np.concatenate([inputs["b_router_group"][0], inputs["b_router_expert"][0]]))
        maps.append(m)
    return maps


def kernel(**inputs):
    inputs = {k: np.asarray(v) for k, v in inputs.items()}
    nc = build_program()
    maps = make_in_maps(inputs, 8)
    res = run_bass_kernel_spmd(nc, maps, core_ids=list(range(8)))
    return np.stack([r["out"] for r in res.results], axis=0).astype(np.float32)
```

```python
import contextlib
import numpy as np
import ml_dtypes
import concourse.bass as bass
import concourse.mybir as mybir
from concourse.bass_utils import run_bass_kernel_spmd

F32 = mybir.dt.float32
BF16 = mybir.dt.bfloat16
I32 = mybir.dt.int32
U32 = mybir.dt.uint32
AF = mybir.ActivationFunctionType
ALU = mybir.AluOpType
AX = mybir.AxisListType

D = 1024
S = 2048
NT = 16
INW = 5128
LN_EPS = 1e-5
GN_EPS = 1e-6
ALPHA = 2.0 ** 0.25
NEG = -30000.0


class Sched:
    def __init__(self, nc, stack, n_dma_sems=40):
        self.nc = nc
        self.E = {'pe': nc.tensor, 'act': nc.scalar, 'dve': nc.vector, 'pool': nc.gpsimd, 'sp': nc.sync}
        self.csem = {e: stack.enter_context(nc.semaphore("c_" + e)) for e in ('pe', 'act', 'dve', 'pool')}
        self.cnt = {e: 0 for e in self.csem}
        self.seen = {e: {} for e in self.E}
        self.lastw = {}
        self.readers = {}
        self.dsems = [stack.enter_context(nc.semaphore("d%d" % i)) for i in range(n_dma_sems)]
        self.dval = [0] * n_dma_sems
        self.dnext = 0
        self.nwait = 0

    def _deps(self, reads, writes):
        toks = []
        for k in reads:
            if k in self.lastw:
                toks.append(self.lastw[k] + (False,))
            if isinstance(k, tuple) and k[0] == 'bank':
                for t in self.readers.get(k, ()):
                    toks.append(t + (True,))
        for k in writes:
            if k in self.lastw:
                toks.append(self.lastw[k] + (False,))
            for t in self.readers.get(k, ()):
                toks.append(t + (True,))
        return toks

    def _wait(self, e, toks):
        eng = self.E[e]
        need = {}
        for (sem, val, owner, war) in toks:
            if owner == e and (e == 'pe' or war):
                continue
            key = id(sem)
            if self.seen[e].get(key, 0) >= val:
                continue
            if key not in need or need[key][1] < val:
                need[key] = (sem, val)
        for key, (sem, val) in need.items():
            eng.wait_ge(sem, val)
            self.seen[e][key] = val
            self.nwait += 1

    def _record(self, tok, reads, writes):
        for k in reads:
            self.readers.setdefault(k, []).append(tok)
        for k in writes:
            self.lastw[k] = tok
            self.readers[k] = []

    def op(self, e, fn, reads=(), writes=(), inc=True, ww=()):
        self._wait(e, self._deps(reads, list(writes) + list(ww)))
        ins = fn(self.E[e])
        n = self.cnt[e] + 1
        tok = (self.csem[e], n, e)
        if inc:
            ins.then_inc(self.csem[e], 1)
            self.cnt[e] = n
        self._record(tok, reads, writes)
        return ins

    def dma(self, q, out, in_, reads=(), writes=(), indirect=None, **kw):
        i = self.dnext % len(self.dsems)
        self.dnext += 1
        sem, val = self.dsems[i], self.dval[i]
        toks = self._deps(reads, writes)
        if val > 0:
            toks.append((sem, val, None, False))
        self._wait(q, toks)
        if indirect is None:
            ins = self.E[q].dma_start(out=out, in_=in_, **kw)
        else:
            ins = self.E[q].indirect_dma_start(out=out, in_=in_, **indirect)
        ins.then_inc(sem, 16)
        self.dval[i] = val + 16
        tok = (sem, val + 16, None)
        self._record(tok, reads, writes)
        return ins

    def barrier(self):
        toks = []
        for i, sem in enumerate(self.dsems):
            if self.dval[i] > 0:
                toks.append((sem, self.dval[i], None, False))
        for e, sem in self.csem.items():
            if self.cnt[e] > 0:
                toks.append((sem, self.cnt[e], None, False))
        for e in self.E:
            self._wait(e, [t for t in toks])

    def finish(self):
        toks = []
        for i, sem in enumerate(self.dsems):
            if self.dval[i] > 0:
                toks.append((sem, self.dval[i], None, False))
        for e, sem in self.csem.items():
            if self.cnt[e] > 0:
                toks.append((sem, self.cnt[e], None, False))
        self._wait('sp', toks)


def build_program(dbg=None):
    dbg = dbg or set()
    nc = bass.Bass("TRN2", target_bir_lowering=False)
    stack = contextlib.ExitStack()
    with stack:
        _emit(nc, stack, dbg)
    return nc


def _emit(nc, stack, dbg):
    def din(name, shape, dt=F32):
        return nc.dram_tensor(name, list(shape), dt, kind="ExternalInput").ap()

    def dout(name, shape, dt=F32):
        return nc.dram_tensor(name, list(shape), dt, kind="ExternalOutput").ap()

    def sb(name, shape, dt):
        return stack.enter_context(nc.sbuf_tensor(name, list(shape), dt))

    x_d = din("x", [S, D])
    ln0g_d = din("ln0_g", [D])
    ln0b_d = din("ln0_b", [D])
    win_d = din("w_in", [D, INW])
    identb_d = din("ident_bf", [128, 128], BF16)
    cos_d = din("cos_t", [S, 32])
    sin_d = din("sin_t", [S, 32])
    tri_d = din("tri_bf", [128, 128], BF16)
    blk_d = din("blkind", [8, S], BF16)
    out_d = dout("out", [S, D])

    sc = Sched(nc, stack)

    ps = stack.enter_context(nc.psum_tensor("ps", [128, 4096], F32))
    identb = sb("identb", [128, 128], BF16)
    x0T = sb("x0T", [128, 8, S], BF16)
    def bank(b, n=512, off=0):
        return ps[:, b * 512 + off: b * 512 + off + n]

    def bankbf(b):
        return ps[:, b * 512:(b + 1) * 512].bitcast(BF16)

    bank_rr = [0]

    def next_bank():
        b = bank_rr[0] % 8
        bank_rr[0] += 1
        return b

    sc.dma('sp', identb[:], identb_d[:, :], writes=['identb'])
    x0_d = nc.dram_tensor("x0_scr", [S, D], F32, kind="Internal").ap()

    st6 = [sb("st6_%d" % i, [128, 2, 6], F32) for i in range(2)]
    mv = [sb("mv%d" % i, [128, 2], F32) for i in range(2)]
    rs = [sb("rs%d" % i, [128, 2], F32) for i in range(2)]
    eps_t = sb("eps_t", [128, 2], F32)
    ph1 = contextlib.ExitStack()

    def sb1(name, shape, dt):
        return ph1.enter_context(nc.sbuf_tensor(name, list(shape), dt))

    g0B = sb1("g0B", [128, D], F32)
    b0B = sb1("b0B", [128, D], F32)
    sc.dma('sp', g0B[:], ln0g_d.partition_broadcast(128), writes=['g0B'])
    sc.dma('sp', b0B[:], ln0b_d.partition_broadcast(128), writes=['b0B'])
    xt = [sb1("xt%d" % i, [128, D], F32) for i in range(2)]
    xn = [sb1("xn%d" % i, [128, D], F32) for i in range(2)]
    xb = [sb1("xb%d" % i, [128, D], BF16) for i in range(2)]

    def layernorm_stats(src, p, tag):
        for hh in range(2):
            sc.op('dve', lambda e, hh=hh: e.bn_stats(out=st6[p][:, hh, :], in_=src[:, hh * 512:(hh + 1) * 512]),
                  reads=[tag], writes=[('st6', p, hh)])
        sc.op('dve', lambda e: e.bn_aggr(out=mv[p][:], in_=st6[p][:].rearrange("p a b -> p (a b)")),
              reads=[('st6', p, 0), ('st6', p, 1)], writes=[('mv', p)])
        sc.op('act', lambda e: e.activation(out=rs[p][:, 0:1], in_=mv[p][:, 1:2], func=AF.Sqrt, bias=eps_t[:, 0:1], scale=1.0),
              reads=[('mv', p), 'eps'], writes=[('rs0', p)])
        sc.op('dve', lambda e: e.reciprocal(out=rs[p][:, 1:2], in_=rs[p][:, 0:1]),
              reads=[('rs0', p)], writes=[('rs', p)])

    sc.op('dve', lambda e: e.memset(eps_t[:, 0:1], LN_EPS), writes=['eps'])
    sc.op('dve', lambda e: e.memset(eps_t[:, 1:2], GN_EPS), writes=['eps'])

    for i in range(NT):
        p = i % 2
        sc.dma('sp', xt[p][:], x_d[i * 128:(i + 1) * 128, :], writes=[('xt', p)])
        layernorm_stats(xt[p], p, ('xt', p))
        sc.op('dve', lambda e: e.tensor_scalar(out=xn[p][:], in0=xt[p][:], scalar1=mv[p][:, 0:1], scalar2=rs[p][:, 1:2],
                                               op0=ALU.subtract, op1=ALU.mult),
              reads=[('xt', p), ('mv', p), ('rs', p)], writes=[('xn', p)])
        sc.op('pool', lambda e: e.tensor_tensor(out=xn[p][:], in0=xn[p][:], in1=g0B[:], op=ALU.mult),
              reads=[('xn', p), 'g0B'], writes=[('xn', p)])
        sc.op('pool', lambda e: e.tensor_tensor(out=xn[p][:], in0=xn[p][:], in1=b0B[:], op=ALU.add),
              reads=[('xn', p), 'b0B'], writes=[('xn', p)])
        sc.dma('sp', x0_d[i * 128:(i + 1) * 128, :], xn[p][:], reads=[('xn', p)], writes=[('x0_d', i)])
        sc.op('act', lambda e: e.copy(out=xb[p][:], in_=xn[p][:]), reads=[('xn', p)], writes=[('xb', p)])
        b = next_bank()
        for kc in range(8):
            sc.op('pe', lambda e, kc=kc: e.transpose(out=bankbf(b)[:, kc * 128:(kc + 1) * 128],
                                                     in_=xb[p][:, kc * 128:(kc + 1) * 128], identity=identb[:]),
                  reads=[('xb', p), 'identb'], writes=[('bank', b)] if kc == 7 else [], ww=[('bank', b)], inc=(kc == 7))
        sc.op('dve', lambda e: e.tensor_copy(out=x0T[:, :, i * 128:(i + 1) * 128],
                                             in_=bankbf(b).rearrange("p (c t) -> p c t", c=8)),
              reads=[('bank', b)], writes=[('x0T', i)])

    if 'x0T' in dbg:
        d = dout("dbg_x0T", [128, 8, S], BF16)
        sc.dma('sp', d[:, :, :], x0T[:], reads=[('x0T', i) for i in range(NT)])

    if 'stage1_only' in dbg:
        for i in range(NT):
            p = i % 2
            sc.dma('sp', xt[p][:], x0_d[i * 128:(i + 1) * 128, :], reads=[('x0_d', i)], writes=[('xt', p)])
            sc.dma('sp', out_d[i * 128:(i + 1) * 128, :], xt[p][:], reads=[('xt', p)], writes=[('out', i)])
        sc.finish()
        return


    sc.barrier()
    ph1.close()
    y_aT = sb("y_aT", [128, 4, S], BF16)
    win_v = win_d.rearrange("(kc p) n -> p kc n", p=128)

    with contextlib.ExitStack() as ph:
        def sbp(name, shape, dt):
            return ph.enter_context(nc.sbuf_tensor(name, list(shape), dt))

        wqkv = sbp("wqkv", [128, 8, 1536], BF16)
        for g in range(3):
            sc.dma('pool', wqkv[:, :, g * 512:(g + 1) * 512], win_v[:, :, g * 512:(g + 1) * 512], writes=[('wqkv', g)])
        qT = sbp("qT", [72, 8, S], BF16)
        kT = sbp("kT", [72, 8, S], BF16)
        va = sbp("va", [128, NT, 8, 65], BF16)
        cosS = sbp("cosS", [128, NT, 32], F32)
        sinS = sbp("sinS", [128, NT, 32], F32)
        trib = sbp("trib", [128, 128], BF16)
        biasT = sbp("biasT", [64, S], BF16)
        sc.dma('sp', cosS[:], cos_d.rearrange("(i p) f -> p i f", p=128), writes=['cos'])
        sc.dma('sp', sinS[:], sin_d.rearrange("(i p) f -> p i f", p=128), writes=['sin'])
        sc.dma('sp', trib[:], tri_d[:, :], writes=['trib'])
        for h in range(8):
            sc.dma('sp', kT[64:72, h, :], blk_d[:, :], writes=[('kTaug', h)])
        sc.op('pool', lambda e: e.memset(va[:, :, :, 64:65], 1.0), writes=['va1'])
        tmp = [[sbp("rt%d_%d" % (a, b_), [128, 16, 32], F32) for b_ in range(4)] for a in range(2)]
        rot = [sbp("rot%d" % a, [128, 16, 64], BF16) for a in range(2)]

        for i in range(NT):
            p = i % 2
            for g in range(3):
                bk = (2 * p + g) if g < 2 else (4 + p)
                for kc in range(8):
                    sc.op('pe', lambda e, kc=kc: e.matmul(bank(bk), lhsT=x0T[:, kc, i * 128:(i + 1) * 128],
                                                         rhs=wqkv[:, kc, g * 512:(g + 1) * 512],
                                                         start=(kc == 0), stop=(kc == 7)),
                          reads=[('x0T', i), ('wqkv', g)], writes=[('bank', bk)] if kc == 7 else [], ww=[('bank', bk)], inc=(kc == 7))
            zz = ps[:, p * 1024:(p + 1) * 1024].rearrange("p (h d) -> p h d", d=64)
            zA, zB = zz[:, :, 0:32], zz[:, :, 32:64]
            cb = cosS[:, i, :].unsqueeze(1).broadcast_to([128, 16, 32])
            sb_ = sinS[:, i, :].unsqueeze(1).broadcast_to([128, 16, 32])
            bks = [('bank', 2 * p), ('bank', 2 * p + 1)]
            for t_i, (zin, tab, tk) in enumerate([(zA, cb, 'cos'), (zB, sb_, 'sin'), (zB, cb, 'cos'), (zA, sb_, 'sin')]):
                sc.op('dve', lambda e, zin=zin, tab=tab, t_i=t_i: e.tensor_tensor(out=tmp[p][t_i][:], in0=zin, in1=tab, op=ALU.mult),
                      reads=bks + [tk], writes=[('rt', p, t_i)])
            sc.op('pool', lambda e: e.tensor_tensor(out=rot[p][:, :, 0:32], in0=tmp[p][0][:], in1=tmp[p][1][:], op=ALU.subtract),
                  reads=[('rt', p, 0), ('rt', p, 1)], writes=[('rotA', p)])
            sc.op('pool', lambda e: e.tensor_tensor(out=rot[p][:, :, 32:64], in0=tmp[p][2][:], in1=tmp[p][3][:], op=ALU.add),
                  reads=[('rt', p, 2), ('rt', p, 3)], writes=[('rotB', p)])
            for qk in range(2):
                bt = 6 + qk
                for h in range(8):
                    sc.op('pe', lambda e, h=h: e.transpose(out=bankbf(bt)[0:64, h * 128:(h + 1) * 128],
                                                         in_=rot[p][:, qk * 8 + h, :], identity=identb[:]),
                          reads=[('rotA', p), ('rotB', p), 'identb'], writes=[('bank', bt)] if h == 7 else [], ww=[('bank', bt)], inc=(h == 7))
                src_v = bankbf(bt)[0:64, :].rearrange("p (h t) -> p h t", h=8)
                if qk == 0:
                    sc.op('act', lambda e: e.mul(out=qT[0:64, :, i * 128:(i + 1) * 128], in_=src_v, mul=0.125),
                          reads=[('bank', bt)], writes=[('qT', i)])
                else:
                    sc.op('act', lambda e: e.copy(out=kT[0:64, :, i * 128:(i + 1) * 128], in_=src_v),
                          reads=[('bank', bt)], writes=[('kT', i)])
            sc.op('act', lambda e: e.copy(out=va[:, i, :, 0:64], in_=bank(4 + p).rearrange("p (h d) -> p h d", d=64)),
                  reads=[('bank', 4 + p)], writes=[('va', i)])

        km32 = sbp("km32", [64, 8, 8], F32)
        kmb = sbp("kmb", [64, 8, 8], BF16)
        sc.op('dve', lambda e: e.tensor_reduce(out=km32[:], in_=kT[0:64, :, :].rearrange("p h (n k) -> p h n k", k=256),
                                               axis=AX.X, op=ALU.add),
              reads=[('kT', i) for i in range(NT)], writes=['km32'])
        sc.op('dve', lambda e: e.tensor_copy(out=kmb[:], in_=km32[:]), reads=['km32'], writes=['kmb'])
        G = [sbp("G%d" % a, [128, 8, 8], F32) for a in range(2)]
        cmpt = [sbp("cmp%d" % a, [128, 8, 8, 8], F32) for a in range(2)]
        rank = [sbp("rank%d" % a, [128, 8, 8], F32) for a in range(2)]
        biasb = [sbp("biasb%d" % a, [128, 8, 8], BF16) for a in range(2)]
        for i in range(NT):
            p = i % 2
            blk = i // 2
            bg = p
            for h in range(8):
                sc.op('pe', lambda e, h=h: e.matmul(bank(bg, 8, h * 8), lhsT=qT[0:64, h, i * 128:(i + 1) * 128], rhs=kmb[:, h, :],
                                                   start=True, stop=True),
                      reads=[('qT', i), 'kmb'], writes=[('bank', bg)] if h == 7 else [], ww=[('bank', bg)], inc=(h == 7))
            sc.op('act', lambda e: e.copy(out=G[p][:].rearrange("p h n -> p (h n)"), in_=bank(bg, 64)),
                  reads=[('bank', bg)], writes=[('G', p)])
            sc.op('pool', lambda e: e.memset(G[p][:, :, blk:8], -1e30), reads=[('G', p)], writes=[('G', p)])
            sc.op('dve', lambda e: e.tensor_tensor(out=cmpt[p][:], in0=G[p][:].unsqueeze(2).broadcast_to([128, 8, 8, 8]),
                                                   in1=G[p][:].unsqueeze(3).broadcast_to([128, 8, 8, 8]), op=ALU.is_gt),
                  reads=[('G', p)], writes=[('cmp', p)])
            sc.op('dve', lambda e: e.tensor_reduce(out=rank[p][:], in_=cmpt[p][:], axis=AX.X, op=ALU.add),
                  reads=[('cmp', p)], writes=[('rank', p)])
            sc.op('dve', lambda e: e.tensor_scalar(out=biasb[p][:], in0=rank[p][:], scalar1=3.0, scalar2=NEG,
                                                   op0=ALU.is_ge, op1=ALU.mult),
                  reads=[('rank', p)], writes=[('biasb', p)])
            sc.op('pool', lambda e: e.memset(biasb[p][:, :, blk:blk + 1], 0.0), reads=[('biasb', p)], writes=[('biasb', p)])
            bt = 2 + p
            sc.op('pe', lambda e: e.transpose(out=bankbf(bt)[0:64, 0:128], in_=biasb[p][:].rearrange("p h n -> p (h n)"),
                                              identity=identb[:]),
                  reads=[('biasb', p), 'identb'], writes=[('bank', bt)])
            sc.op('act', lambda e: e.copy(out=biasT[:, i * 128:(i + 1) * 128], in_=bankbf(bt)[0:64, 0:128]),
                  reads=[('bank', bt)], writes=[('biasT', i)])
        for h in range(8):
            sc.dma('sp', qT[64:72, h, :], biasT[h * 8:(h + 1) * 8, :], reads=[('biasT', i) for i in range(NT)],
                   writes=[('qTaug', h)])

        if 'qk' in dbg:
            dq = dout("dbg_qT", [72, 8, S], BF16)
            dk = dout("dbg_kT", [72, 8, S], BF16)
            sc.dma('sp', dq[:, :, :], qT[:], reads=[('qT', i) for i in range(NT)] + [('qTaug', h) for h in range(8)])
            sc.dma('sp', dk[:, :, :], kT[:], reads=[('kT', i) for i in range(NT)] + [('kTaug', h) for h in range(8)])

        PT = [sbp("PT%d" % a, [128, 4, 128], BF16) for a in range(3)]
        ya = [sbp("ya%d" % a, [128, 512], BF16) for a in range(2)]
        rden = [sbp("rden%d" % a, [128, 1], F32) for a in range(2)]
        groups = []
        for qt in range(NT):
            for h in range(8):
                for g0 in range(0, qt + 1, 4):
                    js = list(range(g0, min(g0 + 4, qt + 1)))
                    groups.append((qt, h, js))

        def emit_ST(k):
            qt, h, js = groups[k]
            sbk = k % 3
            for jj, j in enumerate(js):
                last = (jj == len(js) - 1)
                sc.op('pe', lambda e, jj=jj, j=j: e.matmul(bank(sbk, 128, jj * 128), lhsT=kT[0:72, h, j * 128:(j + 1) * 128],
                                                         rhs=qT[0:72, h, qt * 128:(qt + 1) * 128], start=True, stop=True),
                      reads=[('kT', j), ('kTaug', h), ('qT', qt), ('qTaug', h)],
                      writes=[('bank', sbk)] if last else [], ww=[('bank', sbk)], inc=last)

        def emit_rest(k):
            qt, h, js = groups[k]
            sbk = k % 3
            yp = qt % 2
            idx = qt * 8 + h
            ab = 3 + (idx % 2)
            ap_ = idx % 2
            n = len(js)
            sc.op('act', lambda e: e.activation(out=PT[sbk][:, 0:n, :].rearrange("p a b -> p (a b)"), in_=bank(sbk, n * 128), func=AF.Exp),
                  reads=[('bank', sbk)], writes=[('PT', sbk)])
            if js[-1] == qt:
                jj = n - 1
                sc.op('pool', lambda e: e.tensor_tensor(out=PT[sbk][:, jj, :], in0=PT[sbk][:, jj, :], in1=trib[:], op=ALU.mult),
                      reads=[('PT', sbk), 'trib'], writes=[('PT', sbk)])
            for jj, j in enumerate(js):
                last = (j == qt)
                sc.op('pe', lambda e, jj=jj, j=j: e.matmul(bank(ab, 65), lhsT=PT[sbk][:, jj, :], rhs=va[:, j, h, :],
                                                         start=(j == 0), stop=(j == qt)),
                      reads=[('PT', sbk), ('va', j), 'va1'], writes=[('bank', ab)] if last else [], ww=[('bank', ab)],
                      inc=(last or jj == n - 1))
            if js[-1] != qt:
                return
            sc.op('dve', lambda e: e.reciprocal(out=rden[ap_][:], in_=bank(ab, 1, 64)), reads=[('bank', ab)], writes=[('rden', ap_)])
            sc.op('dve', lambda e: e.tensor_scalar(out=ya[yp][:, h * 64:(h + 1) * 64], in0=bank(ab, 64), scalar1=rden[ap_][:, 0:1],
                                                   scalar2=None, op0=ALU.mult),
                  reads=[('bank', ab), ('rden', ap_)], writes=[('ya', yp, h)])
            if h != 7:
                return
            bt = 5
            for c in range(4):
                sc.op('pe', lambda e, c=c: e.transpose(out=bankbf(bt)[:, c * 128:(c + 1) * 128], in_=ya[yp][:, c * 128:(c + 1) * 128],
                                                     identity=identb[:]),
                      reads=[('ya', yp, hh_) for hh_ in range(8)] + ['identb'], writes=[('bank', bt)] if c == 3 else [], ww=[('bank', bt)],
                      inc=(c == 3))
            sc.op('act', lambda e: e.copy(out=y_aT[:, :, qt * 128:(qt + 1) * 128], in_=bankbf(bt)[:, 0:512].rearrange("p (c t) -> p c t", c=4)),
                  reads=[('bank', bt)], writes=[('y_aT', qt)])

        emit_ST(0)
        for k in range(len(groups)):
            if k + 1 < len(groups):
                emit_ST(k + 1)
            emit_rest(k)

    sc.barrier()
    if 'y_aT' in dbg:
        d = dout("dbg_y_aT", [128, 4, S], BF16)
        sc.dma('sp', d[:, :, :], y_aT[:], reads=[('y_aT', i) for i in range(NT)])

    y_mT = sb("y_mT", [128, 4, S], BF16)
    chanv_d = din("chanv", [128, 4, 7])
    wqk_d = din("wqk_m", [4, 128, 256])
    bif_d = din("b_if", [8])
    trif_d = din("tri_f32", [128, 128])
    onesf_d = din("ones_f32", [128, 128])
    with contextlib.ExitStack() as ph:
        def sbp(name, shape, dt):
            return ph.enter_context(nc.sbuf_tensor(name, list(shape), dt))

        chanv = sbp("chanv_s", [128, 4, 7], F32)
        wqk = sbp("wqk", [128, 4, 256], BF16)
        bifB = sbp("bifB", [128, 8], F32)
        trif = sbp("trif", [128, 128], F32)
        onesf = sbp("onesf", [128, 128], F32)
        trib = sbp("tribB", [128, 128], BF16)
        ones1 = sbp("ones1", [128, 1], F32)
        sc.dma('sp', chanv[:], chanv_d[:, :, :], writes=['chanv'])
        sc.dma('pool', wqk[:], wqk_d.rearrange("h p n -> p h n"), writes=['wqk'])
        sc.dma('sp', bifB[:], bif_d.partition_broadcast(128), writes=['bifB'])
        sc.dma('sp', trif[:], trif_d[:, :], writes=['trif'])
        sc.dma('sp', onesf[:], onesf_d[:, :], writes=['onesf'])
        sc.dma('sp', trib[:], tri_d[:, :], writes=['trib'])
        sc.op('dve', lambda e: e.memset(ones1[:], 1.0), writes=['ones1'])
        ucT = sbp("ucT", [128, 4, S], BF16)
        su = sbp("su", [128, 4, S], BF16)
        vm = sbp("vm", [128, NT, 4, 129], BF16)
        om = sbp("om", [128, NT, 512], BF16)
        pre = sbp("pre", [128, NT, 8], F32)
        sc.op('pool', lambda e: e.memset(vm[:, :, :, 128:129], 1.0), writes=['vm1'])

        with contextlib.ExitStack() as ph2:
            def sbq(name, shape, dt):
                return ph2.enter_context(nc.sbuf_tensor(name, list(shape), dt))
            wu = sbq("wu", [128, 8, 512], BF16)
            wvo = sbq("wvo", [128, 8, 1024], BF16)
            wif = sbq("wif", [128, 8, 8], BF16)
            sc.dma('pool', wu[:], win_v[:, :, 1536:2048], writes=['wu'])
            sc.dma('pool', wvo[:, :, 0:512], win_v[:, :, 2048:2560], writes=[('wvo', 0)])
            sc.dma('pool', wvo[:, :, 512:1024], win_v[:, :, 2560:3072], writes=[('wvo', 1)])
            sc.dma('pool', wif[:], win_v[:, :, 3072:3080], writes=['wif'])
            upad = [sbq("upad%d" % a, [128, S + 3], F32) for a in range(2)]
            cacc = [sbq("cacc%d" % a, [128, S], F32) for a in range(2)]
            for a in range(2):
                sc.op('pool', lambda e, a=a: e.memset(upad[a][:, 0:3], 0.0), writes=[('upadz', a)])
            for h in range(4):
                p = h % 2
                for tg in range(4):
                    b = next_bank()
                    for kc in range(8):
                        sc.op('pe', lambda e, kc=kc: e.matmul(bank(b), lhsT=wu[:, kc, h * 128:(h + 1) * 128],
                                                             rhs=x0T[:, kc, tg * 512:(tg + 1) * 512], start=(kc == 0), stop=(kc == 7)),
                              reads=['wu'] + [('x0T', tg * 4 + q) for q in range(4)], writes=[('bank', b)] if kc == 7 else [], ww=[('bank', b)], inc=(kc == 7))
                    sc.op('act', lambda e: e.copy(out=upad[p][:, 3 + tg * 512: 3 + (tg + 1) * 512], in_=bank(b)),
                          reads=[('bank', b)], writes=[('upad', p, tg)])
                ur = [('upad', p, tg) for tg in range(4)] + [('upadz', p)]
                sc.op('dve', lambda e: e.tensor_scalar(out=cacc[p][:], in0=upad[p][:, 0:S], scalar1=chanv[:, h, 0:1], scalar2=chanv[:, h, 4:5],
                                                       op0=ALU.mult, op1=ALU.add),
                      reads=ur + ['chanv'], writes=[('cacc', p)])
                for j in range(1, 4):
                    sc.op('dve', lambda e, j=j: e.scalar_tensor_tensor(out=cacc[p][:], in0=upad[p][:, j:S + j], scalar=chanv[:, h, j:j + 1],
                                                                       in1=cacc[p][:], op0=ALU.mult, op1=ALU.add),
                          reads=ur + ['chanv', ('cacc', p)], writes=[('cacc', p)])
                sc.op('act', lambda e: e.activation(out=ucT[:, h, :], in_=cacc[p][:], func=AF.Silu),
                      reads=[('cacc', p)], writes=[('ucT', h)])
                sc.op('pool', lambda e: e.tensor_scalar(out=su[:, h, :], in0=ucT[:, h, :], scalar1=chanv[:, h, 6:7], scalar2=None, op0=ALU.mult),
                      reads=[('ucT', h), 'chanv'], writes=[('su', h)])
            for i in range(NT):
                bs = []
                for g in range(3):
                    b = next_bank()
                    bs.append(b)
                    n = 512 if g < 2 else 8
                    for kc in range(8):
                        rhs = wvo[:, kc, g * 512:(g + 1) * 512] if g < 2 else wif[:, kc, :]
                        sc.op('pe', lambda e, kc=kc, rhs=rhs: e.matmul(bank(b, n), lhsT=x0T[:, kc, i * 128:(i + 1) * 128], rhs=rhs,
                                                                     start=(kc == 0), stop=(kc == 7)),
                              reads=[('x0T', i), ('wvo', g) if g < 2 else 'wif'], writes=[('bank', b)] if kc == 7 else [], ww=[('bank', b)], inc=(kc == 7))
                sc.op('act', lambda e: e.copy(out=vm[:, i, :, 0:128], in_=bank(bs[0]).rearrange("p (h d) -> p h d", d=128)),
                      reads=[('bank', bs[0])], writes=[('vm', i)])
                sc.op('act', lambda e: e.activation(out=om[:, i, :], in_=bank(bs[1]), func=AF.Sigmoid),
                      reads=[('bank', bs[1])], writes=[('om', i)])
                sc.op('dve', lambda e: e.tensor_tensor(out=pre[:, i, :], in0=bank(bs[2], 8), in1=bifB[:], op=ALU.add),
                      reads=[('bank', bs[2]), 'bifB'], writes=[('pre', i)])
        sc.barrier()
        if 'stopB1' in dbg:
            sc.finish()
            return

        Lall = sbp("Lall", [128, NT, 4], F32)
        eb = sbp("eb", [128, NT, 4], F32)
        e2 = sbp("e2", [128, NT, 4], F32)
        e3 = sbp("e3", [128, NT, 4], F32)
        dec = sbp("dec", [128, NT, 4], F32)
        a2 = sbp("a2", [128, NT, 4], F32)
        a3 = sbp("a3", [128, NT, 4], F32)
        prs = [('pre', i) for i in range(NT)]
        import os as _os
        _cut = int(_os.environ.get('GCUT', '99'))
        _real_op = sc.op
        _k = [0]

        def _cut_op(*a, **kw):
            _k[0] += 1
            if _k[0] <= _cut:
                return _real_op(*a, **kw)
            return None
        if 'stopB2' in dbg:
            sc.op = _cut_op
        sc.op('act', lambda e: e.activation(out=Lall[:], in_=pre[:, :, 4:8], func=AF.Exp, scale=-1.0), reads=prs, writes=['Lall'])
        sc.op('act', lambda e: e.activation(out=Lall[:], in_=Lall[:], func=AF.Ln, bias=ones1[:, 0:1], scale=1.0),
              reads=['Lall', 'ones1'], writes=['Lall'])
        bc, bg_ = next_bank(), next_bank()
        sc.op('pe', lambda e: e.matmul(bank(bc, 64), lhsT=trif[:], rhs=Lall[:].rearrange("p i h -> p (i h)"), start=True, stop=True),
              reads=['trif', 'Lall'], writes=[('bank', bc)])
        sc.op('pe', lambda e: e.matmul(bank(bg_, 64), lhsT=onesf[:], rhs=Lall[:].rearrange("p i h -> p (i h)"), start=True, stop=True),
              reads=['onesf', 'Lall'], writes=[('bank', bg_)])
        f64 = lambda t: t[:].rearrange("p i h -> p (i h)")
        sc.op('act', lambda e: e.activation(out=f64(eb), in_=bank(bc, 64), func=AF.Exp, scale=-1.0), reads=[('bank', bc)], writes=['eb'])
        sc.op('act', lambda e: e.activation(out=f64(dec), in_=bank(bg_, 64), func=AF.Exp, scale=-1.0), reads=[('bank', bg_)], writes=['dec'])
        sc.op('dve', lambda e: e.tensor_tensor(out=a2[:], in0=bank(bc, 64).rearrange("p (i h) -> p i h", h=4), in1=pre[:, :, 0:4], op=ALU.add),
              reads=[('bank', bc)] + prs, writes=['a2'])
        sc.op('dve', lambda e: e.tensor_tensor(out=f64(a3), in0=f64(a2), in1=bank(bg_, 64), op=ALU.subtract),
              reads=[('bank', bg_), 'a2'], writes=['a3'])
        sc.op('act', lambda e: e.activation(out=f64(e2), in_=f64(a2), func=AF.Exp), reads=['a2'], writes=['e2'])
        sc.op('act', lambda e: e.activation(out=f64(e3), in_=f64(a3), func=AF.Exp), reads=['a3'], writes=['e3'])
        KS = 128.0 ** -0.5
        sc.op('dve', lambda e: e.tensor_scalar(out=f64(e2), in0=f64(e2), scalar1=KS, scalar2=None, op0=ALU.mult), reads=['e2'], writes=['e2'])
        sc.op('dve', lambda e: e.tensor_scalar(out=f64(e3), in0=f64(e3), scalar1=KS, scalar2=None, op0=ALU.mult), reads=['e3'], writes=['e3'])

        if 'stopB2' in dbg:
            sc.op = _real_op
            sc.barrier()
            sc.finish()
            return
        S32 = sbp("S32", [128, 4, 129], F32)
        Sb = sbp("Sb", [128, 4, 129], BF16)
        sc.op('dve', lambda e: e.memset(S32[:], 0.0), writes=[('S32', h) for h in range(4)])
        sc.op('dve', lambda e: e.memset(Sb[:], 0.0), writes=[('Sb', h) for h in range(4)])
        NB = 8
        qs = [sbp("qs%d" % a, [128, 128], BF16) for a in range(NB)]
        k2 = [sbp("k2_%d" % a, [128, 128], BF16) for a in range(NB)]
        k3 = [sbp("k3_%d" % a, [128, 128], BF16) for a in range(NB)]
        qkT = [sbp("qkT%d" % a, [128, 2, 128], BF16) for a in range(NB)]
        PTm = [sbp("PTm%d" % a, [128, 128], BF16) for a in range(NB)]
        hh32 = [sbp("hh32_%d" % a, [128, 128], F32) for a in range(NB)]
        hn = [sbp("hn%d" % a, [128, 128], BF16) for a in range(NB)]
        gst = [sbp("gst%d" % a, [128, 6], F32) for a in range(NB)]
        gmv = [sbp("gmv%d" % a, [128, 2], F32) for a in range(NB)]
        grs = [sbp("grs%d" % a, [128, 4], F32) for a in range(NB)]
        HS = range(4)

        def partA(i):
            P = [(i % 2) * 4 + h for h in HS]
            bA = [next_bank() for h in HS]
            for h in HS:
                sc.op('pe', lambda e: e.matmul(bank(bA[h], 256), lhsT=ucT[:, h, i * 128:(i + 1) * 128], rhs=wqk[:, h, :], start=True, stop=True),
                      reads=[('ucT', h), 'wqk'], writes=[('bank', bA[h])])
            for h in HS:
                p = P[h]
                sc.op('act', lambda e: e.activation(out=qs[p][:], in_=bank(bA[h], 128, 0), func=AF.Copy, scale=eb[:, i, h:h + 1]),
                      reads=[('bank', bA[h]), 'eb'], writes=[('qs', p)])
                sc.op('act', lambda e: e.activation(out=k2[p][:], in_=bank(bA[h], 128, 128), func=AF.Copy, scale=e2[:, i, h:h + 1]),
                      reads=[('bank', bA[h]), 'e2'], writes=[('k2', p)])
                sc.op('dve', lambda e: e.tensor_scalar(out=k3[p][:], in0=bank(bA[h], 128, 128), scalar1=e3[:, i, h:h + 1], scalar2=None, op0=ALU.mult),
                      reads=[('bank', bA[h]), 'e3'], writes=[('k3', p)])
            bC = [next_bank() for h in HS]
            for h in HS:
                p = P[h]
                sc.op('pe', lambda e: e.transpose(out=bankbf(bC[h])[:, 0:128], in_=qs[p][:], identity=identb[:]),
                      reads=[('qs', p), 'identb'], inc=False, ww=[('bank', bC[h])])
                sc.op('pe', lambda e: e.transpose(out=bankbf(bC[h])[:, 128:256], in_=k2[p][:], identity=identb[:]),
                      reads=[('k2', p), 'identb'], writes=[('bank', bC[h])])
            for h in HS:
                p = P[h]
                sc.op('dve' if h % 2 == 0 else 'act',
                      (lambda e: e.tensor_copy(out=qkT[p][:].rearrange("p a t -> p (a t)"), in_=bankbf(bC[h])[:, 0:256])) if h % 2 == 0 else
                      (lambda e: e.copy(out=qkT[p][:].rearrange("p a t -> p (a t)"), in_=bankbf(bC[h])[:, 0:256])),
                      reads=[('bank', bC[h])], writes=[('qkT', p)])
            bD = [next_bank() for h in HS]
            for h in HS:
                p = P[h]
                sc.op('pe', lambda e: e.matmul(bank(bD[h], 128), lhsT=qkT[p][:, 1, :], rhs=qkT[p][:, 0, :], start=True, stop=True),
                      reads=[('qkT', p)], writes=[('bank', bD[h])])
            for h in HS:
                p = P[h]
                sc.op('dve', lambda e: e.tensor_tensor(out=PTm[p][:], in0=bank(bD[h], 128), in1=trib[:], op=ALU.mult),
                      reads=[('bank', bD[h]), 'trib'], writes=[('PTm', p)])

        def partB(i):
            P = [(i % 2) * 4 + h for h in HS]
            bF = [next_bank() for h in HS]
            for h in HS:
                p = P[h]
                sc.op('pe', lambda e: e.matmul(bank(bF[h], 129), lhsT=qkT[p][:, 0, :], rhs=Sb[:, h, :], start=True, stop=False),
                      reads=[('qkT', p), ('Sb', h)], inc=False, ww=[('bank', bF[h])])
                sc.op('pe', lambda e: e.matmul(bank(bF[h], 129), lhsT=PTm[p][:], rhs=vm[:, i, h, :], start=False, stop=True),
                      reads=[('PTm', p), ('vm', i), 'vm1'], writes=[('bank', bF[h])])
            bJ = [next_bank() for h in HS]
            for h in HS:
                p = P[h]
                sc.op('pe', lambda e: e.matmul(bank(bJ[h], 129), lhsT=k3[p][:], rhs=vm[:, i, h, :], start=True, stop=True),
                      reads=[('k3', p), ('vm', i), 'vm1'], writes=[('bank', bJ[h])])
            for h in HS:
                sc.op('dve', lambda e: e.scalar_tensor_tensor(out=S32[:, h, :], in0=S32[:, h, :], scalar=dec[:, i, h:h + 1],
                                                              in1=bank(bJ[h], 129), op0=ALU.mult, op1=ALU.add),
                      reads=[('S32', h), 'dec', ('bank', bJ[h])], writes=[('S32', h)])
            for h in HS:
                sc.op('act', lambda e: e.copy(out=Sb[:, h, :], in_=S32[:, h, :]), reads=[('S32', h)], writes=[('Sb', h)])
            for h in HS:
                p = P[h]
                sc.op('act', lambda e: e.activation(out=grs[p][:, 0:1], in_=bank(bF[h], 1, 128), func=AF.Abs),
                      reads=[('bank', bF[h])], writes=[('grs0', p)])
            for h in HS:
                p = P[h]
                sc.op('dve', lambda e: e.tensor_scalar(out=grs[p][:, 0:1], in0=grs[p][:, 0:1], scalar1=1.0, scalar2=None, op0=ALU.max),
                      reads=[('grs0', p)], writes=[('grs0', p)])
            for h in HS:
                p = P[h]
                sc.op('dve', lambda e: e.reciprocal(out=grs[p][:, 1:2], in_=grs[p][:, 0:1]), reads=[('grs0', p)], writes=[('grs1', p)])
            for h in HS:
                p = P[h]
                sc.op('dve', lambda e: e.scalar_tensor_tensor(out=hh32[p][:], in0=bank(bF[h], 128), scalar=grs[p][:, 1:2],
                                                              in1=om[:, i, h * 128:(h + 1) * 128], op0=ALU.mult, op1=ALU.mult),
                      reads=[('bank', bF[h]), ('grs1', p), ('om', i)], writes=[('hh32', p)])
            for h in HS:
                p = P[h]
                sc.op('dve', lambda e: e.bn_stats(out=gst[p][:], in_=hh32[p][:]), reads=[('hh32', p)], writes=[('gst', p)])
            for h in HS:
                p = P[h]
                sc.op('dve', lambda e: e.bn_aggr(out=gmv[p][:], in_=gst[p][:]), reads=[('gst', p)], writes=[('gmv', p)])
            for h in HS:
                p = P[h]
                sc.op('act', lambda e: e.activation(out=grs[p][:, 2:3], in_=gmv[p][:, 1:2], func=AF.Sqrt, bias=eps_t[:, 1:2], scale=1.0),
                      reads=[('gmv', p), 'eps'], writes=[('grs2', p)])
            for h in HS:
                p = P[h]
                sc.op('dve', lambda e: e.reciprocal(out=grs[p][:, 3:4], in_=grs[p][:, 2:3]), reads=[('grs2', p)], writes=[('grs3', p)])
            for h in HS:
                p = P[h]
                sc.op('dve', lambda e: e.tensor_scalar(out=hn[p][:], in0=hh32[p][:], scalar1=gmv[p][:, 0:1], scalar2=grs[p][:, 3:4],
                                                       op0=ALU.subtract, op1=ALU.mult),
                      reads=[('hh32', p), ('gmv', p), ('grs3', p)], writes=[('hn', p)])
            bI = [next_bank() for h in HS]
            for h in HS:
                p = P[h]
                sc.op('pe', lambda e: e.transpose(out=bankbf(bI[h])[:, 0:128], in_=hn[p][:], identity=identb[:]),
                      reads=[('hn', p), 'identb'], writes=[('bank', bI[h])])
            for h in HS:
                sc.op('dve', lambda e: e.scalar_tensor_tensor(out=y_mT[:, h, i * 128:(i + 1) * 128], in0=bankbf(bI[h])[:, 0:128],
                                                              scalar=chanv[:, h, 5:6], in1=su[:, h, i * 128:(i + 1) * 128],
                                                              op0=ALU.mult, op1=ALU.add),
                      reads=[('bank', bI[h]), 'chanv', ('su', h)], writes=[('y_mT', i, h)])

        partA(0)
        for i in range(NT):
            if i + 1 < NT:
                partA(i + 1)
            partB(i)

    sc.barrier()
    if 'y_mT' in dbg:
        d = dout("dbg_y_mT", [128, 4, S], BF16)
        sc.dma('sp', d[:, :, :], y_mT[:], reads=[('y_mT', i, h) for i in range(NT) for h in range(4)])

    CAP = 128
    YROWS = 2 * S + 128
    x1_d = nc.dram_tensor("x1_scr", [S, D], F32, kind="Internal").ap()
    tab_d = nc.dram_tensor("tab_scr", [64 * CAP, 4], F32, kind="Internal").ap()
    ybuf_d = nc.dram_tensor("ybuf_scr", [YROWS, D], F32, kind="Internal").ap()
    wau_d = din("w_attn_up", [512, D])
    wmu_d = din("w_mlstm_up", [512, D])
    wout_d = din("w_out", [D, D])
    ln1g_d = din("ln1_g", [D])
    ln1b_d = din("ln1_b", [D])
    wr_d = din("w_router", [D, 72])
    br_d = din("b_router", [72])
    identf_d = din("ident_f32", [128, 128])
    stri_d = din("stri_bf", [128, 128], BF16)
    onesb_d = din("ones_bf", [128, 128], BF16)
    iota64_d = din("iota64", [128, 64])
    tokid_d = din("tokid", [128, NT])
    tabinit_d = din("tab_init", [64 * CAP, 4])
    sc.dma('sp', tab_d[:, :], tabinit_d[:, :], writes=['tab_d'])

    with contextlib.ExitStack() as ph:
        def sbp(name, shape, dt):
            return ph.enter_context(nc.sbuf_tensor(name, list(shape), dt))
        mixT = sbp("mixT", [128, 8, S], BF16)
        phc = contextlib.ExitStack()
        _sbp_outer = sbp

        def sbp(name, shape, dt):
            return phc.enter_context(nc.sbuf_tensor(name, list(shape), dt))
        wg = sbp("wg", [128, 8, 2048], BF16)
        wau = sbp("wau", [128, 4, D], BF16)
        wmu = sbp("wmu", [128, 4, D], BF16)
        for g in range(4):
            sc.dma('pool', wg[:, :, g * 512:(g + 1) * 512], win_v[:, :, 3080 + g * 512: 3080 + (g + 1) * 512], writes=[('wg', g)])
        for g in range(2):
            sc.dma('pool', wau[:, :, g * 512:(g + 1) * 512], wau_d.rearrange("(c p) n -> p c n", p=128)[:, :, g * 512:(g + 1) * 512], writes=[('wau', g)])
            sc.dma('pool', wmu[:, :, g * 512:(g + 1) * 512], wmu_d.rearrange("(c p) n -> p c n", p=128)[:, :, g * 512:(g + 1) * 512], writes=[('wmu', g)])
        sg = [[sbp("sg%d_%d" % (a, b_), [128, 512], F32) for b_ in range(2)] for a in range(2)]
        it = 0
        for fc in range(8):
            for tg in range(4):
                p = it % 2
                it += 1
                bA, bB, bC, bD = 4 * p, 4 * p + 1, 4 * p + 2, 4 * p + 3
                xr = [('x0T', tg * 4 + q) for q in range(4)]
                for kc in range(8):
                    sc.op('pe', lambda e, kc=kc: e.matmul(bank(bA), lhsT=wg[:, kc, fc * 128:(fc + 1) * 128], rhs=x0T[:, kc, tg * 512:(tg + 1) * 512],
                                                         start=(kc == 0), stop=(kc == 7)),
                          reads=xr + [('wg', fc // 4)], writes=[('bank', bA)] if kc == 7 else [], ww=[('bank', bA)], inc=(kc == 7))
                for kc in range(8):
                    sc.op('pe', lambda e, kc=kc: e.matmul(bank(bB), lhsT=wg[:, kc, 1024 + fc * 128: 1024 + (fc + 1) * 128],
                                                         rhs=x0T[:, kc, tg * 512:(tg + 1) * 512], start=(kc == 0), stop=(kc == 7)),
                          reads=xr + [('wg', 2 + fc // 4)], writes=[('bank', bB)] if kc == 7 else [], ww=[('bank', bB)], inc=(kc == 7))
                for c in range(4):
                    sc.op('pe', lambda e, c=c: e.matmul(bank(bC), lhsT=wau[:, c, fc * 128:(fc + 1) * 128], rhs=y_aT[:, c, tg * 512:(tg + 1) * 512],
                                                       start=(c == 0), stop=(c == 3)),
                          reads=[('y_aT', tg * 4 + q) for q in range(4)] + [('wau', fc // 4)], writes=[('bank', bC)] if c == 3 else [],
                          ww=[('bank', bC)], inc=(c == 3))
                for c in range(4):
                    sc.op('pe', lambda e, c=c: e.matmul(bank(bD), lhsT=wmu[:, c, fc * 128:(fc + 1) * 128], rhs=y_mT[:, c, tg * 512:(tg + 1) * 512],
                                                       start=(c == 0), stop=(c == 3)),
                          reads=[('y_mT', tg * 4 + q, h) for q in range(4) for h in range(4)] + [('wmu', fc // 4)],
                          writes=[('bank', bD)] if c == 3 else [], ww=[('bank', bD)], inc=(c == 3))
                sc.op('act', lambda e: e.activation(out=sg[p][0][:], in_=bank(bA), func=AF.Sigmoid), reads=[('bank', bA)], writes=[('sg', p, 0)])
                sc.op('act', lambda e: e.activation(out=sg[p][1][:], in_=bank(bB), func=AF.Sigmoid), reads=[('bank', bB)], writes=[('sg', p, 1)])
                sc.op('dve', lambda e: e.tensor_tensor(out=sg[p][0][:], in0=sg[p][0][:], in1=bank(bC), op=ALU.mult),
                      reads=[('sg', p, 0), ('bank', bC)], writes=[('sg', p, 0)])
                sc.op('dve', lambda e: e.tensor_tensor(out=sg[p][1][:], in0=sg[p][1][:], in1=bank(bD), op=ALU.mult),
                      reads=[('sg', p, 1), ('bank', bD)], writes=[('sg', p, 1)])
                sc.op('pool', lambda e: e.tensor_tensor(out=mixT[:, fc, tg * 512:(tg + 1) * 512], in0=sg[p][0][:], in1=sg[p][1][:], op=ALU.add),
                      reads=[('sg', p, 0), ('sg', p, 1)], writes=[('mixT', fc, tg)])
        sc.barrier()
        phc.close()
        sbp = _sbp_outer

        wout = sbp("wout", [128, 8, D], BF16)
        for g in range(2):
            sc.dma('pool', wout[:, :, g * 512:(g + 1) * 512], wout_d.rearrange("(c p) n -> p c n", p=128)[:, :, g * 512:(g + 1) * 512], writes=[('wout', g)])
        g1B = sbp("g1B", [128, D], F32)
        b1B = sbp("b1B", [128, D], F32)
        wr = sbp("wr", [128, 8, 72], F32)
        brB = sbp("brB", [128, 72], F32)
        identf = sbp("identf", [128, 128], F32)
        strib = sbp("strib", [128, 128], BF16)
        onesb = sbp("onesb", [128, 128], BF16)
        iota64 = sbp("iota64s", [128, 64], F32)
        tokid = sbp("tokids", [128, NT], F32)
        sc.dma('sp', g1B[:], ln1g_d.partition_broadcast(128), writes=['g1B'])
        sc.dma('sp', b1B[:], ln1b_d.partition_broadcast(128), writes=['b1B'])
        sc.dma('sp', wr[:], wr_d.rearrange("(c p) n -> p c n", p=128), writes=['wr'])
        sc.dma('sp', brB[:], br_d.partition_broadcast(128), writes=['brB'])
        sc.dma('sp', identf[:], identf_d[:, :], writes=['identf'])
        sc.dma('sp', strib[:], stri_d[:, :], writes=['strib'])
        sc.dma('sp', onesb[:], onesb_d[:, :], writes=['onesb'])
        sc.dma('sp', iota64[:], iota64_d[:, :], writes=['iota64'])
        sc.dma('sp', tokid[:], tokid_d[:, :], writes=['tokid'])
        x0t = [sbp("x0t%d" % a, [128, D], F32) for a in range(2)]
        xp = [sbp("xp%d" % a, [128, D], F32) for a in range(2)]
        x1T = [sbp("x1T%d" % a, [128, 8, 128], F32) for a in range(2)]
        run = sbp("run", [128, 64], F32)
        sc.op('dve', lambda e: e.memset(run[:], 0.0), writes=['run'])

        def small(name, n, dt=F32):
            return [sbp("%s%d" % (name, a), [128, n], dt) for a in range(2)]
        lg = small("lg", 72)
        mx8 = small("mx8", 8)
        ohg = small("ohg", 8)
        exg = small("exg", 8)
        sc1 = small("sc1", 8)
        tmp88 = small("tmp88", 64)
        el = small("el", 8)
        mxe = small("mxe", 8)
        oh = [small("oh1_", 8), small("oh2_", 8)]
        Ac = [small("A1_", 64), small("A2_", 64)]
        Ab = small("Ab", 64, BF16)
        pos = small("pos", 64)
        prod = small("prod", 64)
        rowt = [small("row1_", 4), small("row2_", 4)]
        ridx = [small("ridx1_", 1, I32), small("ridx2_", 1, I32)]
        ridf = small("ridf", 2)

        for i in range(NT):
            p = i % 2
            sc.dma('sp', x0t[p][:], x0_d[i * 128:(i + 1) * 128, :], reads=[('x0_d', i)], writes=[('x0t', p)])
            bp = 2 * p
            for half in range(2):
                for kc in range(8):
                    sc.op('pe', lambda e, kc=kc: e.matmul(bank(bp + half), lhsT=mixT[:, kc, i * 128:(i + 1) * 128],
                                                         rhs=wout[:, kc, half * 512:(half + 1) * 512], start=(kc == 0), stop=(kc == 7)),
                          reads=[('mixT', kc, i // 4), ('wout', half)], writes=[('bank', bp + half)] if kc == 7 else [],
                          ww=[('bank', bp + half)], inc=(kc == 7))
            sc.op('dve', lambda e: e.scalar_tensor_tensor(out=xp[p][:], in0=x0t[p][:], scalar=ALPHA, in1=ps[:, bp * 512:(bp + 2) * 512],
                                                          op0=ALU.mult, op1=ALU.add),
                  reads=[('x0t', p), ('bank', bp), ('bank', bp + 1)], writes=[('xp', p)])
            layernorm_stats(xp[p], p, ('xp', p))
            sc.op('dve', lambda e: e.tensor_scalar(out=xp[p][:], in0=xp[p][:], scalar1=mv[p][:, 0:1], scalar2=rs[p][:, 1:2],
                                                   op0=ALU.subtract, op1=ALU.mult),
                  reads=[('xp', p), ('mv', p), ('rs', p)], writes=[('xp', p)])
            sc.op('pool', lambda e: e.tensor_tensor(out=xp[p][:], in0=xp[p][:], in1=g1B[:], op=ALU.mult),
                  reads=[('xp', p), 'g1B'], writes=[('xp', p)])
            sc.op('pool', lambda e: e.tensor_tensor(out=xp[p][:], in0=xp[p][:], in1=b1B[:], op=ALU.add),
                  reads=[('xp', p), 'b1B'], writes=[('xp', p)])
            sc.dma('sp', x1_d[i * 128:(i + 1) * 128, :], xp[p][:], reads=[('xp', p)], writes=[('x1_d', i)])
            bt = 4
            for kc in range(8):
                sc.op('pe', lambda e, kc=kc: e.transpose(out=ps[:, bt * 512 + kc * 128: bt * 512 + (kc + 1) * 128],
                                                         in_=xp[p][:, kc * 128:(kc + 1) * 128], identity=identf[:]),
                      reads=[('xp', p), 'identf'], writes=[('bank', bt), ('bank', bt + 1)] if kc == 7 else [],
                      ww=[('bank', bt), ('bank', bt + 1)], inc=(kc == 7))
            sc.op('act', lambda e: e.copy(out=x1T[p][:].rearrange("p c t -> p (c t)"), in_=ps[:, bt * 512:(bt + 2) * 512]),
                  reads=[('bank', bt), ('bank', bt + 1)], writes=[('x1T', p)])
            bl = 6
            for kc in range(8):
                sc.op('pe', lambda e, kc=kc: e.matmul(bank(bl, 72), lhsT=x1T[p][:, kc, :], rhs=wr[:, kc, :], start=(kc == 0), stop=(kc == 7)),
                      reads=[('x1T', p), 'wr'], writes=[('bank', bl)] if kc == 7 else [], ww=[('bank', bl)], inc=(kc == 7))
            sc.op('dve', lambda e: e.tensor_tensor(out=lg[p][:], in0=bank(bl, 72), in1=brB[:], op=ALU.add),
                  reads=[('bank', bl), 'brB'], writes=[('lg', p)])
            sc.op('dve', lambda e: e.max(out=mx8[p][:], in_=lg[p][:, 0:8]), reads=[('lg', p)], writes=[('mx8', p)])
            sc.op('dve', lambda e: e.tensor_scalar(out=ohg[p][:], in0=lg[p][:, 0:8], scalar1=mx8[p][:, 0:1], scalar2=None, op0=ALU.is_equal),
                  reads=[('lg', p), ('mx8', p)], writes=[('ohg', p)])
            sc.op('dve', lambda e: e.tensor_scalar(out=exg[p][:], in0=lg[p][:, 0:8], scalar1=mx8[p][:, 0:1], scalar2=None, op0=ALU.subtract),
                  reads=[('lg', p), ('mx8', p)], writes=[('exg', p)])
            sc.op('act', lambda e: e.activation(out=exg[p][:], in_=exg[p][:], func=AF.Exp), reads=[('exg', p)], writes=[('exg', p)])
            sc.op('dve', lambda e: e.tensor_reduce(out=sc1[p][:, 0:1], in_=exg[p][:], axis=AX.X, op=ALU.add), reads=[('exg', p)], writes=[('sc1a', p)])
            sc.op('dve', lambda e: e.reciprocal(out=sc1[p][:, 1:2], in_=sc1[p][:, 0:1]), reads=[('sc1a', p)], writes=[('gw', p)])
            sc.op('dve', lambda e: e.tensor_tensor(out=tmp88[p][:].rearrange("p (g e) -> p g e", e=8),
                                                   in0=lg[p][:, 8:72].rearrange("p (g e) -> p g e", e=8),
                                                   in1=ohg[p][:].unsqueeze(2).broadcast_to([128, 8, 8]), op=ALU.mult),
                  reads=[('lg', p), ('ohg', p)], writes=[('tmp88', p)])
            sc.op('dve', lambda e: e.tensor_reduce(out=el[p][:], in_=tmp88[p][:].rearrange("p (g e) -> p e g", e=8), axis=AX.X, op=ALU.add),
                  reads=[('tmp88', p)], writes=[('el', p)])
            sc.op('dve', lambda e: e.max(out=mxe[p][:], in_=el[p][:]), reads=[('el', p)], writes=[('mxe', p)])
            for c in range(2):
                sc.op('dve', lambda e, c=c: e.tensor_scalar(out=oh[c][p][:], in0=el[p][:], scalar1=mxe[p][:, c:c + 1], scalar2=None, op0=ALU.is_equal),
                      reads=[('el', p), ('mxe', p)], writes=[('oh', c, p)])
            sc.op('dve', lambda e: e.tensor_tensor(out=sc1[p][:, 2:3], in0=mxe[p][:, 1:2], in1=mxe[p][:, 0:1], op=ALU.subtract),
                  reads=[('mxe', p)], writes=[('sc1c', p)])
            sc.op('act', lambda e: e.activation(out=sc1[p][:, 2:3], in_=sc1[p][:, 2:3], func=AF.Exp), reads=[('sc1c', p)], writes=[('sc1c', p)])
            sc.op('dve', lambda e: e.tensor_scalar(out=sc1[p][:, 3:4], in0=sc1[p][:, 2:3], scalar1=1.0, scalar2=None, op0=ALU.add),
                  reads=[('sc1c', p)], writes=[('sc1d', p)])
            sc.op('dve', lambda e: e.reciprocal(out=sc1[p][:, 4:5], in_=sc1[p][:, 3:4]), reads=[('sc1d', p)], writes=[('p1', p)])
            sc.op('dve', lambda e: e.tensor_scalar(out=sc1[p][:, 5:6], in0=sc1[p][:, 4:5], scalar1=-1.0, scalar2=1.0, op0=ALU.mult, op1=ALU.add),
                  reads=[('p1', p)], writes=[('p2', p)])
            for c in range(2):
                sc.op('dve', lambda e, c=c: e.tensor_tensor(out=Ac[c][p][:].rearrange("p (g e) -> p g e", e=8),
                                                            in0=ohg[p][:].unsqueeze(2).broadcast_to([128, 8, 8]),
                                                            in1=oh[c][p][:].unsqueeze(1).broadcast_to([128, 8, 8]), op=ALU.mult),
                      reads=[('ohg', p), ('oh', c, p)], writes=[('Ac', c, p)])
            sc.op('dve', lambda e: e.tensor_tensor(out=Ab[p][:], in0=Ac[0][p][:], in1=Ac[1][p][:], op=ALU.add),
                  reads=[('Ac', 0, p), ('Ac', 1, p)], writes=[('Ab', p)])
            bq = 7
            sc.op('pe', lambda e: e.matmul(bank(bq, 64, 0), lhsT=strib[:], rhs=Ab[p][:], start=True, stop=True),
                  reads=['strib', ('Ab', p)], ww=[('bank', bq)], inc=False)
            sc.op('pe', lambda e: e.matmul(bank(bq, 64, 64), lhsT=onesb[:], rhs=Ab[p][:], start=True, stop=True),
                  reads=['onesb', ('Ab', p)], writes=[('bank', bq)])
            sc.op('dve', lambda e: e.tensor_tensor(out=pos[p][:], in0=bank(bq, 64, 0), in1=run[:], op=ALU.add),
                  reads=[('bank', bq), 'run'], writes=[('pos', p)])
            sc.op('dve', lambda e: e.tensor_tensor(out=run[:], in0=bank(bq, 64, 64), in1=run[:], op=ALU.add),
                  reads=[('bank', bq), 'run'], writes=['run'])
            for c in range(2):
                sc.op('dve', lambda e, c=c: e.tensor_tensor(out=prod[p][:], in0=Ac[c][p][:], in1=pos[p][:], op=ALU.mult),
                      reads=[('Ac', c, p), ('pos', p)], writes=[('prod', p)])
                sc.op('dve', lambda e: e.tensor_reduce(out=ridf[p][:, 0:1], in_=prod[p][:], axis=AX.X, op=ALU.add),
                      reads=[('prod', p)], writes=[('ridf0', p)])
                sc.op('dve', lambda e, c=c: e.tensor_tensor(out=prod[p][:], in0=Ac[c][p][:], in1=iota64[:], op=ALU.mult),
                      reads=[('Ac', c, p), 'iota64'], writes=[('prod', p)])
                sc.op('dve', lambda e: e.tensor_reduce(out=ridf[p][:, 1:2], in_=prod[p][:], axis=AX.X, op=ALU.add),
                      reads=[('prod', p)], writes=[('ridf1', p)])
                sc.op('dve', lambda e: e.scalar_tensor_tensor(out=ridf[p][:, 0:1], in0=ridf[p][:, 1:2], scalar=float(CAP), in1=ridf[p][:, 0:1],
                                                              op0=ALU.mult, op1=ALU.add),
                      reads=[('ridf0', p), ('ridf1', p)], writes=[('ridf0', p)])
                sc.op('dve', lambda e, c=c: e.tensor_copy(out=ridx[c][p][:], in_=ridf[p][:, 0:1]), reads=[('ridf0', p)], writes=[('ridx', c, p)])
                sc.op('dve', lambda e, c=c: e.tensor_copy(out=rowt[c][p][:, 0:1], in_=tokid[:, i:i + 1]), reads=['tokid'], writes=[('row', c, p)])
                sc.op('dve', lambda e, c=c: e.tensor_scalar(out=rowt[c][p][:, 1:2], in0=tokid[:, i:i + 1], scalar1=float(c * S), scalar2=None, op0=ALU.add),
                      reads=['tokid', ('row', c, p)], writes=[('row', c, p)])
                sc.op('dve', lambda e, c=c: e.tensor_tensor(out=rowt[c][p][:, 2:3], in0=sc1[p][:, 1:2], in1=sc1[p][:, 4 + c:5 + c], op=ALU.mult),
                      reads=[('gw', p), ('p1', p), ('p2', p), ('row', c, p)], writes=[('row', c, p)])
                sc.op('dve', lambda e, c=c: e.memset(rowt[c][p][:, 3:4], 0.0), reads=[('row', c, p)], writes=[('row', c, p)])
                sc.dma('pool', tab_d[:, :], rowt[c][p][:], reads=[('row', c, p), ('ridx', c, p), 'tab_d'], writes=[('tabw', i, c)],
                       indirect=dict(out_offset=bass.IndirectOffsetOnAxis(ap=ridx[c][p][:, 0:1], axis=0), in_offset=None))
    sc.barrier()
    if 'x1' in dbg:
        d = dout("dbg_x1", [S, D])
        sc.dma('sp', d[:, :], x1_d[:, :], reads=[('x1_d', i) for i in range(NT)])
        d2 = dout("dbg_tab", [64 * CAP, 4])
        sc.dma('sp', d2[:, :], tab_d[:, :], reads=[('tabw', i, c) for i in range(NT) for c in range(2)])
    if 'stopC' in dbg:
        sc.barrier()
        sc.finish()
        return

    wgate_d = din("w_gate", [64, D, 512])
    wup_d = din("w_up", [64, D, 512])
    wdown_d = din("w_down", [64, 512, D])
    ln2g_d = din("ln2_g", [D])
    ln2b_d = din("ln2_b", [D])
    with contextlib.ExitStack() as ph:
        def sbp(name, shape, dt):
            return ph.enter_context(nc.sbuf_tensor(name, list(shape), dt))
        tabS = sbp("tabS", [128, 64, 4], F32)
        gidx = sbp("gidx", [128, 64], I32)
        sidx = sbp("sidx", [128, 64], I32)
        tabw = [('tabw', i, c) for i in range(NT) for c in range(2)]
        tab_v = tab_d.rearrange("(e s) c -> s e c", s=CAP)
        for q in range(4):
            sc.dma('sp', tabS[:, q * 16:(q + 1) * 16, :], tab_v[:, q * 16:(q + 1) * 16, :], reads=tabw, writes=[('tabS', q)])
        tq = [('tabS', q) for q in range(4)]
        sc.op('dve', lambda e: e.tensor_copy(out=gidx[:], in_=tabS[:, :, 0]), reads=tq, writes=['gidx'])
        sc.op('dve', lambda e: e.tensor_copy(out=sidx[:], in_=tabS[:, :, 1]), reads=tq, writes=['sidx'])
        wge = [sbp("wge%d" % a, [128, 8, 512], BF16) for a in range(2)]
        wue = [sbp("wue%d" % a, [128, 8, 512], BF16) for a in range(2)]
        wde = [sbp("wde%d" % a, [128, 4, D], BF16) for a in range(2)]
        xg = [sbp("xg%d" % a, [128, D], BF16) for a in range(2)]
        xgT = [sbp("xgT%d" % a, [128, 8, 128], BF16) for a in range(2)]
        sil = [sbp("sil%d" % a, [128, 512], F32) for a in range(2)]
        hT = [sbp("hT%d" % a, [128, 4, 128], BF16) for a in range(2)]
        yw = [sbp("yw%d" % a, [128, D], F32) for a in range(2)]

        def load_expert(ex):
            p = ex % 2
            sc.dma('pool', xg[p][:], x1_d[:, :], reads=[('x1_d', i) for i in range(NT)] + ['gidx'], writes=[('xg', p)],
                   indirect=dict(out_offset=None, in_offset=bass.IndirectOffsetOnAxis(ap=gidx[:, ex:ex + 1], axis=0)))
            sc.dma('pool', wge[p][:], wgate_d[ex].rearrange("(c p) n -> p c n", p=128), writes=[('wge', p)])
            sc.dma('pool', wue[p][:], wup_d[ex].rearrange("(c p) n -> p c n", p=128), writes=[('wue', p)])
            for half in range(2):
                sc.dma('pool', wde[p][:, :, half * 512:(half + 1) * 512],
                       wdown_d[ex].rearrange("(c p) n -> p c n", p=128)[:, :, half * 512:(half + 1) * 512], writes=[('wde', p, half)])

        load_expert(0)
        for ex in range(64):
            p = ex % 2
            if ex + 1 < 64:
                load_expert(ex + 1)
            bt = p
            for kc in range(8):
                sc.op('pe', lambda e, kc=kc: e.transpose(out=bankbf(bt)[:, kc * 128:(kc + 1) * 128], in_=xg[p][:, kc * 128:(kc + 1) * 128],
                                                         identity=identb[:]),
                      reads=[('xg', p), 'identb'], writes=[('bank', bt)] if kc == 7 else [], ww=[('bank', bt)], inc=(kc == 7))
            sc.op('act', lambda e: e.copy(out=xgT[p][:].rearrange("p c t -> p (c t)"), in_=bankbf(bt)), reads=[('bank', bt)], writes=[('xgT', p)])
            bG, bU = 2 + p, 4 + p
            for (bk, wt, wk) in ((bG, wge, 'wge'), (bU, wue, 'wue')):
                for fcn in range(4):
                    for kc in range(8):
                        last = (fcn == 3 and kc == 7)
                        sc.op('pe', lambda e, kc=kc, fcn=fcn, wt=wt, bk=bk: e.matmul(bank(bk, 128, fcn * 128), lhsT=wt[p][:, kc, fcn * 128:(fcn + 1) * 128],
                                                                                   rhs=xgT[p][:, kc, :], start=(kc == 0), stop=(kc == 7)),
                              reads=[('xgT', p), (wk, p)], writes=[('bank', bk)] if last else [], ww=[('bank', bk)], inc=last)
            sc.op('act', lambda e: e.activation(out=sil[p][:], in_=bank(bG), func=AF.Silu), reads=[('bank', bG)], writes=[('sil', p)])
            sc.op('dve', lambda e: e.tensor_tensor(out=hT[p][:].rearrange("p c t -> p (c t)"), in0=sil[p][:], in1=bank(bU), op=ALU.mult),
                  reads=[('sil', p), ('bank', bU)], writes=[('hT', p)])
            for half in range(2):
                for fcn in range(4):
                    sc.op('pe', lambda e, fcn=fcn: e.matmul(bank(6 + half), lhsT=hT[p][:, fcn, :], rhs=wde[p][:, fcn, half * 512:(half + 1) * 512],
                                                           start=(fcn == 0), stop=(fcn == 3)),
                          reads=[('hT', p), ('wde', p, half)], writes=[('bank', 6 + half)] if fcn == 3 else [], ww=[('bank', 6 + half)], inc=(fcn == 3))
            sc.op('act', lambda e: e.activation(out=yw[p][:], in_=ps[:, 6 * 512:8 * 512], func=AF.Copy, scale=tabS[:, ex, 2:3]),
                  reads=[('bank', 6), ('bank', 7)] + tq, writes=[('yw', p)])
            sc.dma('pool', ybuf_d[:, :], yw[p][:], reads=[('yw', p), 'sidx'], writes=[('ybuf', ex)],
                   indirect=dict(out_offset=bass.IndirectOffsetOnAxis(ap=sidx[:, ex:ex + 1], axis=0), in_offset=None))
        sc.barrier()

        g2B = sbp("g2B", [128, D], F32)
        b2B = sbp("b2B", [128, D], F32)
        sc.dma('sp', g2B[:], ln2g_d.partition_broadcast(128), writes=['g2B'])
        sc.dma('sp', b2B[:], ln2b_d.partition_broadcast(128), writes=['b2B'])
        xa = [sbp("xa%d" % a, [128, D], F32) for a in range(2)]
        ya_ = [sbp("yA%d" % a, [128, D], F32) for a in range(2)]
        yb_ = [sbp("yB%d" % a, [128, D], F32) for a in range(2)]
        for i in range(NT):
            p = i % 2
            sc.dma('sp', xa[p][:], x1_d[i * 128:(i + 1) * 128, :], writes=[('xa', p)])
            sc.dma('sp', ya_[p][:], ybuf_d[i * 128:(i + 1) * 128, :], writes=[('yA', p)])
            sc.dma('sp', yb_[p][:], ybuf_d[S + i * 128: S + (i + 1) * 128, :], writes=[('yB', p)])
            sc.op('pool', lambda e: e.tensor_tensor(out=ya_[p][:], in0=ya_[p][:], in1=yb_[p][:], op=ALU.add),
                  reads=[('yA', p), ('yB', p)], writes=[('yA', p)])
            sc.op('dve', lambda e: e.scalar_tensor_tensor(out=xa[p][:], in0=xa[p][:], scalar=ALPHA, in1=ya_[p][:], op0=ALU.mult, op1=ALU.add),
                  reads=[('xa', p), ('yA', p)], writes=[('xa', p)])
            layernorm_stats(xa[p], p, ('xa', p))
            sc.op('dve', lambda e: e.tensor_scalar(out=xa[p][:], in0=xa[p][:], scalar1=mv[p][:, 0:1], scalar2=rs[p][:, 1:2],
                                                   op0=ALU.subtract, op1=ALU.mult),
                  reads=[('xa', p), ('mv', p), ('rs', p)], writes=[('xa', p)])
            sc.op('pool', lambda e: e.tensor_tensor(out=xa[p][:], in0=xa[p][:], in1=g2B[:], op=ALU.mult),
                  reads=[('xa', p), 'g2B'], writes=[('xa', p)])
            sc.op('pool', lambda e: e.tensor_tensor(out=xa[p][:], in0=xa[p][:], in1=b2B[:], op=ALU.add),
                  reads=[('xa', p), 'b2B'], writes=[('xa', p)])
            sc.dma('sp', out_d[i * 128:(i + 1) * 128, :], xa[p][:], reads=[('xa', p)], writes=[('out', i)])

    sc.finish()


def host_consts():
    c = {}
    c["ident_bf"] = np.eye(128, dtype=np.float32).astype(ml_dtypes.bfloat16)
    half = 32
    inv_freq = (10000.0 ** (-np.arange(half, dtype=np.float32) / half)).astype(np.float32)
    ang = np.arange(S, dtype=np.float32)[:, None] * inv_freq[None, :]
    c["cos_t"] = np.cos(ang).astype(np.float32)
    c["sin_t"] = np.sin(ang).astype(np.float32)
    pp = np.arange(128)
    c["tri_bf"] = (pp[None, :] >= pp[:, None]).astype(np.float32).astype(ml_dtypes.bfloat16)
    c["blkind"] = (np.arange(S)[None, :] // 256 == np.arange(8)[:, None]).astype(np.float32).astype(ml_dtypes.bfloat16)
    c["tri_f32"] = (pp[None, :] >= pp[:, None]).astype(np.float32)
    c["ones_f32"] = np.ones((128, 128), np.float32)
    c["ident_f32"] = np.eye(128, dtype=np.float32)
    c["stri_bf"] = (pp[None, :] > pp[:, None]).astype(np.float32).astype(ml_dtypes.bfloat16)
    c["ones_bf"] = np.ones((128, 128), np.float32).astype(ml_dtypes.bfloat16)
    c["iota64"] = np.tile(np.arange(64, dtype=np.float32)[None, :], (128, 1))
    c["tokid"] = (np.arange(NT, dtype=np.float32)[None, :] * 128 + pp[:, None]).astype(np.float32)
    ti = np.zeros((64 * 128, 4), np.float32)
    ti[:, 1] = 2 * S + np.tile(np.arange(128, dtype=np.float32), 64)
    c["tab_init"] = ti
    return c


def make_in_maps(inputs, n_cores=8):
    c = host_consts()
    maps = []
    for b in range(n_cores):
        m = dict(c)
        m["x"] = np.ascontiguousarray(inputs["x"][b])
        m["ln0_g"] = np.ascontiguousarray(inputs["ln0_g"])
        m["ln0_b"] = np.ascontiguousarray(inputs["ln0_b"])
        m["w_in"] = np.ascontiguousarray(inputs["w_in"][0])
        cv = np.concatenate([inputs["conv_w"][0], inputs["conv_b"][0][None], inputs["gn_g"][0][None], inputs["skip"][0][None]], axis=0)
        m["chanv"] = np.ascontiguousarray(cv.reshape(7, 4, 128).transpose(2, 1, 0))
        m["wqk_m"] = np.ascontiguousarray(np.concatenate([inputs["w_mq"][0], inputs["w_mk"][0]], axis=2))
        m["b_if"] = np.ascontiguousarray(np.concatenate([inputs["b_i"][0], inputs["b_f"][0]]))
        m["w_attn_up"] = np.ascontiguousarray(inputs["w_attn_up"][0])
        m["w_mlstm_up"] = np.ascontiguousarray(inputs["w_mlstm_up"][0])
        m["w_out"] = np.ascontiguousarray(inputs["w_out"][0])
        m["ln1_g"] = np.ascontiguousarray(inputs["ln1_g"][0])
        m["w_gate"] = np.ascontiguousarray(inputs["w_gate"][0])
        m["w_up"] = np.ascontiguousarray(inputs["w_up"][0])
        m["w_down"] = np.ascontiguousarray(inputs["w_down"][0])
        m["ln2_g"] = np.ascontiguousarray(inputs["ln2_g"][0])
        m["ln2_b"] = np.ascontiguousarray(inputs["ln2_b"][0])
        m["ln1_b"] = np.ascontiguousarray(inputs["ln1_b"][0])
        m["w_router"] = np.ascontiguousarray(np.concatenate([inputs["w_router_group"][0], inputs["w_router_expert"][0]], axis=1))
        m["b_router"] = np.ascontiguousarray(np.concatenate([inputs["b_router_group"][0], inputs["b_router_expert"][0]]))
        maps.append(m)
    return maps


def kernel(**inputs):
    inputs = {k: np.asarray(v) for k, v in inputs.items()}
    nc = build_program()
    maps = make_in_maps(inputs, 8)
    res = run_bass_kernel_spmd(nc, maps, core_ids=list(range(8)))
    return np.stack([r["out"] for r in res.results], axis=0).astype(np.float32)
```

```python
import contextlib
import numpy as np
import ml_dtypes
import concourse.bass as bass
import concourse.mybir as mybir
from concourse.bass_utils import run_bass_kernel_spmd

F32 = mybir.dt.float32
BF16 = mybir.dt.bfloat16
I32 = mybir.dt.int32
U32 = mybir.dt.uint32
AF = mybir.ActivationFunctionType
ALU = mybir.AluOpType
AX = mybir.AxisListType

D = 1024
S = 2048
NT = 16
INW = 5128
LN_EPS = 1e-5
GN_EPS = 1e-6
ALPHA = 2.0 ** 0.25
NEG = -30000.0


class Sched:
    def __init__(self, nc, stack, n_dma_sems=40):
        self.nc = nc
        self.E = {'pe': nc.tensor, 'act': nc.scalar, 'dve': nc.vector, 'pool': nc.gpsimd, 'sp': nc.sync}
        self.csem = {e: stack.enter_context(nc.semaphore("c_" + e)) for e in ('pe', 'act', 'dve', 'pool')}
        self.cnt = {e: 0 for e in self.csem}
        self.seen = {e: {} for e in self.E}
        self.lastw = {}
        self.readers = {}
        self.dsems = [stack.enter_context(nc.semaphore("d%d" % i)) for i in range(n_dma_sems)]
        self.dval = [0] * n_dma_sems
        self.dnext = 0
        self.nwait = 0

    def _deps(self, reads, writes):
        toks = []
        for k in reads:
            if k in self.lastw:
                toks.append(self.lastw[k] + (False,))
            if isinstance(k, tuple) and k[0] == 'bank':
                for t in self.readers.get(k, ()):
                    toks.append(t + (True,))
        for k in writes:
            if k in self.lastw:
                toks.append(self.lastw[k] + (False,))
            for t in self.readers.get(k, ()):
                toks.append(t + (True,))
        return toks

    def _wait(self, e, toks):
        eng = self.E[e]
        need = {}
        for (sem, val, owner, war) in toks:
            if owner == e and (e == 'pe' or war):
                continue
            key = id(sem)
            if self.seen[e].get(key, 0) >= val:
                continue
            if key not in need or need[key][1] < val:
                need[key] = (sem, val)
        for key, (sem, val) in need.items():
            eng.wait_ge(sem, val)
            self.seen[e][key] = val
            self.nwait += 1

    def _record(self, tok, reads, writes):
        for k in reads:
            self.readers.setdefault(k, []).append(tok)
        for k in writes:
            self.lastw[k] = tok
            self.readers[k] = []

    def op(self, e, fn, reads=(), writes=(), inc=True, ww=()):
        self._wait(e, self._deps(reads, list(writes) + list(ww)))
        ins = fn(self.E[e])
        n = self.cnt[e] + 1
        tok = (self.csem[e], n, e)
        if inc:
            ins.then_inc(self.csem[e], 1)
            self.cnt[e] = n
        self._record(tok, reads, writes)
        return ins

    def dma(self, q, out, in_, reads=(), writes=(), indirect=None, **kw):
        i = self.dnext % len(self.dsems)
        self.dnext += 1
        sem, val = self.dsems[i], self.dval[i]
        toks = self._deps(reads, writes)
        if val > 0:
            toks.append((sem, val, None, False))
        self._wait(q, toks)
        if indirect is None:
            ins = self.E[q].dma_start(out=out, in_=in_, **kw)
        else:
            ins = self.E[q].indirect_dma_start(out=out, in_=in_, **indirect)
        ins.then_inc(sem, 16)
        self.dval[i] = val + 16
        tok = (sem, val + 16, None)
        self._record(tok, reads, writes)
        return ins

    def barrier(self):
        toks = []
        for i, sem in enumerate(self.dsems):
            if self.dval[i] > 0:
                toks.append((sem, self.dval[i], None, False))
        for e, sem in self.csem.items():
            if self.cnt[e] > 0:
                toks.append((sem, self.cnt[e], None, False))
        for e in self.E:
            self._wait(e, [t for t in toks])

    def finish(self):
        toks = []
        for i, sem in enumerate(self.dsems):
            if self.dval[i] > 0:
                toks.append((sem, self.dval[i], None, False))
        for e, sem in self.csem.items():
            if self.cnt[e] > 0:
                toks.append((sem, self.cnt[e], None, False))
        self._wait('sp', toks)


def build_program(dbg=None):
    dbg = dbg or set()
    nc = bass.Bass("TRN2", target_bir_lowering=False)
    stack = contextlib.ExitStack()
    with stack:
        _emit(nc, stack, dbg)
    return nc


def _emit(nc, stack, dbg):
    def din(name, shape, dt=F32):
        return nc.dram_tensor(name, list(shape), dt, kind="ExternalInput").ap()

    def dout(name, shape, dt=F32):
        return nc.dram_tensor(name, list(shape), dt, kind="ExternalOutput").ap()

    def sb(name, shape, dt):
        return stack.enter_context(nc.sbuf_tensor(name, list(shape), dt))

    x_d = din("x", [S, D])
    ln0g_d = din("ln0_g", [D])
    ln0b_d = din("ln0_b", [D])
    win_d = din("w_in", [D, INW])
    identb_d = din("ident_bf", [128, 128], BF16)
    cos_d = din("cos_t", [S, 32])
    sin_d = din("sin_t", [S, 32])
    tri_d = din("tri_bf", [128, 128], BF16)
    blk_d = din("blkind", [8, S], BF16)
    out_d = dout("out", [S, D])

    sc = Sched(nc, stack)

    ps = stack.enter_context(nc.psum_tensor("ps", [128, 4096], F32))
    identb = sb("identb", [128, 128], BF16)
    x0T = sb("x0T", [128, 8, S], BF16)
    def bank(b, n=512, off=0):
        return ps[:, b * 512 + off: b * 512 + off + n]

    def bankbf(b):
        return ps[:, b * 512:(b + 1) * 512].bitcast(BF16)

    bank_rr = [0]

    def next_bank():
        b = bank_rr[0] % 8
        bank_rr[0] += 1
        return b

    sc.dma('sp', identb[:], identb_d[:, :], writes=['identb'])
    x0_d = nc.dram_tensor("x0_scr", [S, D], F32, kind="Internal").ap()

    st6 = [sb("st6_%d" % i, [128, 2, 6], F32) for i in range(2)]
    mv = [sb("mv%d" % i, [128, 2], F32) for i in range(2)]
    rs = [sb("rs%d" % i, [128, 2], F32) for i in range(2)]
    eps_t = sb("eps_t", [128, 2], F32)
    ph1 = contextlib.ExitStack()

    def sb1(name, shape, dt):
        return ph1.enter_context(nc.sbuf_tensor(name, list(shape), dt))

    g0B = sb1("g0B", [128, D], F32)
    b0B = sb1("b0B", [128, D], F32)
    sc.dma('sp', g0B[:], ln0g_d.partition_broadcast(128), writes=['g0B'])
    sc.dma('sp', b0B[:], ln0b_d.partition_broadcast(128), writes=['b0B'])
    xt = [sb1("xt%d" % i, [128, D], F32) for i in range(4)]
    xn = [sb1("xn%d" % i, [128, D], F32) for i in range(2)]
    xb = [sb1("xb%d" % i, [128, D], BF16) for i in range(2)]

    def layernorm_stats(src, p, tag):
        for hh in range(2):
            sc.op('dve', lambda e, hh=hh: e.bn_stats(out=st6[p][:, hh, :], in_=src[:, hh * 512:(hh + 1) * 512]),
                  reads=[tag], writes=[('st6', p, hh)])
        sc.op('dve', lambda e: e.bn_aggr(out=mv[p][:], in_=st6[p][:].rearrange("p a b -> p (a b)")),
              reads=[('st6', p, 0), ('st6', p, 1)], writes=[('mv', p)])
        sc.op('act', lambda e: e.activation(out=rs[p][:, 0:1], in_=mv[p][:, 1:2], func=AF.Sqrt, bias=eps_t[:, 0:1], scale=1.0),
              reads=[('mv', p), 'eps'], writes=[('rs0', p)])
        sc.op('dve', lambda e: e.reciprocal(out=rs[p][:, 1:2], in_=rs[p][:, 0:1]),
              reads=[('rs0', p)], writes=[('rs', p)])

    sc.op('dve', lambda e: e.memset(eps_t[:, 0:1], LN_EPS), writes=['eps'])
    sc.op('dve', lambda e: e.memset(eps_t[:, 1:2], GN_EPS), writes=['eps'])

    def ld_x(i):
        sc.dma('sp', xt[i % 4][:], x_d[i * 128:(i + 1) * 128, :], writes=[('xt', i % 4)])
    ld_x(0)
    ld_x(1)
    for i in range(NT):
        p = i % 2
        p4 = i % 4
        if i + 2 < NT:
            ld_x(i + 2)
        layernorm_stats(xt[p4], p, ('xt', p4))
        sc.op('dve', lambda e: e.tensor_scalar(out=xn[p][:], in0=xt[p4][:], scalar1=mv[p][:, 0:1], scalar2=rs[p][:, 1:2],
                                               op0=ALU.subtract, op1=ALU.mult),
              reads=[('xt', p4), ('mv', p), ('rs', p)], writes=[('xn', p)])
        sc.op('pool', lambda e: e.tensor_tensor(out=xn[p][:], in0=xn[p][:], in1=g0B[:], op=ALU.mult),
              reads=[('xn', p), 'g0B'], writes=[('xn', p)])
        sc.op('pool', lambda e: e.tensor_tensor(out=xn[p][:], in0=xn[p][:], in1=b0B[:], op=ALU.add),
              reads=[('xn', p), 'b0B'], writes=[('xn', p)])
        sc.dma('sp', x0_d[i * 128:(i + 1) * 128, :], xn[p][:], reads=[('xn', p)], writes=[('x0_d', i)])
        sc.op('act', lambda e: e.copy(out=xb[p][:], in_=xn[p][:]), reads=[('xn', p)], writes=[('xb', p)])
        b = next_bank()
        for kc in range(8):
            sc.op('pe', lambda e, kc=kc: e.transpose(out=bankbf(b)[:, kc * 128:(kc + 1) * 128],
                                                     in_=xb[p][:, kc * 128:(kc + 1) * 128], identity=identb[:]),
                  reads=[('xb', p), 'identb'], writes=[('bank', b)] if kc == 7 else [], ww=[('bank', b)], inc=(kc == 7))
        sc.op('dve', lambda e: e.tensor_copy(out=x0T[:, :, i * 128:(i + 1) * 128],
                                             in_=bankbf(b).rearrange("p (c t) -> p c t", c=8)),
              reads=[('bank', b)], writes=[('x0T', i)])

    if 'x0T' in dbg:
        d = dout("dbg_x0T", [128, 8, S], BF16)
        sc.dma('sp', d[:, :, :], x0T[:], reads=[('x0T', i) for i in range(NT)])

    if 'stage1_only' in dbg:
        for i in range(NT):
            p = i % 2
            sc.dma('sp', xt[p][:], x0_d[i * 128:(i + 1) * 128, :], reads=[('x0_d', i)], writes=[('xt', p)])
            sc.dma('sp', out_d[i * 128:(i + 1) * 128, :], xt[p][:], reads=[('xt', p)], writes=[('out', i)])
        sc.finish()
        return


    sc.barrier()
    ph1.close()
    y_aT = sb("y_aT", [128, 4, S], BF16)
    win_v = win_d.rearrange("(kc p) n -> p kc n", p=128)

    with contextlib.ExitStack() as ph:
        def sbp(name, shape, dt):
            return ph.enter_context(nc.sbuf_tensor(name, list(shape), dt))

        wqkv = sbp("wqkv", [128, 8, 1536], BF16)
        for g in range(3):
            sc.dma('pool', wqkv[:, :, g * 512:(g + 1) * 512], win_v[:, :, g * 512:(g + 1) * 512], writes=[('wqkv', g)])
        qT = sbp("qT", [72, 8, S], BF16)
        kT = sbp("kT", [72, 8, S], BF16)
        va = sbp("va", [128, NT, 8, 65], BF16)
        cosS = sbp("cosS", [128, NT, 32], F32)
        sinS = sbp("sinS", [128, NT, 32], F32)
        trib = sbp("trib", [128, 128], BF16)
        biasT = sbp("biasT", [64, S], BF16)
        sc.dma('sp', cosS[:], cos_d.rearrange("(i p) f -> p i f", p=128), writes=['cos'])
        sc.dma('sp', sinS[:], sin_d.rearrange("(i p) f -> p i f", p=128), writes=['sin'])
        sc.dma('sp', trib[:], tri_d[:, :], writes=['trib'])
        for h in range(8):
            sc.dma('sp', kT[64:72, h, :], blk_d[:, :], writes=[('kTaug', h)])
        sc.op('pool', lambda e: e.memset(va[:, :, :, 64:65], 1.0), writes=['va1'])
        tmp = [[sbp("rt%d_%d" % (a, b_), [128, 16, 32], F32) for b_ in range(4)] for a in range(2)]
        rot = [sbp("rot%d" % a, [128, 16, 64], BF16) for a in range(2)]

        for i in range(NT):
            p = i % 2
            for g in range(3):
                bk = (2 * p + g) if g < 2 else (4 + p)
                for kc in range(8):
                    sc.op('pe', lambda e, kc=kc: e.matmul(bank(bk), lhsT=x0T[:, kc, i * 128:(i + 1) * 128],
                                                         rhs=wqkv[:, kc, g * 512:(g + 1) * 512],
                                                         start=(kc == 0), stop=(kc == 7)),
                          reads=[('x0T', i), ('wqkv', g)], writes=[('bank', bk)] if kc == 7 else [], ww=[('bank', bk)], inc=(kc == 7))
            zz = ps[:, p * 1024:(p + 1) * 1024].rearrange("p (h d) -> p h d", d=64)
            zA, zB = zz[:, :, 0:32], zz[:, :, 32:64]
            cb = cosS[:, i, :].unsqueeze(1).broadcast_to([128, 16, 32])
            sb_ = sinS[:, i, :].unsqueeze(1).broadcast_to([128, 16, 32])
            bks = [('bank', 2 * p), ('bank', 2 * p + 1)]
            for t_i, (zin, tab, tk) in enumerate([(zA, cb, 'cos'), (zB, sb_, 'sin'), (zB, cb, 'cos'), (zA, sb_, 'sin')]):
                sc.op('dve', lambda e, zin=zin, tab=tab, t_i=t_i: e.tensor_tensor(out=tmp[p][t_i][:], in0=zin, in1=tab, op=ALU.mult),
                      reads=bks + [tk], writes=[('rt', p, t_i)])
            sc.op('pool', lambda e: e.tensor_tensor(out=rot[p][:, :, 0:32], in0=tmp[p][0][:], in1=tmp[p][1][:], op=ALU.subtract),
                  reads=[('rt', p, 0), ('rt', p, 1)], writes=[('rotA', p)])
            sc.op('pool', lambda e: e.tensor_tensor(out=rot[p][:, :, 32:64], in0=tmp[p][2][:], in1=tmp[p][3][:], op=ALU.add),
                  reads=[('rt', p, 2), ('rt', p, 3)], writes=[('rotB', p)])
            for qk in range(2):
                bt = 6 + qk
                for h in range(8):
                    sc.op('pe', lambda e, h=h: e.transpose(out=bankbf(bt)[0:64, h * 128:(h + 1) * 128],
                                                         in_=rot[p][:, qk * 8 + h, :], identity=identb[:]),
                          reads=[('rotA', p), ('rotB', p), 'identb'], writes=[('bank', bt)] if h == 7 else [], ww=[('bank', bt)], inc=(h == 7))
                src_v = bankbf(bt)[0:64, :].rearrange("p (h t) -> p h t", h=8)
                if qk == 0:
                    sc.op('act', lambda e: e.mul(out=qT[0:64, :, i * 128:(i + 1) * 128], in_=src_v, mul=0.125),
                          reads=[('bank', bt)], writes=[('qT', i)])
                else:
                    sc.op('act', lambda e: e.copy(out=kT[0:64, :, i * 128:(i + 1) * 128], in_=src_v),
                          reads=[('bank', bt)], writes=[('kT', i)])
            sc.op('act', lambda e: e.copy(out=va[:, i, :, 0:64], in_=bank(4 + p).rearrange("p (h d) -> p h d", d=64)),
                  reads=[('bank', 4 + p)], writes=[('va', i)])

        km32 = sbp("km32", [64, 8, 8], F32)
        kmb = sbp("kmb", [64, 8, 8], BF16)
        sc.op('dve', lambda e: e.tensor_reduce(out=km32[:], in_=kT[0:64, :, :].rearrange("p h (n k) -> p h n k", k=256),
                                               axis=AX.X, op=ALU.add),
              reads=[('kT', i) for i in range(NT)], writes=['km32'])
        sc.op('dve', lambda e: e.tensor_copy(out=kmb[:], in_=km32[:]), reads=['km32'], writes=['kmb'])
        G = [sbp("G%d" % a, [128, 8, 8], F32) for a in range(2)]
        cmpt = [sbp("cmp%d" % a, [128, 8, 8, 8], F32) for a in range(2)]
        rank = [sbp("rank%d" % a, [128, 8, 8], F32) for a in range(2)]
        biasb = [sbp("biasb%d" % a, [128, 8, 8], BF16) for a in range(2)]
        for i in range(NT):
            p = i % 2
            blk = i // 2
            bg = p
            for h in range(8):
                sc.op('pe', lambda e, h=h: e.matmul(bank(bg, 8, h * 8), lhsT=qT[0:64, h, i * 128:(i + 1) * 128], rhs=kmb[:, h, :],
                                                   start=True, stop=True),
                      reads=[('qT', i), 'kmb'], writes=[('bank', bg)] if h == 7 else [], ww=[('bank', bg)], inc=(h == 7))
            sc.op('act', lambda e: e.copy(out=G[p][:].rearrange("p h n -> p (h n)"), in_=bank(bg, 64)),
                  reads=[('bank', bg)], writes=[('G', p)])
            sc.op('pool', lambda e: e.memset(G[p][:, :, blk:8], -1e30), reads=[('G', p)], writes=[('G', p)])
            sc.op('dve', lambda e: e.tensor_tensor(out=cmpt[p][:], in0=G[p][:].unsqueeze(2).broadcast_to([128, 8, 8, 8]),
                                                   in1=G[p][:].unsqueeze(3).broadcast_to([128, 8, 8, 8]), op=ALU.is_gt),
                  reads=[('G', p)], writes=[('cmp', p)])
            sc.op('dve', lambda e: e.tensor_reduce(out=rank[p][:], in_=cmpt[p][:], axis=AX.X, op=ALU.add),
                  reads=[('cmp', p)], writes=[('rank', p)])
            sc.op('dve', lambda e: e.tensor_scalar(out=biasb[p][:], in0=rank[p][:], scalar1=3.0, scalar2=NEG,
                                                   op0=ALU.is_ge, op1=ALU.mult),
                  reads=[('rank', p)], writes=[('biasb', p)])
            sc.op('pool', lambda e: e.memset(biasb[p][:, :, blk:blk + 1], 0.0), reads=[('biasb', p)], writes=[('biasb', p)])
            bt = 2 + p
            sc.op('pe', lambda e: e.transpose(out=bankbf(bt)[0:64, 0:128], in_=biasb[p][:].rearrange("p h n -> p (h n)"),
                                              identity=identb[:]),
                  reads=[('biasb', p), 'identb'], writes=[('bank', bt)])
            sc.op('act', lambda e: e.copy(out=biasT[:, i * 128:(i + 1) * 128], in_=bankbf(bt)[0:64, 0:128]),
                  reads=[('bank', bt)], writes=[('biasT', i)])
        for h in range(8):
            sc.dma('sp', qT[64:72, h, :], biasT[h * 8:(h + 1) * 8, :], reads=[('biasT', i) for i in range(NT)],
                   writes=[('qTaug', h)])

        if 'qk' in dbg:
            dq = dout("dbg_qT", [72, 8, S], BF16)
            dk = dout("dbg_kT", [72, 8, S], BF16)
            sc.dma('sp', dq[:, :, :], qT[:], reads=[('qT', i) for i in range(NT)] + [('qTaug', h) for h in range(8)])
            sc.dma('sp', dk[:, :, :], kT[:], reads=[('kT', i) for i in range(NT)] + [('kTaug', h) for h in range(8)])

        PT = [sbp("PT%d" % a, [128, 4, 128], BF16) for a in range(3)]
        ya = [sbp("ya%d" % a, [128, 512], BF16) for a in range(2)]
        rden = [sbp("rden%d" % a, [128, 1], F32) for a in range(2)]
        groups = []
        for qt in range(NT):
            for h in range(8):
                for g0 in range(0, qt + 1, 4):
                    js = list(range(g0, min(g0 + 4, qt + 1)))
                    groups.append((qt, h, js))

        def emit_ST(k):
            qt, h, js = groups[k]
            sbk = k % 3
            for jj, j in enumerate(js):
                last = (jj == len(js) - 1)
                sc.op('pe', lambda e, jj=jj, j=j: e.matmul(bank(sbk, 128, jj * 128), lhsT=kT[0:72, h, j * 128:(j + 1) * 128],
                                                         rhs=qT[0:72, h, qt * 128:(qt + 1) * 128], start=True, stop=True),
                      reads=[('kT', j), ('kTaug', h), ('qT', qt), ('qTaug', h)],
                      writes=[('bank', sbk)] if last else [], ww=[('bank', sbk)], inc=last)

        def emit_rest(k):
            qt, h, js = groups[k]
            sbk = k % 3
            yp = qt % 2
            idx = qt * 8 + h
            ab = 3 + (idx % 2)
            ap_ = idx % 2
            n = len(js)
            sc.op('act', lambda e: e.activation(out=PT[sbk][:, 0:n, :].rearrange("p a b -> p (a b)"), in_=bank(sbk, n * 128), func=AF.Exp),
                  reads=[('bank', sbk)], writes=[('PT', sbk)])
            if js[-1] == qt:
                jj = n - 1
                sc.op('pool', lambda e: e.tensor_tensor(out=PT[sbk][:, jj, :], in0=PT[sbk][:, jj, :], in1=trib[:], op=ALU.mult),
                      reads=[('PT', sbk), 'trib'], writes=[('PT', sbk)])
            for jj, j in enumerate(js):
                last = (j == qt)
                sc.op('pe', lambda e, jj=jj, j=j: e.matmul(bank(ab, 65), lhsT=PT[sbk][:, jj, :], rhs=va[:, j, h, :],
                                                         start=(j == 0), stop=(j == qt)),
                      reads=[('PT', sbk), ('va', j), 'va1'], writes=[('bank', ab)] if last else [], ww=[('bank', ab)],
                      inc=(last or jj == n - 1))
            if js[-1] != qt:
                return
            sc.op('dve', lambda e: e.reciprocal(out=rden[ap_][:], in_=bank(ab, 1, 64)), reads=[('bank', ab)], writes=[('rden', ap_)])
            sc.op('dve', lambda e: e.tensor_scalar(out=ya[yp][:, h * 64:(h + 1) * 64], in0=bank(ab, 64), scalar1=rden[ap_][:, 0:1],
                                                   scalar2=None, op0=ALU.mult),
                  reads=[('bank', ab), ('rden', ap_)], writes=[('ya', yp, h)])
            if h != 7:
                return
            bt = 5
            for c in range(4):
                sc.op('pe', lambda e, c=c: e.transpose(out=bankbf(bt)[:, c * 128:(c + 1) * 128], in_=ya[yp][:, c * 128:(c + 1) * 128],
                                                     identity=identb[:]),
                      reads=[('ya', yp, hh_) for hh_ in range(8)] + ['identb'], writes=[('bank', bt)] if c == 3 else [], ww=[('bank', bt)],
                      inc=(c == 3))
            sc.op('act', lambda e: e.copy(out=y_aT[:, :, qt * 128:(qt + 1) * 128], in_=bankbf(bt)[:, 0:512].rearrange("p (c t) -> p c t", c=4)),
                  reads=[('bank', bt)], writes=[('y_aT', qt)])

        emit_ST(0)
        for k in range(len(groups)):
            if k + 1 < len(groups):
                emit_ST(k + 1)
            emit_rest(k)

    sc.barrier()
    if 'y_aT' in dbg:
        d = dout("dbg_y_aT", [128, 4, S], BF16)
        sc.dma('sp', d[:, :, :], y_aT[:], reads=[('y_aT', i) for i in range(NT)])

    y_mT = sb("y_mT", [128, 4, S], BF16)
    chanv_d = din("chanv", [128, 4, 7])
    wqk_d = din("wqk_m", [4, 128, 256])
    bif_d = din("b_if", [8])
    trif_d = din("tri_f32", [128, 128])
    onesf_d = din("ones_f32", [128, 128])
    with contextlib.ExitStack() as ph:
        def sbp(name, shape, dt):
            return ph.enter_context(nc.sbuf_tensor(name, list(shape), dt))

        chanv = sbp("chanv_s", [128, 4, 7], F32)
        wqk = sbp("wqk", [128, 4, 256], BF16)
        bifB = sbp("bifB", [128, 8], F32)
        trif = sbp("trif", [128, 128], F32)
        onesf = sbp("onesf", [128, 128], F32)
        trib = sbp("tribB", [128, 128], BF16)
        ones1 = sbp("ones1", [128, 1], F32)
        sc.dma('sp', chanv[:], chanv_d[:, :, :], writes=['chanv'])
        sc.dma('pool', wqk[:], wqk_d.rearrange("h p n -> p h n"), writes=['wqk'])
        sc.dma('sp', bifB[:], bif_d.partition_broadcast(128), writes=['bifB'])
        sc.dma('sp', trif[:], trif_d[:, :], writes=['trif'])
        sc.dma('sp', onesf[:], onesf_d[:, :], writes=['onesf'])
        sc.dma('sp', trib[:], tri_d[:, :], writes=['trib'])
        sc.op('dve', lambda e: e.memset(ones1[:], 1.0), writes=['ones1'])
        ucT = sbp("ucT", [128, 4, S], BF16)
        su = sbp("su", [128, 4, S], BF16)
        vm = sbp("vm", [128, NT, 4, 129], BF16)
        om = sbp("om", [128, NT, 512], BF16)
        pre = sbp("pre", [128, NT, 8], F32)
        sc.op('pool', lambda e: e.memset(vm[:, :, :, 128:129], 1.0), writes=['vm1'])

        with contextlib.ExitStack() as ph2:
            def sbq(name, shape, dt):
                return ph2.enter_context(nc.sbuf_tensor(name, list(shape), dt))
            wu = sbq("wu", [128, 8, 512], BF16)
            wvo = sbq("wvo", [128, 8, 1024], BF16)
            wif = sbq("wif", [128, 8, 8], BF16)
            sc.dma('pool', wu[:], win_v[:, :, 1536:2048], writes=['wu'])
            sc.dma('pool', wvo[:, :, 0:512], win_v[:, :, 2048:2560], writes=[('wvo', 0)])
            sc.dma('pool', wvo[:, :, 512:1024], win_v[:, :, 2560:3072], writes=[('wvo', 1)])
            sc.dma('pool', wif[:], win_v[:, :, 3072:3080], writes=['wif'])
            upad = [sbq("upad%d" % a, [128, S + 3], F32) for a in range(2)]
            cacc = [sbq("cacc%d" % a, [128, S], F32) for a in range(2)]
            for a in range(2):
                sc.op('pool', lambda e, a=a: e.memset(upad[a][:, 0:3], 0.0), writes=[('upadz', a)])
            for h in range(4):
                p = h % 2
                for tg in range(4):
                    b = next_bank()
                    for kc in range(8):
                        sc.op('pe', lambda e, kc=kc: e.matmul(bank(b), lhsT=wu[:, kc, h * 128:(h + 1) * 128],
                                                             rhs=x0T[:, kc, tg * 512:(tg + 1) * 512], start=(kc == 0), stop=(kc == 7)),
                              reads=['wu'] + [('x0T', tg * 4 + q) for q in range(4)], writes=[('bank', b)] if kc == 7 else [], ww=[('bank', b)], inc=(kc == 7))
                    sc.op('act', lambda e: e.copy(out=upad[p][:, 3 + tg * 512: 3 + (tg + 1) * 512], in_=bank(b)),
                          reads=[('bank', b)], writes=[('upad', p, tg)])
                ur = [('upad', p, tg) for tg in range(4)] + [('upadz', p)]
                sc.op('dve', lambda e: e.tensor_scalar(out=cacc[p][:], in0=upad[p][:, 0:S], scalar1=chanv[:, h, 0:1], scalar2=chanv[:, h, 4:5],
                                                       op0=ALU.mult, op1=ALU.add),
                      reads=ur + ['chanv'], writes=[('cacc', p)])
                for j in range(1, 4):
                    sc.op('dve', lambda e, j=j: e.scalar_tensor_tensor(out=cacc[p][:], in0=upad[p][:, j:S + j], scalar=chanv[:, h, j:j + 1],
                                                                       in1=cacc[p][:], op0=ALU.mult, op1=ALU.add),
                          reads=ur + ['chanv', ('cacc', p)], writes=[('cacc', p)])
                sc.op('act', lambda e: e.activation(out=ucT[:, h, :], in_=cacc[p][:], func=AF.Silu),
                      reads=[('cacc', p)], writes=[('ucT', h)])
                sc.op('pool', lambda e: e.tensor_scalar(out=su[:, h, :], in0=ucT[:, h, :], scalar1=chanv[:, h, 6:7], scalar2=None, op0=ALU.mult),
                      reads=[('ucT', h), 'chanv'], writes=[('su', h)])
            for i in range(NT):
                bs = []
                for g in range(3):
                    b = next_bank()
                    bs.append(b)
                    n = 512 if g < 2 else 8
                    for kc in range(8):
                        rhs = wvo[:, kc, g * 512:(g + 1) * 512] if g < 2 else wif[:, kc, :]
                        sc.op('pe', lambda e, kc=kc, rhs=rhs: e.matmul(bank(b, n), lhsT=x0T[:, kc, i * 128:(i + 1) * 128], rhs=rhs,
                                                                     start=(kc == 0), stop=(kc == 7)),
                              reads=[('x0T', i), ('wvo', g) if g < 2 else 'wif'], writes=[('bank', b)] if kc == 7 else [], ww=[('bank', b)], inc=(kc == 7))
                sc.op('act', lambda e: e.copy(out=vm[:, i, :, 0:128], in_=bank(bs[0]).rearrange("p (h d) -> p h d", d=128)),
                      reads=[('bank', bs[0])], writes=[('vm', i)])
                sc.op('act', lambda e: e.activation(out=om[:, i, :], in_=bank(bs[1]), func=AF.Sigmoid),
                      reads=[('bank', bs[1])], writes=[('om', i)])
                sc.op('dve', lambda e: e.tensor_tensor(out=pre[:, i, :], in0=bank(bs[2], 8), in1=bifB[:], op=ALU.add),
                      reads=[('bank', bs[2]), 'bifB'], writes=[('pre', i)])
        sc.barrier()
        if 'stopB1' in dbg:
            sc.finish()
            return

        Lall = sbp("Lall", [128, NT, 4], F32)
        eb = sbp("eb", [128, NT, 4], F32)
        e2 = sbp("e2", [128, NT, 4], F32)
        e3 = sbp("e3", [128, NT, 4], F32)
        dec = sbp("dec", [128, NT, 4], F32)
        a2 = sbp("a2", [128, NT, 4], F32)
        a3 = sbp("a3", [128, NT, 4], F32)
        prs = [('pre', i) for i in range(NT)]
        import os as _os
        _cut = int(_os.environ.get('GCUT', '99'))
        _real_op = sc.op
        _k = [0]

        def _cut_op(*a, **kw):
            _k[0] += 1
            if _k[0] <= _cut:
                return _real_op(*a, **kw)
            return None
        if 'stopB2' in dbg:
            sc.op = _cut_op
        sc.op('act', lambda e: e.activation(out=Lall[:], in_=pre[:, :, 4:8], func=AF.Exp, scale=-1.0), reads=prs, writes=['Lall'])
        sc.op('act', lambda e: e.activation(out=Lall[:], in_=Lall[:], func=AF.Ln, bias=ones1[:, 0:1], scale=1.0),
              reads=['Lall', 'ones1'], writes=['Lall'])
        bc, bg_ = next_bank(), next_bank()
        sc.op('pe', lambda e: e.matmul(bank(bc, 64), lhsT=trif[:], rhs=Lall[:].rearrange("p i h -> p (i h)"), start=True, stop=True),
              reads=['trif', 'Lall'], writes=[('bank', bc)])
        sc.op('pe', lambda e: e.matmul(bank(bg_, 64), lhsT=onesf[:], rhs=Lall[:].rearrange("p i h -> p (i h)"), start=True, stop=True),
              reads=['onesf', 'Lall'], writes=[('bank', bg_)])
        f64 = lambda t: t[:].rearrange("p i h -> p (i h)")
        sc.op('act', lambda e: e.activation(out=f64(eb), in_=bank(bc, 64), func=AF.Exp, scale=-1.0), reads=[('bank', bc)], writes=['eb'])
        sc.op('act', lambda e: e.activation(out=f64(dec), in_=bank(bg_, 64), func=AF.Exp, scale=-1.0), reads=[('bank', bg_)], writes=['dec'])
        sc.op('dve', lambda e: e.tensor_tensor(out=a2[:], in0=bank(bc, 64).rearrange("p (i h) -> p i h", h=4), in1=pre[:, :, 0:4], op=ALU.add),
              reads=[('bank', bc)] + prs, writes=['a2'])
        sc.op('dve', lambda e: e.tensor_tensor(out=f64(a3), in0=f64(a2), in1=bank(bg_, 64), op=ALU.subtract),
              reads=[('bank', bg_), 'a2'], writes=['a3'])
        sc.op('act', lambda e: e.activation(out=f64(e2), in_=f64(a2), func=AF.Exp), reads=['a2'], writes=['e2'])
        sc.op('act', lambda e: e.activation(out=f64(e3), in_=f64(a3), func=AF.Exp), reads=['a3'], writes=['e3'])
        KS = 128.0 ** -0.5
        sc.op('dve', lambda e: e.tensor_scalar(out=f64(e2), in0=f64(e2), scalar1=KS, scalar2=None, op0=ALU.mult), reads=['e2'], writes=['e2'])
        sc.op('dve', lambda e: e.tensor_scalar(out=f64(e3), in0=f64(e3), scalar1=KS, scalar2=None, op0=ALU.mult), reads=['e3'], writes=['e3'])

        if 'stopB2' in dbg:
            sc.op = _real_op
            sc.barrier()
            sc.finish()
            return
        S32 = sbp("S32", [128, 4, 129], F32)
        Sb = sbp("Sb", [128, 4, 129], BF16)
        sc.op('dve', lambda e: e.memset(S32[:], 0.0), writes=[('S32', h) for h in range(4)])
        sc.op('dve', lambda e: e.memset(Sb[:], 0.0), writes=[('Sb', h) for h in range(4)])
        NB = 8
        qs = [sbp("qs%d" % a, [128, 128], BF16) for a in range(NB)]
        k2 = [sbp("k2_%d" % a, [128, 128], BF16) for a in range(NB)]
        k3 = [sbp("k3_%d" % a, [128, 128], BF16) for a in range(NB)]
        qkT = [sbp("qkT%d" % a, [128, 2, 128], BF16) for a in range(NB)]
        PTm = [sbp("PTm%d" % a, [128, 128], BF16) for a in range(NB)]
        hh32 = [sbp("hh32_%d" % a, [128, 128], F32) for a in range(NB)]
        hn = [sbp("hn%d" % a, [128, 128], BF16) for a in range(NB)]
        gst = [sbp("gst%d" % a, [128, 6], F32) for a in range(NB)]
        gmv = [sbp("gmv%d" % a, [128, 2], F32) for a in range(NB)]
        grs = [sbp("grs%d" % a, [128, 4], F32) for a in range(NB)]
        HS = range(4)

        def partA(i):
            P = [(i % 2) * 4 + h for h in HS]
            bA = [next_bank() for h in HS]
            for h in HS:
                sc.op('pe', lambda e: e.matmul(bank(bA[h], 256), lhsT=ucT[:, h, i * 128:(i + 1) * 128], rhs=wqk[:, h, :], start=True, stop=True),
                      reads=[('ucT', h), 'wqk'], writes=[('bank', bA[h])])
            for h in HS:
                p = P[h]
                sc.op('act', lambda e: e.activation(out=qs[p][:], in_=bank(bA[h], 128, 0), func=AF.Copy, scale=eb[:, i, h:h + 1]),
                      reads=[('bank', bA[h]), 'eb'], writes=[('qs', p)])
                sc.op('act', lambda e: e.activation(out=k2[p][:], in_=bank(bA[h], 128, 128), func=AF.Copy, scale=e2[:, i, h:h + 1]),
                      reads=[('bank', bA[h]), 'e2'], writes=[('k2', p)])
                sc.op('dve', lambda e: e.tensor_scalar(out=k3[p][:], in0=bank(bA[h], 128, 128), scalar1=e3[:, i, h:h + 1], scalar2=None, op0=ALU.mult),
                      reads=[('bank', bA[h]), 'e3'], writes=[('k3', p)])
            bC = [next_bank() for h in HS]
            for h in HS:
                p = P[h]
                sc.op('pe', lambda e: e.transpose(out=bankbf(bC[h])[:, 0:128], in_=qs[p][:], identity=identb[:]),
                      reads=[('qs', p), 'identb'], inc=False, ww=[('bank', bC[h])])
                sc.op('pe', lambda e: e.transpose(out=bankbf(bC[h])[:, 128:256], in_=k2[p][:], identity=identb[:]),
                      reads=[('k2', p), 'identb'], writes=[('bank', bC[h])])
            for h in HS:
                p = P[h]
                sc.op('dve' if h % 2 == 0 else 'act',
                      (lambda e: e.tensor_copy(out=qkT[p][:].rearrange("p a t -> p (a t)"), in_=bankbf(bC[h])[:, 0:256])) if h % 2 == 0 else
                      (lambda e: e.copy(out=qkT[p][:].rearrange("p a t -> p (a t)"), in_=bankbf(bC[h])[:, 0:256])),
                      reads=[('bank', bC[h])], writes=[('qkT', p)])
            bD = [next_bank() for h in HS]
            for h in HS:
                p = P[h]
                sc.op('pe', lambda e: e.matmul(bank(bD[h], 128), lhsT=qkT[p][:, 1, :], rhs=qkT[p][:, 0, :], start=True, stop=True),
                      reads=[('qkT', p)], writes=[('bank', bD[h])])
            for h in HS:
                p = P[h]
                sc.op('dve', lambda e: e.tensor_tensor(out=PTm[p][:], in0=bank(bD[h], 128), in1=trib[:], op=ALU.mult),
                      reads=[('bank', bD[h]), 'trib'], writes=[('PTm', p)])

        def partB(i):
            P = [(i % 2) * 4 + h for h in HS]
            bF = [next_bank() for h in HS]
            for h in HS:
                p = P[h]
                sc.op('pe', lambda e: e.matmul(bank(bF[h], 129), lhsT=qkT[p][:, 0, :], rhs=Sb[:, h, :], start=True, stop=False),
                      reads=[('qkT', p), ('Sb', h)], inc=False, ww=[('bank', bF[h])])
                sc.op('pe', lambda e: e.matmul(bank(bF[h], 129), lhsT=PTm[p][:], rhs=vm[:, i, h, :], start=False, stop=True),
                      reads=[('PTm', p), ('vm', i), 'vm1'], writes=[('bank', bF[h])])
            bJ = [next_bank() for h in HS]
            for h in HS:
                p = P[h]
                sc.op('pe', lambda e: e.matmul(bank(bJ[h], 129), lhsT=k3[p][:], rhs=vm[:, i, h, :], start=True, stop=True),
                      reads=[('k3', p), ('vm', i), 'vm1'], writes=[('bank', bJ[h])])
            for h in HS:
                sc.op('dve', lambda e: e.scalar_tensor_tensor(out=S32[:, h, :], in0=S32[:, h, :], scalar=dec[:, i, h:h + 1],
                                                              in1=bank(bJ[h], 129), op0=ALU.mult, op1=ALU.add),
                      reads=[('S32', h), 'dec', ('bank', bJ[h])], writes=[('S32', h)])
            for h in HS:
                sc.op('act', lambda e: e.copy(out=Sb[:, h, :], in_=S32[:, h, :]), reads=[('S32', h)], writes=[('Sb', h)])
            for h in HS:
                p = P[h]
                sc.op('act', lambda e: e.activation(out=grs[p][:, 0:1], in_=bank(bF[h], 1, 128), func=AF.Abs),
                      reads=[('bank', bF[h])], writes=[('grs0', p)])
            for h in HS:
                p = P[h]
                sc.op('dve', lambda e: e.tensor_scalar(out=grs[p][:, 0:1], in0=grs[p][:, 0:1], scalar1=1.0, scalar2=None, op0=ALU.max),
                      reads=[('grs0', p)], writes=[('grs0', p)])
            for h in HS:
                p = P[h]
                sc.op('dve', lambda e: e.reciprocal(out=grs[p][:, 1:2], in_=grs[p][:, 0:1]), reads=[('grs0', p)], writes=[('grs1', p)])
            for h in HS:
                p = P[h]
                sc.op('dve', lambda e: e.scalar_tensor_tensor(out=hh32[p][:], in0=bank(bF[h], 128), scalar=grs[p][:, 1:2],
                                                              in1=om[:, i, h * 128:(h + 1) * 128], op0=ALU.mult, op1=ALU.mult),
                      reads=[('bank', bF[h]), ('grs1', p), ('om', i)], writes=[('hh32', p)])
            for h in HS:
                p = P[h]
                sc.op('dve', lambda e: e.bn_stats(out=gst[p][:], in_=hh32[p][:]), reads=[('hh32', p)], writes=[('gst', p)])
            for h in HS:
                p = P[h]
                sc.op('dve', lambda e: e.bn_aggr(out=gmv[p][:], in_=gst[p][:]), reads=[('gst', p)], writes=[('gmv', p)])
            for h in HS:
                p = P[h]
                sc.op('act', lambda e: e.activation(out=grs[p][:, 2:3], in_=gmv[p][:, 1:2], func=AF.Sqrt, bias=eps_t[:, 1:2], scale=1.0),
                      reads=[('gmv', p), 'eps'], writes=[('grs2', p)])
            for h in HS:
                p = P[h]
                sc.op('dve', lambda e: e.reciprocal(out=grs[p][:, 3:4], in_=grs[p][:, 2:3]), reads=[('grs2', p)], writes=[('grs3', p)])
            for h in HS:
                p = P[h]
                sc.op('dve', lambda e: e.tensor_scalar(out=hn[p][:], in0=hh32[p][:], scalar1=gmv[p][:, 0:1], scalar2=grs[p][:, 3:4],
                                                       op0=ALU.subtract, op1=ALU.mult),
                      reads=[('hh32', p), ('gmv', p), ('grs3', p)], writes=[('hn', p)])
            bI = [next_bank() for h in HS]
            for h in HS:
                p = P[h]
                sc.op('pe', lambda e: e.transpose(out=bankbf(bI[h])[:, 0:128], in_=hn[p][:], identity=identb[:]),
                      reads=[('hn', p), 'identb'], writes=[('bank', bI[h])])
            for h in HS:
                sc.op('dve', lambda e: e.scalar_tensor_tensor(out=y_mT[:, h, i * 128:(i + 1) * 128], in0=bankbf(bI[h])[:, 0:128],
                                                              scalar=chanv[:, h, 5:6], in1=su[:, h, i * 128:(i + 1) * 128],
                                                              op0=ALU.mult, op1=ALU.add),
                      reads=[('bank', bI[h]), 'chanv', ('su', h)], writes=[('y_mT', i, h)])

        partA(0)
        for i in range(NT):
            if i + 1 < NT:
                partA(i + 1)
            partB(i)

    sc.barrier()
    if 'y_mT' in dbg:
        d = dout("dbg_y_mT", [128, 4, S], BF16)
        sc.dma('sp', d[:, :, :], y_mT[:], reads=[('y_mT', i, h) for i in range(NT) for h in range(4)])

    CAP = 128
    YROWS = 2 * S + 128
    x1_d = nc.dram_tensor("x1_scr", [S, D], F32, kind="Internal").ap()
    tab_d = nc.dram_tensor("tab_scr", [64 * CAP, 4], F32, kind="Internal").ap()
    ybuf_d = nc.dram_tensor("ybuf_scr", [YROWS, D], F32, kind="Internal").ap()
    wau_d = din("w_attn_up", [512, D])
    wmu_d = din("w_mlstm_up", [512, D])
    wout_d = din("w_out", [D, D])
    ln1g_d = din("ln1_g", [D])
    ln1b_d = din("ln1_b", [D])
    wr_d = din("w_router", [D, 72])
    br_d = din("b_router", [72])
    identf_d = din("ident_f32", [128, 128])
    stri_d = din("stri_bf", [128, 128], BF16)
    onesb_d = din("ones_bf", [128, 128], BF16)
    iota64_d = din("iota64", [128, 64])
    tokid_d = din("tokid", [128, NT])
    tabinit_d = din("tab_init", [64 * CAP, 4])
    sc.dma('sp', tab_d[:, :], tabinit_d[:, :], writes=['tab_d'])

    with contextlib.ExitStack() as ph:
        def sbp(name, shape, dt):
            return ph.enter_context(nc.sbuf_tensor(name, list(shape), dt))
        mixT = sbp("mixT", [128, 8, S], BF16)
        phc = contextlib.ExitStack()
        _sbp_outer = sbp

        def sbp(name, shape, dt):
            return phc.enter_context(nc.sbuf_tensor(name, list(shape), dt))
        wg = sbp("wg", [128, 8, 2048], BF16)
        wau = sbp("wau", [128, 4, D], BF16)
        wmu = sbp("wmu", [128, 4, D], BF16)
        for g in range(4):
            sc.dma('pool', wg[:, :, g * 512:(g + 1) * 512], win_v[:, :, 3080 + g * 512: 3080 + (g + 1) * 512], writes=[('wg', g)])
        for g in range(2):
            sc.dma('pool', wau[:, :, g * 512:(g + 1) * 512], wau_d.rearrange("(c p) n -> p c n", p=128)[:, :, g * 512:(g + 1) * 512], writes=[('wau', g)])
            sc.dma('pool', wmu[:, :, g * 512:(g + 1) * 512], wmu_d.rearrange("(c p) n -> p c n", p=128)[:, :, g * 512:(g + 1) * 512], writes=[('wmu', g)])
        sg = [[sbp("sg%d_%d" % (a, b_), [128, 512], F32) for b_ in range(2)] for a in range(2)]
        it = 0
        for fc in range(8):
            for tg in range(4):
                p = it % 2
                it += 1
                bA, bB, bC, bD = 4 * p, 4 * p + 1, 4 * p + 2, 4 * p + 3
                xr = [('x0T', tg * 4 + q) for q in range(4)]
                for kc in range(8):
                    sc.op('pe', lambda e, kc=kc: e.matmul(bank(bA), lhsT=wg[:, kc, fc * 128:(fc + 1) * 128], rhs=x0T[:, kc, tg * 512:(tg + 1) * 512],
                                                         start=(kc == 0), stop=(kc == 7)),
                          reads=xr + [('wg', fc // 4)], writes=[('bank', bA)] if kc == 7 else [], ww=[('bank', bA)], inc=(kc == 7))
                for kc in range(8):
                    sc.op('pe', lambda e, kc=kc: e.matmul(bank(bB), lhsT=wg[:, kc, 1024 + fc * 128: 1024 + (fc + 1) * 128],
                                                         rhs=x0T[:, kc, tg * 512:(tg + 1) * 512], start=(kc == 0), stop=(kc == 7)),
                          reads=xr + [('wg', 2 + fc // 4)], writes=[('bank', bB)] if kc == 7 else [], ww=[('bank', bB)], inc=(kc == 7))
                for c in range(4):
                    sc.op('pe', lambda e, c=c: e.matmul(bank(bC), lhsT=wau[:, c, fc * 128:(fc + 1) * 128], rhs=y_aT[:, c, tg * 512:(tg + 1) * 512],
                                                       start=(c == 0), stop=(c == 3)),
                          reads=[('y_aT', tg * 4 + q) for q in range(4)] + [('wau', fc // 4)], writes=[('bank', bC)] if c == 3 else [],
                          ww=[('bank', bC)], inc=(c == 3))
                for c in range(4):
                    sc.op('pe', lambda e, c=c: e.matmul(bank(bD), lhsT=wmu[:, c, fc * 128:(fc + 1) * 128], rhs=y_mT[:, c, tg * 512:(tg + 1) * 512],
                                                       start=(c == 0), stop=(c == 3)),
                          reads=[('y_mT', tg * 4 + q, h) for q in range(4) for h in range(4)] + [('wmu', fc // 4)],
                          writes=[('bank', bD)] if c == 3 else [], ww=[('bank', bD)], inc=(c == 3))
                sc.op('act', lambda e: e.activation(out=sg[p][0][:], in_=bank(bA), func=AF.Sigmoid), reads=[('bank', bA)], writes=[('sg', p, 0)])
                sc.op('act', lambda e: e.activation(out=sg[p][1][:], in_=bank(bB), func=AF.Sigmoid), reads=[('bank', bB)], writes=[('sg', p, 1)])
                sc.op('dve', lambda e: e.tensor_tensor(out=sg[p][0][:], in0=sg[p][0][:], in1=bank(bC), op=ALU.mult),
                      reads=[('sg', p, 0), ('bank', bC)], writes=[('sg', p, 0)])
                sc.op('dve', lambda e: e.tensor_tensor(out=sg[p][1][:], in0=sg[p][1][:], in1=bank(bD), op=ALU.mult),
                      reads=[('sg', p, 1), ('bank', bD)], writes=[('sg', p, 1)])
                sc.op('pool', lambda e: e.tensor_tensor(out=mixT[:, fc, tg * 512:(tg + 1) * 512], in0=sg[p][0][:], in1=sg[p][1][:], op=ALU.add),
                      reads=[('sg', p, 0), ('sg', p, 1)], writes=[('mixT', fc, tg)])
        sc.barrier()
        phc.close()
        sbp = _sbp_outer

        wout = sbp("wout", [128, 8, D], BF16)
        for g in range(2):
            sc.dma('pool', wout[:, :, g * 512:(g + 1) * 512], wout_d.rearrange("(c p) n -> p c n", p=128)[:, :, g * 512:(g + 1) * 512], writes=[('wout', g)])
        g1B = sbp("g1B", [128, D], F32)
        b1B = sbp("b1B", [128, D], F32)
        wr = sbp("wr", [128, 8, 72], F32)
        brB = sbp("brB", [128, 72], F32)
        identf = sbp("identf", [128, 128], F32)
        strib = sbp("strib", [128, 128], BF16)
        onesb = sbp("onesb", [128, 128], BF16)
        iota64 = sbp("iota64s", [128, 64], F32)
        tokid = sbp("tokids", [128, NT], F32)
        sc.dma('sp', g1B[:], ln1g_d.partition_broadcast(128), writes=['g1B'])
        sc.dma('sp', b1B[:], ln1b_d.partition_broadcast(128), writes=['b1B'])
        sc.dma('sp', wr[:], wr_d.rearrange("(c p) n -> p c n", p=128), writes=['wr'])
        sc.dma('sp', brB[:], br_d.partition_broadcast(128), writes=['brB'])
        sc.dma('sp', identf[:], identf_d[:, :], writes=['identf'])
        sc.dma('sp', strib[:], stri_d[:, :], writes=['strib'])
        sc.dma('sp', onesb[:], onesb_d[:, :], writes=['onesb'])
        sc.dma('sp', iota64[:], iota64_d[:, :], writes=['iota64'])
        sc.dma('sp', tokid[:], tokid_d[:, :], writes=['tokid'])
        x0t = [sbp("x0t%d" % a, [128, D], F32) for a in range(4)]
        xp = [sbp("xp%d" % a, [128, D], F32) for a in range(2)]
        x1T = [sbp("x1T%d" % a, [128, 8, 128], F32) for a in range(2)]
        run = sbp("run", [128, 64], F32)
        sc.op('dve', lambda e: e.memset(run[:], 0.0), writes=['run'])

        def small(name, n, dt=F32):
            return [sbp("%s%d" % (name, a), [128, n], dt) for a in range(2)]
        lg = small("lg", 72)
        mx8 = small("mx8", 8)
        ohg = small("ohg", 8)
        exg = small("exg", 8)
        sc1 = small("sc1", 8)
        tmp88 = small("tmp88", 64)
        el = small("el", 8)
        mxe = small("mxe", 8)
        oh = [small("oh1_", 8), small("oh2_", 8)]
        Ac = [small("A1_", 64), small("A2_", 64)]
        Ab = small("Ab", 64, BF16)
        pos = small("pos", 64)
        prod = small("prod", 64)
        rowt = [small("row1_", 4), small("row2_", 4)]
        ridx = [small("ridx1_", 1, I32), small("ridx2_", 1, I32)]
        ridf = small("ridf", 2)

        def ld_x0(i):
            sc.dma('sp', x0t[i % 4][:], x0_d[i * 128:(i + 1) * 128, :], reads=[('x0_d', i)], writes=[('x0t', i % 4)])
        ld_x0(0)
        ld_x0(1)
        for i in range(NT):
            p = i % 2
            p4 = i % 4
            if i + 2 < NT:
                ld_x0(i + 2)
            bp = 2 * p
            for half in range(2):
                for kc in range(8):
                    sc.op('pe', lambda e, kc=kc: e.matmul(bank(bp + half), lhsT=mixT[:, kc, i * 128:(i + 1) * 128],
                                                         rhs=wout[:, kc, half * 512:(half + 1) * 512], start=(kc == 0), stop=(kc == 7)),
                          reads=[('mixT', kc, i // 4), ('wout', half)], writes=[('bank', bp + half)] if kc == 7 else [],
                          ww=[('bank', bp + half)], inc=(kc == 7))
            sc.op('dve', lambda e: e.scalar_tensor_tensor(out=xp[p][:], in0=x0t[p4][:], scalar=ALPHA, in1=ps[:, bp * 512:(bp + 2) * 512],
                                                          op0=ALU.mult, op1=ALU.add),
                  reads=[('x0t', p4), ('bank', bp), ('bank', bp + 1)], writes=[('xp', p)])
            layernorm_stats(xp[p], p, ('xp', p))
            sc.op('dve', lambda e: e.tensor_scalar(out=xp[p][:], in0=xp[p][:], scalar1=mv[p][:, 0:1], scalar2=rs[p][:, 1:2],
                                                   op0=ALU.subtract, op1=ALU.mult),
                  reads=[('xp', p), ('mv', p), ('rs', p)], writes=[('xp', p)])
            sc.op('pool', lambda e: e.tensor_tensor(out=xp[p][:], in0=xp[p][:], in1=g1B[:], op=ALU.mult),
                  reads=[('xp', p), 'g1B'], writes=[('xp', p)])
            sc.op('pool', lambda e: e.tensor_tensor(out=xp[p][:], in0=xp[p][:], in1=b1B[:], op=ALU.add),
                  reads=[('xp', p), 'b1B'], writes=[('xp', p)])
            sc.dma('sp', x1_d[i * 128:(i + 1) * 128, :], xp[p][:], reads=[('xp', p)], writes=[('x1_d', i)])
            bt = 4
            for kc in range(8):
                sc.op('pe', lambda e, kc=kc: e.transpose(out=ps[:, bt * 512 + kc * 128: bt * 512 + (kc + 1) * 128],
                                                         in_=xp[p][:, kc * 128:(kc + 1) * 128], identity=identf[:]),
                      reads=[('xp', p), 'identf'], writes=[('bank', bt), ('bank', bt + 1)] if kc == 7 else [],
                      ww=[('bank', bt), ('bank', bt + 1)], inc=(kc == 7))
            sc.op('act', lambda e: e.copy(out=x1T[p][:].rearrange("p c t -> p (c t)"), in_=ps[:, bt * 512:(bt + 2) * 512]),
                  reads=[('bank', bt), ('bank', bt + 1)], writes=[('x1T', p)])
            bl = 6
            for kc in range(8):
                sc.op('pe', lambda e, kc=kc: e.matmul(bank(bl, 72), lhsT=x1T[p][:, kc, :], rhs=wr[:, kc, :], start=(kc == 0), stop=(kc == 7)),
                      reads=[('x1T', p), 'wr'], writes=[('bank', bl)] if kc == 7 else [], ww=[('bank', bl)], inc=(kc == 7))
            sc.op('dve', lambda e: e.tensor_tensor(out=lg[p][:], in0=bank(bl, 72), in1=brB[:], op=ALU.add),
                  reads=[('bank', bl), 'brB'], writes=[('lg', p)])
            sc.op('dve', lambda e: e.max(out=mx8[p][:], in_=lg[p][:, 0:8]), reads=[('lg', p)], writes=[('mx8', p)])
            sc.op('dve', lambda e: e.tensor_scalar(out=ohg[p][:], in0=lg[p][:, 0:8], scalar1=mx8[p][:, 0:1], scalar2=None, op0=ALU.is_equal),
                  reads=[('lg', p), ('mx8', p)], writes=[('ohg', p)])
            sc.op('dve', lambda e: e.tensor_scalar(out=exg[p][:], in0=lg[p][:, 0:8], scalar1=mx8[p][:, 0:1], scalar2=None, op0=ALU.subtract),
                  reads=[('lg', p), ('mx8', p)], writes=[('exg', p)])
            sc.op('act', lambda e: e.activation(out=exg[p][:], in_=exg[p][:], func=AF.Exp), reads=[('exg', p)], writes=[('exg', p)])
            sc.op('dve', lambda e: e.tensor_reduce(out=sc1[p][:, 0:1], in_=exg[p][:], axis=AX.X, op=ALU.add), reads=[('exg', p)], writes=[('sc1a', p)])
            sc.op('dve', lambda e: e.reciprocal(out=sc1[p][:, 1:2], in_=sc1[p][:, 0:1]), reads=[('sc1a', p)], writes=[('gw', p)])
            sc.op('dve', lambda e: e.tensor_tensor(out=tmp88[p][:].rearrange("p (g e) -> p g e", e=8),
                                                   in0=lg[p][:, 8:72].rearrange("p (g e) -> p g e", e=8),
                                                   in1=ohg[p][:].unsqueeze(2).broadcast_to([128, 8, 8]), op=ALU.mult),
                  reads=[('lg', p), ('ohg', p)], writes=[('tmp88', p)])
            sc.op('dve', lambda e: e.tensor_reduce(out=el[p][:], in_=tmp88[p][:].rearrange("p (g e) -> p e g", e=8), axis=AX.X, op=ALU.add),
                  reads=[('tmp88', p)], writes=[('el', p)])
            sc.op('dve', lambda e: e.max(out=mxe[p][:], in_=el[p][:]), reads=[('el', p)], writes=[('mxe', p)])
            for c in range(2):
                sc.op('dve', lambda e, c=c: e.tensor_scalar(out=oh[c][p][:], in0=el[p][:], scalar1=mxe[p][:, c:c + 1], scalar2=None, op0=ALU.is_equal),
                      reads=[('el', p), ('mxe', p)], writes=[('oh', c, p)])
            sc.op('dve', lambda e: e.tensor_tensor(out=sc1[p][:, 2:3], in0=mxe[p][:, 1:2], in1=mxe[p][:, 0:1], op=ALU.subtract),
                  reads=[('mxe', p)], writes=[('sc1c', p)])
            sc.op('act', lambda e: e.activation(out=sc1[p][:, 2:3], in_=sc1[p][:, 2:3], func=AF.Exp), reads=[('sc1c', p)], writes=[('sc1c', p)])
            sc.op('dve', lambda e: e.tensor_scalar(out=sc1[p][:, 3:4], in0=sc1[p][:, 2:3], scalar1=1.0, scalar2=None, op0=ALU.add),
                  reads=[('sc1c', p)], writes=[('sc1d', p)])
            sc.op('dve', lambda e: e.reciprocal(out=sc1[p][:, 4:5], in_=sc1[p][:, 3:4]), reads=[('sc1d', p)], writes=[('p1', p)])
            sc.op('dve', lambda e: e.tensor_scalar(out=sc1[p][:, 5:6], in0=sc1[p][:, 4:5], scalar1=-1.0, scalar2=1.0, op0=ALU.mult, op1=ALU.add),
                  reads=[('p1', p)], writes=[('p2', p)])
            for c in range(2):
                sc.op('dve', lambda e, c=c: e.tensor_tensor(out=Ac[c][p][:].rearrange("p (g e) -> p g e", e=8),
                                                            in0=ohg[p][:].unsqueeze(2).broadcast_to([128, 8, 8]),
                                                            in1=oh[c][p][:].unsqueeze(1).broadcast_to([128, 8, 8]), op=ALU.mult),
                      reads=[('ohg', p), ('oh', c, p)], writes=[('Ac', c, p)])
            sc.op('dve', lambda e: e.tensor_tensor(out=Ab[p][:], in0=Ac[0][p][:], in1=Ac[1][p][:], op=ALU.add),
                  reads=[('Ac', 0, p), ('Ac', 1, p)], writes=[('Ab', p)])
            bq = 7
            sc.op('pe', lambda e: e.matmul(bank(bq, 64, 0), lhsT=strib[:], rhs=Ab[p][:], start=True, stop=True),
                  reads=['strib', ('Ab', p)], ww=[('bank', bq)], inc=False)
            sc.op('pe', lambda e: e.matmul(bank(bq, 64, 64), lhsT=onesb[:], rhs=Ab[p][:], start=True, stop=True),
                  reads=['onesb', ('Ab', p)], writes=[('bank', bq)])
            sc.op('dve', lambda e: e.tensor_tensor(out=pos[p][:], in0=bank(bq, 64, 0), in1=run[:], op=ALU.add),
                  reads=[('bank', bq), 'run'], writes=[('pos', p)])
            sc.op('dve', lambda e: e.tensor_tensor(out=run[:], in0=bank(bq, 64, 64), in1=run[:], op=ALU.add),
                  reads=[('bank', bq), 'run'], writes=['run'])
            for c in range(2):
                sc.op('dve', lambda e, c=c: e.tensor_tensor(out=prod[p][:], in0=Ac[c][p][:], in1=pos[p][:], op=ALU.mult),
                      reads=[('Ac', c, p), ('pos', p)], writes=[('prod', p)])
                sc.op('dve', lambda e: e.tensor_reduce(out=ridf[p][:, 0:1], in_=prod[p][:], axis=AX.X, op=ALU.add),
                      reads=[('prod', p)], writes=[('ridf0', p)])
                sc.op('dve', lambda e, c=c: e.tensor_tensor(out=prod[p][:], in0=Ac[c][p][:], in1=iota64[:], op=ALU.mult),
                      reads=[('Ac', c, p), 'iota64'], writes=[('prod', p)])
                sc.op('dve', lambda e: e.tensor_reduce(out=ridf[p][:, 1:2], in_=prod[p][:], axis=AX.X, op=ALU.add),
                      reads=[('prod', p)], writes=[('ridf1', p)])
                sc.op('dve', lambda e: e.scalar_tensor_tensor(out=ridf[p][:, 0:1], in0=ridf[p][:, 1:2], scalar=float(CAP), in1=ridf[p][:, 0:1],
                                                              op0=ALU.mult, op1=ALU.add),
                      reads=[('ridf0', p), ('ridf1', p)], writes=[('ridf0', p)])
                sc.op('dve', lambda e, c=c: e.tensor_copy(out=ridx[c][p][:], in_=ridf[p][:, 0:1]), reads=[('ridf0', p)], writes=[('ridx', c, p)])
                sc.op('dve', lambda e, c=c: e.tensor_copy(out=rowt[c][p][:, 0:1], in_=tokid[:, i:i + 1]), reads=['tokid'], writes=[('row', c, p)])
                sc.op('dve', lambda e, c=c: e.tensor_scalar(out=rowt[c][p][:, 1:2], in0=tokid[:, i:i + 1], scalar1=float(c * S), scalar2=None, op0=ALU.add),
                      reads=['tokid', ('row', c, p)], writes=[('row', c, p)])
                sc.op('dve', lambda e, c=c: e.tensor_tensor(out=rowt[c][p][:, 2:3], in0=sc1[p][:, 1:2], in1=sc1[p][:, 4 + c:5 + c], op=ALU.mult),
                      reads=[('gw', p), ('p1', p), ('p2', p), ('row', c, p)], writes=[('row', c, p)])
                sc.op('dve', lambda e, c=c: e.memset(rowt[c][p][:, 3:4], 0.0), reads=[('row', c, p)], writes=[('row', c, p)])
                sc.dma('pool', tab_d[:, :], rowt[c][p][:], reads=[('row', c, p), ('ridx', c, p), 'tab_d'], writes=[('tabw', i, c)],
                       indirect=dict(out_offset=bass.IndirectOffsetOnAxis(ap=ridx[c][p][:, 0:1], axis=0), in_offset=None))
    sc.barrier()
    if 'x1' in dbg:
        d = dout("dbg_x1", [S, D])
        sc.dma('sp', d[:, :], x1_d[:, :], reads=[('x1_d', i) for i in range(NT)])
        d2 = dout("dbg_tab", [64 * CAP, 4])
        sc.dma('sp', d2[:, :], tab_d[:, :], reads=[('tabw', i, c) for i in range(NT) for c in range(2)])
    if 'stopC' in dbg:
        sc.barrier()
        sc.finish()
        return

    wgate_d = din("w_gate", [64, D, 512])
    wup_d = din("w_up", [64, D, 512])
    wdown_d = din("w_down", [64, 512, D])
    ln2g_d = din("ln2_g", [D])
    ln2b_d = din("ln2_b", [D])
    with contextlib.ExitStack() as ph:
        def sbp(name, shape, dt):
            return ph.enter_context(nc.sbuf_tensor(name, list(shape), dt))
        tabS = sbp("tabS", [128, 64, 4], F32)
        gidx = sbp("gidx", [128, 64], I32)
        sidx = sbp("sidx", [128, 64], I32)
        tabw = [('tabw', i, c) for i in range(NT) for c in range(2)]
        tab_v = tab_d.rearrange("(e s) c -> s e c", s=CAP)
        for q in range(4):
            sc.dma('sp', tabS[:, q * 16:(q + 1) * 16, :], tab_v[:, q * 16:(q + 1) * 16, :], reads=tabw, writes=[('tabS', q)])
        tq = [('tabS', q) for q in range(4)]
        sc.op('dve', lambda e: e.tensor_copy(out=gidx[:], in_=tabS[:, :, 0]), reads=tq, writes=['gidx'])
        sc.op('dve', lambda e: e.tensor_copy(out=sidx[:], in_=tabS[:, :, 1]), reads=tq, writes=['sidx'])
        wge = [sbp("wge%d" % a, [128, 8, 512], BF16) for a in range(2)]
        wue = [sbp("wue%d" % a, [128, 8, 512], BF16) for a in range(2)]
        wde = [sbp("wde%d" % a, [128, 4, D], BF16) for a in range(2)]
        xg = [sbp("xg%d" % a, [128, D], BF16) for a in range(2)]
        xgT = [sbp("xgT%d" % a, [128, 8, 128], BF16) for a in range(2)]
        sil = [sbp("sil%d" % a, [128, 512], F32) for a in range(2)]
        hT = [sbp("hT%d" % a, [128, 4, 128], BF16) for a in range(2)]
        yw = [sbp("yw%d" % a, [128, D], F32) for a in range(2)]

        def load_expert(ex):
            p = ex % 2
            sc.dma('pool', xg[p][:], x1_d[:, :], reads=[('x1_d', i) for i in range(NT)] + ['gidx'], writes=[('xg', p)],
                   indirect=dict(out_offset=None, in_offset=bass.IndirectOffsetOnAxis(ap=gidx[:, ex:ex + 1], axis=0)))
            sc.dma('pool', wge[p][:], wgate_d[ex].rearrange("(c p) n -> p c n", p=128), writes=[('wge', p)])
            sc.dma('pool', wue[p][:], wup_d[ex].rearrange("(c p) n -> p c n", p=128), writes=[('wue', p)])
            for half in range(2):
                sc.dma('pool', wde[p][:, :, half * 512:(half + 1) * 512],
                       wdown_d[ex].rearrange("(c p) n -> p c n", p=128)[:, :, half * 512:(half + 1) * 512], writes=[('wde', p, half)])

        load_expert(0)
        for ex in range(64):
            p = ex % 2
            if ex + 1 < 64:
                load_expert(ex + 1)
            bt = p
            for kc in range(8):
                sc.op('pe', lambda e, kc=kc: e.transpose(out=bankbf(bt)[:, kc * 128:(kc + 1) * 128], in_=xg[p][:, kc * 128:(kc + 1) * 128],
                                                         identity=identb[:]),
                      reads=[('xg', p), 'identb'], writes=[('bank', bt)] if kc == 7 else [], ww=[('bank', bt)], inc=(kc == 7))
            sc.op('act', lambda e: e.copy(out=xgT[p][:].rearrange("p c t -> p (c t)"), in_=bankbf(bt)), reads=[('bank', bt)], writes=[('xgT', p)])
            bG, bU = 2 + p, 4 + p
            for (bk, wt, wk) in ((bG, wge, 'wge'), (bU, wue, 'wue')):
                for fcn in range(4):
                    for kc in range(8):
                        last = (fcn == 3 and kc == 7)
                        sc.op('pe', lambda e, kc=kc, fcn=fcn, wt=wt, bk=bk: e.matmul(bank(bk, 128, fcn * 128), lhsT=wt[p][:, kc, fcn * 128:(fcn + 1) * 128],
                                                                                   rhs=xgT[p][:, kc, :], start=(kc == 0), stop=(kc == 7)),
                              reads=[('xgT', p), (wk, p)], writes=[('bank', bk)] if last else [], ww=[('bank', bk)], inc=last)
            sc.op('act', lambda e: e.activation(out=sil[p][:], in_=bank(bG), func=AF.Silu), reads=[('bank', bG)], writes=[('sil', p)])
            sc.op('dve', lambda e: e.tensor_tensor(out=hT[p][:].rearrange("p c t -> p (c t)"), in0=sil[p][:], in1=bank(bU), op=ALU.mult),
                  reads=[('sil', p), ('bank', bU)], writes=[('hT', p)])
            for half in range(2):
                for fcn in range(4):
                    sc.op('pe', lambda e, fcn=fcn: e.matmul(bank(6 + half), lhsT=hT[p][:, fcn, :], rhs=wde[p][:, fcn, half * 512:(half + 1) * 512],
                                                           start=(fcn == 0), stop=(fcn == 3)),
                          reads=[('hT', p), ('wde', p, half)], writes=[('bank', 6 + half)] if fcn == 3 else [], ww=[('bank', 6 + half)], inc=(fcn == 3))
            sc.op('act', lambda e: e.activation(out=yw[p][:], in_=ps[:, 6 * 512:8 * 512], func=AF.Copy, scale=tabS[:, ex, 2:3]),
                  reads=[('bank', 6), ('bank', 7)] + tq, writes=[('yw', p)])
            sc.dma('pool', ybuf_d[:, :], yw[p][:], reads=[('yw', p), 'sidx'], writes=[('ybuf', ex)],
                   indirect=dict(out_offset=bass.IndirectOffsetOnAxis(ap=sidx[:, ex:ex + 1], axis=0), in_offset=None))
        sc.barrier()

        g2B = sbp("g2B", [128, D], F32)
        b2B = sbp("b2B", [128, D], F32)
        sc.dma('sp', g2B[:], ln2g_d.partition_broadcast(128), writes=['g2B'])
        sc.dma('sp', b2B[:], ln2b_d.partition_broadcast(128), writes=['b2B'])
        xa = [sbp("xa%d" % a, [128, D], F32) for a in range(4)]
        ya_ = [sbp("yA%d" % a, [128, D], F32) for a in range(4)]
        yb_ = [sbp("yB%d" % a, [128, D], F32) for a in range(4)]

        def ld_fin(i):
            q = i % 4
            sc.dma('sp', xa[q][:], x1_d[i * 128:(i + 1) * 128, :], writes=[('xa', q)])
            sc.dma('sp', ya_[q][:], ybuf_d[i * 128:(i + 1) * 128, :], writes=[('yA', q)])
            sc.dma('sp', yb_[q][:], ybuf_d[S + i * 128: S + (i + 1) * 128, :], writes=[('yB', q)])
        ld_fin(0)
        ld_fin(1)
        for i in range(NT):
            p = i % 4
            pl = i % 2
            if i + 2 < NT:
                ld_fin(i + 2)
            sc.op('pool', lambda e: e.tensor_tensor(out=ya_[p][:], in0=ya_[p][:], in1=yb_[p][:], op=ALU.add),
                  reads=[('yA', p), ('yB', p)], writes=[('yA', p)])
            sc.op('dve', lambda e: e.scalar_tensor_tensor(out=xa[p][:], in0=xa[p][:], scalar=ALPHA, in1=ya_[p][:], op0=ALU.mult, op1=ALU.add),
                  reads=[('xa', p), ('yA', p)], writes=[('xa', p)])
            layernorm_stats(xa[p], pl, ('xa', p))
            sc.op('dve', lambda e: e.tensor_scalar(out=xa[p][:], in0=xa[p][:], scalar1=mv[pl][:, 0:1], scalar2=rs[pl][:, 1:2],
                                                   op0=ALU.subtract, op1=ALU.mult),
                  reads=[('xa', p), ('mv', pl), ('rs', pl)], writes=[('xa', p)])
            sc.op('pool', lambda e: e.tensor_tensor(out=xa[p][:], in0=xa[p][:], in1=g2B[:], op=ALU.mult),
                  reads=[('xa', p), 'g2B'], writes=[('xa', p)])
            sc.op('pool', lambda e: e.tensor_tensor(out=xa[p][:], in0=xa[p][:], in1=b2B[:], op=ALU.add),
                  reads=[('xa', p), 'b2B'], writes=[('xa', p)])
            sc.dma('sp', out_d[i * 128:(i + 1) * 128, :], xa[p][:], reads=[('xa', p)], writes=[('out', i)])

    sc.finish()


def host_consts():
    c = {}
    c["ident_bf"] = np.eye(128, dtype=np.float32).astype(ml_dtypes.bfloat16)
    half = 32
    inv_freq = (10000.0 ** (-np.arange(half, dtype=np.float32) / half)).astype(np.float32)
    ang = np.arange(S, dtype=np.float32)[:, None] * inv_freq[None, :]
    c["cos_t"] = np.cos(ang).astype(np.float32)
    c["sin_t"] = np.sin(ang).astype(np.float32)
    pp = np.arange(128)
    c["tri_bf"] = (pp[None, :] >= pp[:, None]).astype(np.float32).astype(ml_dtypes.bfloat16)
    c["blkind"] = (np.arange(S)[None, :] // 256 == np.arange(8)[:, None]).astype(np.float32).astype(ml_dtypes.bfloat16)
    c["tri_f32"] = (pp[None, :] >= pp[:, None]).astype(np.float32)
    c["ones_f32"] = np.ones((128, 128), np.float32)
    c["ident_f32"] = np.eye(128, dtype=np.float32)
    c["stri_bf"] = (pp[None, :] > pp[:, None]).astype(np.float32).astype(ml_dtypes.bfloat16)
    c["ones_bf"] = np.ones((128, 128), np.float32).astype(ml_dtypes.bfloat16)
    c["iota64"] = np.tile(np.arange(64, dtype=np.float32)[None, :], (128, 1))
    c["tokid"] = (np.arange(NT, dtype=np.float32)[None, :] * 128 + pp[:, None]).astype(np.float32)
    ti = np.zeros((64 * 128, 4), np.float32)
    ti[:, 1] = 2 * S + np.tile(np.arange(128, dtype=np.float32), 64)
    c["tab_init"] = ti
    return c


def make_in_maps(inputs, n_cores=8):
    c = host_consts()
    maps = []
    for b in range(n_cores):
        m = dict(c)
        m["x"] = np.ascontiguousarray(inputs["x"][b])
        m["ln0_g"] = np.ascontiguousarray(inputs["ln0_g"])
        m["ln0_b"] = np.ascontiguousarray(inputs["ln0_b"])
        m["w_in"] = np.ascontiguousarray(inputs["w_in"][0])
        cv = np.concatenate([inputs["conv_w"][0], inputs["conv_b"][0][None], inputs["gn_g"][0][None], inputs["skip"][0][None]], axis=0)
        m["chanv"] = np.ascontiguousarray(cv.reshape(7, 4, 128).transpose(2, 1, 0))
        m["wqk_m"] = np.ascontiguousarray(np.concatenate([inputs["w_mq"][0], inputs["w_mk"][0]], axis=2))
        m["b_if"] = np.ascontiguousarray(np.concatenate([inputs["b_i"][0], inputs["b_f"][0]]))
        m["w_attn_up"] = np.ascontiguousarray(inputs["w_attn_up"][0])
        m["w_mlstm_up"] = np.ascontiguousarray(inputs["w_mlstm_up"][0])
        m["w_out"] = np.ascontiguousarray(inputs["w_out"][0])
        m["ln1_g"] = np.ascontiguousarray(inputs["ln1_g"][0])
        m["w_gate"] = np.ascontiguousarray(inputs["w_gate"][0])
        m["w_up"] = np.ascontiguousarray(inputs["w_up"][0])
        m["w_down"] = np.ascontiguousarray(inputs["w_down"][0])
        m["ln2_g"] = np.ascontiguousarray(inputs["ln2_g"][0])
        m["ln2_b"] = np.ascontiguousarray(inputs["ln2_b"][0])
        m["ln1_b"] = np.ascontiguousarray(inputs["ln1_b"][0])
        m["w_router"] = np.ascontiguousarray(np.concatenate([inputs["w_router_group"][0], inputs["w_router_expert"][0]], axis=1))
        m["b_router"] = np.ascontiguousarray(np.concatenate([inputs["b_router_group"][0], inputs["b_router_expert"][0]]))
        maps.append(m)
    return maps


def kernel(**inputs):
    inputs = {k: np.asarray(v) for k, v in inputs.items()}
    nc = build_program()
    maps = make_in_maps(inputs, 8)
    res = run_bass_kernel_spmd(nc, maps, core_ids=list(range(8)))
    return np.stack([r["out"] for r in res.results], axis=0).astype(np.float32)
```

```python
import contextlib
import numpy as np
import ml_dtypes
import concourse.bass as bass
import concourse.mybir as mybir
from concourse.bass_utils import run_bass_kernel_spmd

F32 = mybir.dt.float32
BF16 = mybir.dt.bfloat16
I32 = mybir.dt.int32
U32 = mybir.dt.uint32
AF = mybir.ActivationFunctionType
ALU = mybir.AluOpType
AX = mybir.AxisListType

D = 1024
S = 2048
NT = 16
INW = 5128
LN_EPS = 1e-5
GN_EPS = 1e-6
ALPHA = 2.0 ** 0.25
NEG = -30000.0


class Sched:
    def __init__(self, nc, stack, n_dma_sems=40):
        self.nc = nc
        self.E = {'pe': nc.tensor, 'act': nc.scalar, 'dve': nc.vector, 'pool': nc.gpsimd, 'sp': nc.sync}
        self.csem = {e: stack.enter_context(nc.semaphore("c_" + e)) for e in ('pe', 'act', 'dve', 'pool')}
        self.cnt = {e: 0 for e in self.csem}
        self.seen = {e: {} for e in self.E}
        self.lastw = {}
        self.readers = {}
        self.dsems = [stack.enter_context(nc.semaphore("d%d" % i)) for i in range(n_dma_sems)]
        self.dval = [0] * n_dma_sems
        self.dnext = 0
        self.nwait = 0

    def _deps(self, reads, writes):
        toks = []
        for k in reads:
            if k in self.lastw:
                toks.append(self.lastw[k] + (False,))
            if isinstance(k, tuple) and k[0] == 'bank':
                for t in self.readers.get(k, ()):
                    toks.append(t + (True,))
        for k in writes:
            if k in self.lastw:
                toks.append(self.lastw[k] + (False,))
            for t in self.readers.get(k, ()):
                toks.append(t + (True,))
        return toks

    def _wait(self, e, toks):
        eng = self.E[e]
        need = {}
        for (sem, val, owner, war) in toks:
            if owner == e and (e == 'pe' or war):
                continue
            key = id(sem)
            if self.seen[e].get(key, 0) >= val:
                continue
            if key not in need or need[key][1] < val:
                need[key] = (sem, val)
        for key, (sem, val) in need.items():
            eng.wait_ge(sem, val)
            self.seen[e][key] = val
            self.nwait += 1

    def _record(self, tok, reads, writes):
        for k in reads:
            self.readers.setdefault(k, []).append(tok)
        for k in writes:
            self.lastw[k] = tok
            self.readers[k] = []

    def op(self, e, fn, reads=(), writes=(), inc=True, ww=()):
        self._wait(e, self._deps(reads, list(writes) + list(ww)))
        ins = fn(self.E[e])
        n = self.cnt[e] + 1
        tok = (self.csem[e], n, e)
        if inc:
            ins.then_inc(self.csem[e], 1)
            self.cnt[e] = n
        self._record(tok, reads, writes)
        return ins

    def dma(self, q, out, in_, reads=(), writes=(), indirect=None, **kw):
        i = self.dnext % len(self.dsems)
        self.dnext += 1
        sem, val = self.dsems[i], self.dval[i]
        toks = self._deps(reads, writes)
        if val > 0:
            toks.append((sem, val, None, False))
        self._wait(q, toks)
        if indirect is None:
            ins = self.E[q].dma_start(out=out, in_=in_, **kw)
        else:
            ins = self.E[q].indirect_dma_start(out=out, in_=in_, **indirect)
        ins.then_inc(sem, 16)
        self.dval[i] = val + 16
        tok = (sem, val + 16, None)
        self._record(tok, reads, writes)
        return ins

    def barrier(self):
        toks = []
        for i, sem in enumerate(self.dsems):
            if self.dval[i] > 0:
                toks.append((sem, self.dval[i], None, False))
        for e, sem in self.csem.items():
            if self.cnt[e] > 0:
                toks.append((sem, self.cnt[e], None, False))
        for e in self.E:
            self._wait(e, [t for t in toks])

    def finish(self):
        toks = []
        for i, sem in enumerate(self.dsems):
            if self.dval[i] > 0:
                toks.append((sem, self.dval[i], None, False))
        for e, sem in self.csem.items():
            if self.cnt[e] > 0:
                toks.append((sem, self.cnt[e], None, False))
        self._wait('sp', toks)


def build_program(dbg=None):
    dbg = dbg or set()
    nc = bass.Bass("TRN2", target_bir_lowering=False)
    stack = contextlib.ExitStack()
    with stack:
        _emit(nc, stack, dbg)
    return nc


def _emit(nc, stack, dbg):
    def din(name, shape, dt=F32):
        return nc.dram_tensor(name, list(shape), dt, kind="ExternalInput").ap()

    def dout(name, shape, dt=F32):
        return nc.dram_tensor(name, list(shape), dt, kind="ExternalOutput").ap()

    def sb(name, shape, dt):
        return stack.enter_context(nc.sbuf_tensor(name, list(shape), dt))

    x_d = din("x", [S, D])
    ln0g_d = din("ln0_g", [D])
    ln0b_d = din("ln0_b", [D])
    win_d = din("w_in", [D, INW])
    identb_d = din("ident_bf", [128, 128], BF16)
    cos_d = din("cos_t", [S, 32])
    sin_d = din("sin_t", [S, 32])
    tri_d = din("tri_bf", [128, 128], BF16)
    blk_d = din("blkind", [8, S], BF16)
    out_d = dout("out", [S, D])

    sc = Sched(nc, stack)

    ps = stack.enter_context(nc.psum_tensor("ps", [128, 4096], F32))
    identb = sb("identb", [128, 128], BF16)
    x0T = sb("x0T", [128, 8, S], BF16)
    def bank(b, n=512, off=0):
        return ps[:, b * 512 + off: b * 512 + off + n]

    def bankbf(b):
        return ps[:, b * 512:(b + 1) * 512].bitcast(BF16)

    bank_rr = [0]

    def next_bank():
        b = bank_rr[0] % 8
        bank_rr[0] += 1
        return b

    sc.dma('sp', identb[:], identb_d[:, :], writes=['identb'])
    x0_d = nc.dram_tensor("x0_scr", [S, D], F32, kind="Internal").ap()

    st6 = [sb("st6_%d" % i, [128, 2, 6], F32) for i in range(2)]
    mv = [sb("mv%d" % i, [128, 2], F32) for i in range(2)]
    rs = [sb("rs%d" % i, [128, 2], F32) for i in range(2)]
    eps_t = sb("eps_t", [128, 2], F32)
    ph1 = contextlib.ExitStack()

    def sb1(name, shape, dt):
        return ph1.enter_context(nc.sbuf_tensor(name, list(shape), dt))

    g0B = sb1("g0B", [128, D], F32)
    b0B = sb1("b0B", [128, D], F32)
    sc.dma('sp', g0B[:], ln0g_d.partition_broadcast(128), writes=['g0B'])
    sc.dma('sp', b0B[:], ln0b_d.partition_broadcast(128), writes=['b0B'])
    xt = [sb1("xt%d" % i, [128, D], F32) for i in range(4)]
    xn = [sb1("xn%d" % i, [128, D], F32) for i in range(2)]
    xb = [sb1("xb%d" % i, [128, D], BF16) for i in range(2)]

    def layernorm_stats(src, p, tag):
        for hh in range(2):
            sc.op('dve', lambda e, hh=hh: e.bn_stats(out=st6[p][:, hh, :], in_=src[:, hh * 512:(hh + 1) * 512]),
                  reads=[tag], writes=[('st6', p, hh)])
        sc.op('dve', lambda e: e.bn_aggr(out=mv[p][:], in_=st6[p][:].rearrange("p a b -> p (a b)")),
              reads=[('st6', p, 0), ('st6', p, 1)], writes=[('mv', p)])
        sc.op('act', lambda e: e.activation(out=rs[p][:, 0:1], in_=mv[p][:, 1:2], func=AF.Sqrt, bias=eps_t[:, 0:1], scale=1.0),
              reads=[('mv', p), 'eps'], writes=[('rs0', p)])
        sc.op('dve', lambda e: e.reciprocal(out=rs[p][:, 1:2], in_=rs[p][:, 0:1]),
              reads=[('rs0', p)], writes=[('rs', p)])

    sc.op('dve', lambda e: e.memset(eps_t[:, 0:1], LN_EPS), writes=['eps'])
    sc.op('dve', lambda e: e.memset(eps_t[:, 1:2], GN_EPS), writes=['eps'])

    def ld_x(i):
        sc.dma('sp', xt[i % 4][:], x_d[i * 128:(i + 1) * 128, :], writes=[('xt', i % 4)])
    ld_x(0)
    ld_x(1)
    for i in range(NT):
        p = i % 2
        p4 = i % 4
        if i + 2 < NT:
            ld_x(i + 2)
        layernorm_stats(xt[p4], p, ('xt', p4))
        sc.op('dve', lambda e: e.tensor_scalar(out=xn[p][:], in0=xt[p4][:], scalar1=mv[p][:, 0:1], scalar2=rs[p][:, 1:2],
                                               op0=ALU.subtract, op1=ALU.mult),
              reads=[('xt', p4), ('mv', p), ('rs', p)], writes=[('xn', p)])
        sc.op('pool', lambda e: e.tensor_tensor(out=xn[p][:], in0=xn[p][:], in1=g0B[:], op=ALU.mult),
              reads=[('xn', p), 'g0B'], writes=[('xn', p)])
        sc.op('pool', lambda e: e.tensor_tensor(out=xn[p][:], in0=xn[p][:], in1=b0B[:], op=ALU.add),
              reads=[('xn', p), 'b0B'], writes=[('xn', p)])
        sc.dma('sp', x0_d[i * 128:(i + 1) * 128, :], xn[p][:], reads=[('xn', p)], writes=[('x0_d', i)])
        sc.op('act', lambda e: e.copy(out=xb[p][:], in_=xn[p][:]), reads=[('xn', p)], writes=[('xb', p)])
        b = next_bank()
        for kc in range(8):
            sc.op('pe', lambda e, kc=kc: e.transpose(out=bankbf(b)[:, kc * 128:(kc + 1) * 128],
                                                     in_=xb[p][:, kc * 128:(kc + 1) * 128], identity=identb[:]),
                  reads=[('xb', p), 'identb'], writes=[('bank', b)] if kc == 7 else [], ww=[('bank', b)], inc=(kc == 7))
        sc.op('dve', lambda e: e.tensor_copy(out=x0T[:, :, i * 128:(i + 1) * 128],
                                             in_=bankbf(b).rearrange("p (c t) -> p c t", c=8)),
              reads=[('bank', b)], writes=[('x0T', i)])

    if 'x0T' in dbg:
        d = dout("dbg_x0T", [128, 8, S], BF16)
        sc.dma('sp', d[:, :, :], x0T[:], reads=[('x0T', i) for i in range(NT)])

    if 'stage1_only' in dbg:
        for i in range(NT):
            p = i % 2
            sc.dma('sp', xt[p][:], x0_d[i * 128:(i + 1) * 128, :], reads=[('x0_d', i)], writes=[('xt', p)])
            sc.dma('sp', out_d[i * 128:(i + 1) * 128, :], xt[p][:], reads=[('xt', p)], writes=[('out', i)])
        sc.finish()
        return


    sc.barrier()
    ph1.close()
    y_aT = sb("y_aT", [128, 4, S], BF16)
    win_v = win_d.rearrange("(kc p) n -> p kc n", p=128)

    with contextlib.ExitStack() as ph:
        def sbp(name, shape, dt):
            return ph.enter_context(nc.sbuf_tensor(name, list(shape), dt))

        wqkv = sbp("wqkv", [128, 8, 1536], BF16)
        for g in range(3):
            sc.dma('pool', wqkv[:, :, g * 512:(g + 1) * 512], win_v[:, :, g * 512:(g + 1) * 512], writes=[('wqkv', g)])
        qT = sbp("qT", [72, 8, S], BF16)
        kT = sbp("kT", [72, 8, S], BF16)
        va = sbp("va", [128, NT, 8, 65], BF16)
        cosS = sbp("cosS", [128, NT, 32], F32)
        sinS = sbp("sinS", [128, NT, 32], F32)
        trib = sbp("trib", [128, 128], BF16)
        biasT = sbp("biasT", [64, S], BF16)
        sc.dma('sp', cosS[:], cos_d.rearrange("(i p) f -> p i f", p=128), writes=['cos'])
        sc.dma('sp', sinS[:], sin_d.rearrange("(i p) f -> p i f", p=128), writes=['sin'])
        sc.dma('sp', trib[:], tri_d[:, :], writes=['trib'])
        for h in range(8):
            sc.dma('sp', kT[64:72, h, :], blk_d[:, :], writes=[('kTaug', h)])
        sc.op('pool', lambda e: e.memset(va[:, :, :, 64:65], 1.0), writes=['va1'])
        tmp = [[sbp("rt%d_%d" % (a, b_), [128, 16, 32], F32) for b_ in range(4)] for a in range(2)]
        rot = [sbp("rot%d" % a, [128, 16, 64], BF16) for a in range(2)]

        for i in range(NT):
            p = i % 2
            for g in range(3):
                bk = (2 * p + g) if g < 2 else (4 + p)
                for kc in range(8):
                    sc.op('pe', lambda e, kc=kc: e.matmul(bank(bk), lhsT=x0T[:, kc, i * 128:(i + 1) * 128],
                                                         rhs=wqkv[:, kc, g * 512:(g + 1) * 512],
                                                         start=(kc == 0), stop=(kc == 7)),
                          reads=[('x0T', i), ('wqkv', g)], writes=[('bank', bk)] if kc == 7 else [], ww=[('bank', bk)], inc=(kc == 7))
            zz = ps[:, p * 1024:(p + 1) * 1024].rearrange("p (h d) -> p h d", d=64)
            zA, zB = zz[:, :, 0:32], zz[:, :, 32:64]
            cb = cosS[:, i, :].unsqueeze(1).broadcast_to([128, 16, 32])
            sb_ = sinS[:, i, :].unsqueeze(1).broadcast_to([128, 16, 32])
            bks = [('bank', 2 * p), ('bank', 2 * p + 1)]
            for t_i, (zin, tab, tk) in enumerate([(zA, cb, 'cos'), (zB, sb_, 'sin'), (zB, cb, 'cos'), (zA, sb_, 'sin')]):
                sc.op('dve', lambda e, zin=zin, tab=tab, t_i=t_i: e.tensor_tensor(out=tmp[p][t_i][:], in0=zin, in1=tab, op=ALU.mult),
                      reads=bks + [tk], writes=[('rt', p, t_i)])
            sc.op('pool', lambda e: e.tensor_tensor(out=rot[p][:, :, 0:32], in0=tmp[p][0][:], in1=tmp[p][1][:], op=ALU.subtract),
                  reads=[('rt', p, 0), ('rt', p, 1)], writes=[('rotA', p)])
            sc.op('pool', lambda e: e.tensor_tensor(out=rot[p][:, :, 32:64], in0=tmp[p][2][:], in1=tmp[p][3][:], op=ALU.add),
                  reads=[('rt', p, 2), ('rt', p, 3)], writes=[('rotB', p)])
            for qk in range(2):
                bt = 6 + qk
                for h in range(8):
                    sc.op('pe', lambda e, h=h: e.transpose(out=bankbf(bt)[0:64, h * 128:(h + 1) * 128],
                                                         in_=rot[p][:, qk * 8 + h, :], identity=identb[:]),
                          reads=[('rotA', p), ('rotB', p), 'identb'], writes=[('bank', bt)] if h == 7 else [], ww=[('bank', bt)], inc=(h == 7))
                src_v = bankbf(bt)[0:64, :].rearrange("p (h t) -> p h t", h=8)
                if qk == 0:
                    sc.op('act', lambda e: e.mul(out=qT[0:64, :, i * 128:(i + 1) * 128], in_=src_v, mul=0.125),
                          reads=[('bank', bt)], writes=[('qT', i)])
                else:
                    sc.op('act', lambda e: e.copy(out=kT[0:64, :, i * 128:(i + 1) * 128], in_=src_v),
                          reads=[('bank', bt)], writes=[('kT', i)])
            sc.op('act', lambda e: e.copy(out=va[:, i, :, 0:64], in_=bank(4 + p).rearrange("p (h d) -> p h d", d=64)),
                  reads=[('bank', 4 + p)], writes=[('va', i)])

        km32 = sbp("km32", [64, 8, 8], F32)
        kmb = sbp("kmb", [64, 8, 8], BF16)
        sc.op('dve', lambda e: e.tensor_reduce(out=km32[:], in_=kT[0:64, :, :].rearrange("p h (n k) -> p h n k", k=256),
                                               axis=AX.X, op=ALU.add),
              reads=[('kT', i) for i in range(NT)], writes=['km32'])
        sc.op('dve', lambda e: e.tensor_copy(out=kmb[:], in_=km32[:]), reads=['km32'], writes=['kmb'])
        G = [sbp("G%d" % a, [128, 8, 8], F32) for a in range(2)]
        cmpt = [sbp("cmp%d" % a, [128, 8, 8, 8], F32) for a in range(2)]
        rank = [sbp("rank%d" % a, [128, 8, 8], F32) for a in range(2)]
        biasb = [sbp("biasb%d" % a, [128, 8, 8], BF16) for a in range(2)]
        for i in range(NT):
            p = i % 2
            blk = i // 2
            bg = p
            for h in range(8):
                sc.op('pe', lambda e, h=h: e.matmul(bank(bg, 8, h * 8), lhsT=qT[0:64, h, i * 128:(i + 1) * 128], rhs=kmb[:, h, :],
                                                   start=True, stop=True),
                      reads=[('qT', i), 'kmb'], writes=[('bank', bg)] if h == 7 else [], ww=[('bank', bg)], inc=(h == 7))
            sc.op('act', lambda e: e.copy(out=G[p][:].rearrange("p h n -> p (h n)"), in_=bank(bg, 64)),
                  reads=[('bank', bg)], writes=[('G', p)])
            sc.op('pool', lambda e: e.memset(G[p][:, :, blk:8], -1e30), reads=[('G', p)], writes=[('G', p)])
            sc.op('dve', lambda e: e.tensor_tensor(out=cmpt[p][:], in0=G[p][:].unsqueeze(2).broadcast_to([128, 8, 8, 8]),
                                                   in1=G[p][:].unsqueeze(3).broadcast_to([128, 8, 8, 8]), op=ALU.is_gt),
                  reads=[('G', p)], writes=[('cmp', p)])
            sc.op('dve', lambda e: e.tensor_reduce(out=rank[p][:], in_=cmpt[p][:], axis=AX.X, op=ALU.add),
                  reads=[('cmp', p)], writes=[('rank', p)])
            sc.op('dve', lambda e: e.tensor_scalar(out=biasb[p][:], in0=rank[p][:], scalar1=3.0, scalar2=NEG,
                                                   op0=ALU.is_ge, op1=ALU.mult),
                  reads=[('rank', p)], writes=[('biasb', p)])
            sc.op('pool', lambda e: e.memset(biasb[p][:, :, blk:blk + 1], 0.0), reads=[('biasb', p)], writes=[('biasb', p)])
            bt = 2 + p
            sc.op('pe', lambda e: e.transpose(out=bankbf(bt)[0:64, 0:128], in_=biasb[p][:].rearrange("p h n -> p (h n)"),
                                              identity=identb[:]),
                  reads=[('biasb', p), 'identb'], writes=[('bank', bt)])
            sc.op('act', lambda e: e.copy(out=biasT[:, i * 128:(i + 1) * 128], in_=bankbf(bt)[0:64, 0:128]),
                  reads=[('bank', bt)], writes=[('biasT', i)])
        for h in range(8):
            sc.dma('sp', qT[64:72, h, :], biasT[h * 8:(h + 1) * 8, :], reads=[('biasT', i) for i in range(NT)],
                   writes=[('qTaug', h)])

        if 'qk' in dbg:
            dq = dout("dbg_qT", [72, 8, S], BF16)
            dk = dout("dbg_kT", [72, 8, S], BF16)
            sc.dma('sp', dq[:, :, :], qT[:], reads=[('qT', i) for i in range(NT)] + [('qTaug', h) for h in range(8)])
            sc.dma('sp', dk[:, :, :], kT[:], reads=[('kT', i) for i in range(NT)] + [('kTaug', h) for h in range(8)])

        PT = [sbp("PT%d" % a, [128, 4, 128], BF16) for a in range(3)]
        ya = [sbp("ya%d" % a, [128, 512], BF16) for a in range(2)]
        rden = [sbp("rden%d" % a, [128, 1], F32) for a in range(2)]
        groups = []
        for qt in range(NT):
            for h in range(8):
                for g0 in range(0, qt + 1, 4):
                    js = list(range(g0, min(g0 + 4, qt + 1)))
                    groups.append((qt, h, js))

        def emit_ST(k):
            qt, h, js = groups[k]
            sbk = k % 3
            for jj, j in enumerate(js):
                last = (jj == len(js) - 1)
                sc.op('pe', lambda e, jj=jj, j=j: e.matmul(bank(sbk, 128, jj * 128), lhsT=kT[0:72, h, j * 128:(j + 1) * 128],
                                                         rhs=qT[0:72, h, qt * 128:(qt + 1) * 128], start=True, stop=True),
                      reads=[('kT', j), ('kTaug', h), ('qT', qt), ('qTaug', h)],
                      writes=[('bank', sbk)] if last else [], ww=[('bank', sbk)], inc=last)

        def emit_rest(k):
            qt, h, js = groups[k]
            sbk = k % 3
            yp = qt % 2
            idx = qt * 8 + h
            ab = 3 + (idx % 2)
            ap_ = idx % 2
            n = len(js)
            sc.op('act', lambda e: e.activation(out=PT[sbk][:, 0:n, :].rearrange("p a b -> p (a b)"), in_=bank(sbk, n * 128), func=AF.Exp),
                  reads=[('bank', sbk)], writes=[('PT', sbk)])
            if js[-1] == qt:
                jj = n - 1
                sc.op('pool', lambda e: e.tensor_tensor(out=PT[sbk][:, jj, :], in0=PT[sbk][:, jj, :], in1=trib[:], op=ALU.mult),
                      reads=[('PT', sbk), 'trib'], writes=[('PT', sbk)])
            for jj, j in enumerate(js):
                last = (j == qt)
                sc.op('pe', lambda e, jj=jj, j=j: e.matmul(bank(ab, 65), lhsT=PT[sbk][:, jj, :], rhs=va[:, j, h, :],
                                                         start=(j == 0), stop=(j == qt)),
                      reads=[('PT', sbk), ('va', j), 'va1'], writes=[('bank', ab)] if last else [], ww=[('bank', ab)],
                      inc=(last or jj == n - 1))
            if js[-1] != qt:
                return
            sc.op('dve', lambda e: e.reciprocal(out=rden[ap_][:], in_=bank(ab, 1, 64)), reads=[('bank', ab)], writes=[('rden', ap_)])
            sc.op('dve', lambda e: e.tensor_scalar(out=ya[yp][:, h * 64:(h + 1) * 64], in0=bank(ab, 64), scalar1=rden[ap_][:, 0:1],
                                                   scalar2=None, op0=ALU.mult),
                  reads=[('bank', ab), ('rden', ap_)], writes=[('ya', yp, h)])
            if h != 7:
                return
            bt = 5
            for c in range(4):
                sc.op('pe', lambda e, c=c: e.transpose(out=bankbf(bt)[:, c * 128:(c + 1) * 128], in_=ya[yp][:, c * 128:(c + 1) * 128],
                                                     identity=identb[:]),
                      reads=[('ya', yp, hh_) for hh_ in range(8)] + ['identb'], writes=[('bank', bt)] if c == 3 else [], ww=[('bank', bt)],
                      inc=(c == 3))
            sc.op('act', lambda e: e.copy(out=y_aT[:, :, qt * 128:(qt + 1) * 128], in_=bankbf(bt)[:, 0:512].rearrange("p (c t) -> p c t", c=4)),
                  reads=[('bank', bt)], writes=[('y_aT', qt)])

        emit_ST(0)
        for k in range(len(groups)):
            if k + 1 < len(groups):
                emit_ST(k + 1)
            emit_rest(k)

    sc.barrier()
    if 'y_aT' in dbg:
        d = dout("dbg_y_aT", [128, 4, S], BF16)
        sc.dma('sp', d[:, :, :], y_aT[:], reads=[('y_aT', i) for i in range(NT)])

    y_mT = sb("y_mT", [128, 4, S], BF16)
    chanv_d = din("chanv", [128, 4, 7])
    wqk_d = din("wqk_m", [4, 128, 256])
    bif_d = din("b_if", [8])
    trif_d = din("tri_f32", [128, 128])
    onesf_d = din("ones_f32", [128, 128])
    with contextlib.ExitStack() as ph:
        def sbp(name, shape, dt):
            return ph.enter_context(nc.sbuf_tensor(name, list(shape), dt))

        chanv = sbp("chanv_s", [128, 4, 7], F32)
        wqk = sbp("wqk", [128, 4, 256], BF16)
        bifB = sbp("bifB", [128, 8], F32)
        trif = sbp("trif", [128, 128], F32)
        onesf = sbp("onesf", [128, 128], F32)
        trib = sbp("tribB", [128, 128], BF16)
        ones1 = sbp("ones1", [128, 1], F32)
        sc.dma('sp', chanv[:], chanv_d[:, :, :], writes=['chanv'])
        sc.dma('pool', wqk[:], wqk_d.rearrange("h p n -> p h n"), writes=['wqk'])
        sc.dma('sp', bifB[:], bif_d.partition_broadcast(128), writes=['bifB'])
        sc.dma('sp', trif[:], trif_d[:, :], writes=['trif'])
        sc.dma('sp', onesf[:], onesf_d[:, :], writes=['onesf'])
        sc.dma('sp', trib[:], tri_d[:, :], writes=['trib'])
        sc.op('dve', lambda e: e.memset(ones1[:], 1.0), writes=['ones1'])
        ucT = sbp("ucT", [128, 4, S], BF16)
        su = sbp("su", [128, 4, S], BF16)
        vm = sbp("vm", [128, NT, 4, 129], BF16)
        om = sbp("om", [128, NT, 512], BF16)
        pre = sbp("pre", [128, NT, 8], F32)
        sc.op('pool', lambda e: e.memset(vm[:, :, :, 128:129], 1.0), writes=['vm1'])

        with contextlib.ExitStack() as ph2:
            def sbq(name, shape, dt):
                return ph2.enter_context(nc.sbuf_tensor(name, list(shape), dt))
            wu = sbq("wu", [128, 8, 512], BF16)
            wvo = sbq("wvo", [128, 8, 1024], BF16)
            wif = sbq("wif", [128, 8, 8], BF16)
            sc.dma('pool', wu[:], win_v[:, :, 1536:2048], writes=['wu'])
            sc.dma('pool', wvo[:, :, 0:512], win_v[:, :, 2048:2560], writes=[('wvo', 0)])
            sc.dma('pool', wvo[:, :, 512:1024], win_v[:, :, 2560:3072], writes=[('wvo', 1)])
            sc.dma('pool', wif[:], win_v[:, :, 3072:3080], writes=['wif'])
            upad = [sbq("upad%d" % a, [128, S + 3], F32) for a in range(2)]
            cacc = [sbq("cacc%d" % a, [128, S], F32) for a in range(2)]
            for a in range(2):
                sc.op('pool', lambda e, a=a: e.memset(upad[a][:, 0:3], 0.0), writes=[('upadz', a)])
            for h in range(4):
                p = h % 2
                for tg in range(4):
                    b = next_bank()
                    for kc in range(8):
                        sc.op('pe', lambda e, kc=kc: e.matmul(bank(b), lhsT=wu[:, kc, h * 128:(h + 1) * 128],
                                                             rhs=x0T[:, kc, tg * 512:(tg + 1) * 512], start=(kc == 0), stop=(kc == 7)),
                              reads=['wu'] + [('x0T', tg * 4 + q) for q in range(4)], writes=[('bank', b)] if kc == 7 else [], ww=[('bank', b)], inc=(kc == 7))
                    sc.op('act', lambda e: e.copy(out=upad[p][:, 3 + tg * 512: 3 + (tg + 1) * 512], in_=bank(b)),
                          reads=[('bank', b)], writes=[('upad', p, tg)])
                ur = [('upad', p, tg) for tg in range(4)] + [('upadz', p)]
                sc.op('dve', lambda e: e.tensor_scalar(out=cacc[p][:], in0=upad[p][:, 0:S], scalar1=chanv[:, h, 0:1], scalar2=chanv[:, h, 4:5],
                                                       op0=ALU.mult, op1=ALU.add),
                      reads=ur + ['chanv'], writes=[('cacc', p)])
                for j in range(1, 4):
                    sc.op('dve', lambda e, j=j: e.scalar_tensor_tensor(out=cacc[p][:], in0=upad[p][:, j:S + j], scalar=chanv[:, h, j:j + 1],
                                                                       in1=cacc[p][:], op0=ALU.mult, op1=ALU.add),
                          reads=ur + ['chanv', ('cacc', p)], writes=[('cacc', p)])
                sc.op('act', lambda e: e.activation(out=ucT[:, h, :], in_=cacc[p][:], func=AF.Silu),
                      reads=[('cacc', p)], writes=[('ucT', h)])
                sc.op('pool', lambda e: e.tensor_scalar(out=su[:, h, :], in0=ucT[:, h, :], scalar1=chanv[:, h, 6:7], scalar2=None, op0=ALU.mult),
                      reads=[('ucT', h), 'chanv'], writes=[('su', h)])
            for i in range(NT):
                bs = []
                for g in range(3):
                    b = next_bank()
                    bs.append(b)
                    n = 512 if g < 2 else 8
                    for kc in range(8):
                        rhs = wvo[:, kc, g * 512:(g + 1) * 512] if g < 2 else wif[:, kc, :]
                        sc.op('pe', lambda e, kc=kc, rhs=rhs: e.matmul(bank(b, n), lhsT=x0T[:, kc, i * 128:(i + 1) * 128], rhs=rhs,
                                                                     start=(kc == 0), stop=(kc == 7)),
                              reads=[('x0T', i), ('wvo', g) if g < 2 else 'wif'], writes=[('bank', b)] if kc == 7 else [], ww=[('bank', b)], inc=(kc == 7))
                sc.op('act', lambda e: e.copy(out=vm[:, i, :, 0:128], in_=bank(bs[0]).rearrange("p (h d) -> p h d", d=128)),
                      reads=[('bank', bs[0])], writes=[('vm', i)])
                sc.op('act', lambda e: e.activation(out=om[:, i, :], in_=bank(bs[1]), func=AF.Sigmoid),
                      reads=[('bank', bs[1])], writes=[('om', i)])
                sc.op('dve', lambda e: e.tensor_tensor(out=pre[:, i, :], in0=bank(bs[2], 8), in1=bifB[:], op=ALU.add),
                      reads=[('bank', bs[2]), 'bifB'], writes=[('pre', i)])
        sc.barrier()
        if 'stopB1' in dbg:
            sc.finish()
            return

        Lall = sbp("Lall", [128, NT, 4], F32)
        eb = sbp("eb", [128, NT, 4], F32)
        e2 = sbp("e2", [128, NT, 4], F32)
        e3 = sbp("e3", [128, NT, 4], F32)
        dec = sbp("dec", [128, NT, 4], F32)
        a2 = sbp("a2", [128, NT, 4], F32)
        a3 = sbp("a3", [128, NT, 4], F32)
        prs = [('pre', i) for i in range(NT)]
        import os as _os
        _cut = int(_os.environ.get('GCUT', '99'))
        _real_op = sc.op
        _k = [0]

        def _cut_op(*a, **kw):
            _k[0] += 1
            if _k[0] <= _cut:
                return _real_op(*a, **kw)
            return None
        if 'stopB2' in dbg:
            sc.op = _cut_op
        sc.op('act', lambda e: e.activation(out=Lall[:], in_=pre[:, :, 4:8], func=AF.Exp, scale=-1.0), reads=prs, writes=['Lall'])
        sc.op('act', lambda e: e.activation(out=Lall[:], in_=Lall[:], func=AF.Ln, bias=ones1[:, 0:1], scale=1.0),
              reads=['Lall', 'ones1'], writes=['Lall'])
        bc, bg_ = next_bank(), next_bank()
        sc.op('pe', lambda e: e.matmul(bank(bc, 64), lhsT=trif[:], rhs=Lall[:].rearrange("p i h -> p (i h)"), start=True, stop=True),
              reads=['trif', 'Lall'], writes=[('bank', bc)])
        sc.op('pe', lambda e: e.matmul(bank(bg_, 64), lhsT=onesf[:], rhs=Lall[:].rearrange("p i h -> p (i h)"), start=True, stop=True),
              reads=['onesf', 'Lall'], writes=[('bank', bg_)])
        f64 = lambda t: t[:].rearrange("p i h -> p (i h)")
        sc.op('act', lambda e: e.activation(out=f64(eb), in_=bank(bc, 64), func=AF.Exp, scale=-1.0), reads=[('bank', bc)], writes=['eb'])
        sc.op('act', lambda e: e.activation(out=f64(dec), in_=bank(bg_, 64), func=AF.Exp, scale=-1.0), reads=[('bank', bg_)], writes=['dec'])
        sc.op('dve', lambda e: e.tensor_tensor(out=a2[:], in0=bank(bc, 64).rearrange("p (i h) -> p i h", h=4), in1=pre[:, :, 0:4], op=ALU.add),
              reads=[('bank', bc)] + prs, writes=['a2'])
        sc.op('dve', lambda e: e.tensor_tensor(out=f64(a3), in0=f64(a2), in1=bank(bg_, 64), op=ALU.subtract),
              reads=[('bank', bg_), 'a2'], writes=['a3'])
        sc.op('act', lambda e: e.activation(out=f64(e2), in_=f64(a2), func=AF.Exp), reads=['a2'], writes=['e2'])
        sc.op('act', lambda e: e.activation(out=f64(e3), in_=f64(a3), func=AF.Exp), reads=['a3'], writes=['e3'])
        KS = 128.0 ** -0.5
        sc.op('dve', lambda e: e.tensor_scalar(out=f64(e2), in0=f64(e2), scalar1=KS, scalar2=None, op0=ALU.mult), reads=['e2'], writes=['e2'])
        sc.op('dve', lambda e: e.tensor_scalar(out=f64(e3), in0=f64(e3), scalar1=KS, scalar2=None, op0=ALU.mult), reads=['e3'], writes=['e3'])

        if 'stopB2' in dbg:
            sc.op = _real_op
            sc.barrier()
            sc.finish()
            return
        S32 = sbp("S32", [128, 4, 129], F32)
        Sb = sbp("Sb", [128, 4, 129], BF16)
        sc.op('dve', lambda e: e.memset(S32[:], 0.0), writes=[('S32', h) for h in range(4)])
        sc.op('dve', lambda e: e.memset(Sb[:], 0.0), writes=[('Sb', h) for h in range(4)])
        NB = 8
        qs = [sbp("qs%d" % a, [128, 128], BF16) for a in range(NB)]
        k2 = [sbp("k2_%d" % a, [128, 128], BF16) for a in range(NB)]
        k3 = [sbp("k3_%d" % a, [128, 128], BF16) for a in range(NB)]
        qkT = [sbp("qkT%d" % a, [128, 2, 128], BF16) for a in range(NB)]
        PTm = [sbp("PTm%d" % a, [128, 128], BF16) for a in range(NB)]
        hh32 = [sbp("hh32_%d" % a, [128, 128], F32) for a in range(NB)]
        hn = [sbp("hn%d" % a, [128, 128], BF16) for a in range(NB)]
        gst = [sbp("gst%d" % a, [128, 6], F32) for a in range(NB)]
        gmv = [sbp("gmv%d" % a, [128, 2], F32) for a in range(NB)]
        grs = [sbp("grs%d" % a, [128, 4], F32) for a in range(NB)]
        HS = range(4)

        def partA(i):
            P = [(i % 2) * 4 + h for h in HS]
            bA = [next_bank() for h in HS]
            for h in HS:
                sc.op('pe', lambda e: e.matmul(bank(bA[h], 256), lhsT=ucT[:, h, i * 128:(i + 1) * 128], rhs=wqk[:, h, :], start=True, stop=True),
                      reads=[('ucT', h), 'wqk'], writes=[('bank', bA[h])])
            for h in HS:
                p = P[h]
                sc.op('act', lambda e: e.activation(out=qs[p][:], in_=bank(bA[h], 128, 0), func=AF.Copy, scale=eb[:, i, h:h + 1]),
                      reads=[('bank', bA[h]), 'eb'], writes=[('qs', p)])
                sc.op('act', lambda e: e.activation(out=k2[p][:], in_=bank(bA[h], 128, 128), func=AF.Copy, scale=e2[:, i, h:h + 1]),
                      reads=[('bank', bA[h]), 'e2'], writes=[('k2', p)])
                sc.op('dve', lambda e: e.tensor_scalar(out=k3[p][:], in0=bank(bA[h], 128, 128), scalar1=e3[:, i, h:h + 1], scalar2=None, op0=ALU.mult),
                      reads=[('bank', bA[h]), 'e3'], writes=[('k3', p)])
            bC = [next_bank() for h in HS]
            for h in HS:
                p = P[h]
                sc.op('pe', lambda e: e.transpose(out=bankbf(bC[h])[:, 0:128], in_=qs[p][:], identity=identb[:]),
                      reads=[('qs', p), 'identb'], inc=False, ww=[('bank', bC[h])])
                sc.op('pe', lambda e: e.transpose(out=bankbf(bC[h])[:, 128:256], in_=k2[p][:], identity=identb[:]),
                      reads=[('k2', p), 'identb'], writes=[('bank', bC[h])])
            for h in HS:
                p = P[h]
                sc.op('dve' if h % 2 == 0 else 'act',
                      (lambda e: e.tensor_copy(out=qkT[p][:].rearrange("p a t -> p (a t)"), in_=bankbf(bC[h])[:, 0:256])) if h % 2 == 0 else
                      (lambda e: e.copy(out=qkT[p][:].rearrange("p a t -> p (a t)"), in_=bankbf(bC[h])[:, 0:256])),
                      reads=[('bank', bC[h])], writes=[('qkT', p)])
            bD = [next_bank() for h in HS]
            for h in HS:
                p = P[h]
                sc.op('pe', lambda e: e.matmul(bank(bD[h], 128), lhsT=qkT[p][:, 1, :], rhs=qkT[p][:, 0, :], start=True, stop=True),
                      reads=[('qkT', p)], writes=[('bank', bD[h])])
            for h in HS:
                p = P[h]
                sc.op('dve', lambda e: e.tensor_tensor(out=PTm[p][:], in0=bank(bD[h], 128), in1=trib[:], op=ALU.mult),
                      reads=[('bank', bD[h]), 'trib'], writes=[('PTm', p)])

        def partB(i):
            P = [(i % 2) * 4 + h for h in HS]
            bF = [next_bank() for h in HS]
            for h in HS:
                p = P[h]
                sc.op('pe', lambda e: e.matmul(bank(bF[h], 129), lhsT=qkT[p][:, 0, :], rhs=Sb[:, h, :], start=True, stop=False),
                      reads=[('qkT', p), ('Sb', h)], inc=False, ww=[('bank', bF[h])])
                sc.op('pe', lambda e: e.matmul(bank(bF[h], 129), lhsT=PTm[p][:], rhs=vm[:, i, h, :], start=False, stop=True),
                      reads=[('PTm', p), ('vm', i), 'vm1'], writes=[('bank', bF[h])])
            bJ = [next_bank() for h in HS]
            for h in HS:
                p = P[h]
                sc.op('pe', lambda e: e.matmul(bank(bJ[h], 129), lhsT=k3[p][:], rhs=vm[:, i, h, :], start=True, stop=True),
                      reads=[('k3', p), ('vm', i), 'vm1'], writes=[('bank', bJ[h])])
            for h in HS:
                sc.op('dve', lambda e: e.scalar_tensor_tensor(out=S32[:, h, :], in0=S32[:, h, :], scalar=dec[:, i, h:h + 1],
                                                              in1=bank(bJ[h], 129), op0=ALU.mult, op1=ALU.add),
                      reads=[('S32', h), 'dec', ('bank', bJ[h])], writes=[('S32', h)])
            for h in HS:
                sc.op('act', lambda e: e.copy(out=Sb[:, h, :], in_=S32[:, h, :]), reads=[('S32', h)], writes=[('Sb', h)])
            for h in HS:
                p = P[h]
                sc.op('act', lambda e: e.activation(out=grs[p][:, 0:1], in_=bank(bF[h], 1, 128), func=AF.Abs),
                      reads=[('bank', bF[h])], writes=[('grs0', p)])
            for h in HS:
                p = P[h]
                sc.op('dve', lambda e: e.tensor_scalar(out=grs[p][:, 0:1], in0=grs[p][:, 0:1], scalar1=1.0, scalar2=None, op0=ALU.max),
                      reads=[('grs0', p)], writes=[('grs0', p)])
            for h in HS:
                p = P[h]
                sc.op('dve', lambda e: e.reciprocal(out=grs[p][:, 1:2], in_=grs[p][:, 0:1]), reads=[('grs0', p)], writes=[('grs1', p)])
            for h in HS:
                p = P[h]
                sc.op('dve', lambda e: e.scalar_tensor_tensor(out=hh32[p][:], in0=bank(bF[h], 128), scalar=grs[p][:, 1:2],
                                                              in1=om[:, i, h * 128:(h + 1) * 128], op0=ALU.mult, op1=ALU.mult),
                      reads=[('bank', bF[h]), ('grs1', p), ('om', i)], writes=[('hh32', p)])
            for h in HS:
                p = P[h]
                sc.op('dve', lambda e: e.bn_stats(out=gst[p][:], in_=hh32[p][:]), reads=[('hh32', p)], writes=[('gst', p)])
            for h in HS:
                p = P[h]
                sc.op('dve', lambda e: e.bn_aggr(out=gmv[p][:], in_=gst[p][:]), reads=[('gst', p)], writes=[('gmv', p)])
            for h in HS:
                p = P[h]
                sc.op('act', lambda e: e.activation(out=grs[p][:, 2:3], in_=gmv[p][:, 1:2], func=AF.Sqrt, bias=eps_t[:, 1:2], scale=1.0),
                      reads=[('gmv', p), 'eps'], writes=[('grs2', p)])
            for h in HS:
                p = P[h]
                sc.op('dve', lambda e: e.reciprocal(out=grs[p][:, 3:4], in_=grs[p][:, 2:3]), reads=[('grs2', p)], writes=[('grs3', p)])
            for h in HS:
                p = P[h]
                sc.op('dve', lambda e: e.tensor_scalar(out=hn[p][:], in0=hh32[p][:], scalar1=gmv[p][:, 0:1], scalar2=grs[p][:, 3:4],
                                                       op0=ALU.subtract, op1=ALU.mult),
                      reads=[('hh32', p), ('gmv', p), ('grs3', p)], writes=[('hn', p)])
            bI = [next_bank() for h in HS]
            for h in HS:
                p = P[h]
                sc.op('pe', lambda e: e.transpose(out=bankbf(bI[h])[:, 0:128], in_=hn[p][:], identity=identb[:]),
                      reads=[('hn', p), 'identb'], writes=[('bank', bI[h])])
            for h in HS:
                sc.op('dve', lambda e: e.scalar_tensor_tensor(out=y_mT[:, h, i * 128:(i + 1) * 128], in0=bankbf(bI[h])[:, 0:128],
                                                              scalar=chanv[:, h, 5:6], in1=su[:, h, i * 128:(i + 1) * 128],
                                                              op0=ALU.mult, op1=ALU.add),
                      reads=[('bank', bI[h]), 'chanv', ('su', h)], writes=[('y_mT', i, h)])

        partA(0)
        for i in range(NT):
            if i + 1 < NT:
                partA(i + 1)
            partB(i)

    sc.barrier()
    if 'y_mT' in dbg:
        d = dout("dbg_y_mT", [128, 4, S], BF16)
        sc.dma('sp', d[:, :, :], y_mT[:], reads=[('y_mT', i, h) for i in range(NT) for h in range(4)])

    CAP = 128
    YROWS = 2 * S + 128
    x1_d = nc.dram_tensor("x1_scr", [S, D], F32, kind="Internal").ap()
    tab_d = nc.dram_tensor("tab_scr", [64 * CAP, 4], F32, kind="Internal").ap()
    ybuf_d = nc.dram_tensor("ybuf_scr", [YROWS, D], F32, kind="Internal").ap()
    wau_d = din("w_attn_up", [512, D])
    wmu_d = din("w_mlstm_up", [512, D])
    wout_d = din("w_out", [D, D])
    ln1g_d = din("ln1_g", [D])
    ln1b_d = din("ln1_b", [D])
    wr_d = din("w_router", [D, 72])
    br_d = din("b_router", [72])
    identf_d = din("ident_f32", [128, 128])
    stri_d = din("stri_bf", [128, 128], BF16)
    onesb_d = din("ones_bf", [128, 128], BF16)
    iota64_d = din("iota64", [128, 64])
    tokid_d = din("tokid", [128, NT])
    tabinit_d = din("tab_init", [64 * CAP, 4])
    sc.dma('sp', tab_d[:, :], tabinit_d[:, :], writes=['tab_d'])

    with contextlib.ExitStack() as ph:
        def sbp(name, shape, dt):
            return ph.enter_context(nc.sbuf_tensor(name, list(shape), dt))
        mixT = sbp("mixT", [128, 8, S], BF16)
        phc = contextlib.ExitStack()
        _sbp_outer = sbp

        def sbp(name, shape, dt):
            return phc.enter_context(nc.sbuf_tensor(name, list(shape), dt))
        wg = sbp("wg", [128, 8, 2048], BF16)
        wau = sbp("wau", [128, 4, D], BF16)
        wmu = sbp("wmu", [128, 4, D], BF16)
        for g in range(4):
            sc.dma('pool', wg[:, :, g * 512:(g + 1) * 512], win_v[:, :, 3080 + g * 512: 3080 + (g + 1) * 512], writes=[('wg', g)])
        for g in range(2):
            sc.dma('pool', wau[:, :, g * 512:(g + 1) * 512], wau_d.rearrange("(c p) n -> p c n", p=128)[:, :, g * 512:(g + 1) * 512], writes=[('wau', g)])
            sc.dma('pool', wmu[:, :, g * 512:(g + 1) * 512], wmu_d.rearrange("(c p) n -> p c n", p=128)[:, :, g * 512:(g + 1) * 512], writes=[('wmu', g)])
        sg = [[sbp("sg%d_%d" % (a, b_), [128, 512], F32) for b_ in range(2)] for a in range(2)]
        it = 0
        for fc in range(8):
            for tg in range(4):
                p = it % 2
                it += 1
                bA, bB, bC, bD = 4 * p, 4 * p + 1, 4 * p + 2, 4 * p + 3
                xr = [('x0T', tg * 4 + q) for q in range(4)]
                for kc in range(8):
                    sc.op('pe', lambda e, kc=kc: e.matmul(bank(bA), lhsT=wg[:, kc, fc * 128:(fc + 1) * 128], rhs=x0T[:, kc, tg * 512:(tg + 1) * 512],
                                                         start=(kc == 0), stop=(kc == 7)),
                          reads=xr + [('wg', fc // 4)], writes=[('bank', bA)] if kc == 7 else [], ww=[('bank', bA)], inc=(kc == 7))
                for kc in range(8):
                    sc.op('pe', lambda e, kc=kc: e.matmul(bank(bB), lhsT=wg[:, kc, 1024 + fc * 128: 1024 + (fc + 1) * 128],
                                                         rhs=x0T[:, kc, tg * 512:(tg + 1) * 512], start=(kc == 0), stop=(kc == 7)),
                          reads=xr + [('wg', 2 + fc // 4)], writes=[('bank', bB)] if kc == 7 else [], ww=[('bank', bB)], inc=(kc == 7))
                for c in range(4):
                    sc.op('pe', lambda e, c=c: e.matmul(bank(bC), lhsT=wau[:, c, fc * 128:(fc + 1) * 128], rhs=y_aT[:, c, tg * 512:(tg + 1) * 512],
                                                       start=(c == 0), stop=(c == 3)),
                          reads=[('y_aT', tg * 4 + q) for q in range(4)] + [('wau', fc // 4)], writes=[('bank', bC)] if c == 3 else [],
                          ww=[('bank', bC)], inc=(c == 3))
                for c in range(4):
                    sc.op('pe', lambda e, c=c: e.matmul(bank(bD), lhsT=wmu[:, c, fc * 128:(fc + 1) * 128], rhs=y_mT[:, c, tg * 512:(tg + 1) * 512],
                                                       start=(c == 0), stop=(c == 3)),
                          reads=[('y_mT', tg * 4 + q, h) for q in range(4) for h in range(4)] + [('wmu', fc // 4)],
                          writes=[('bank', bD)] if c == 3 else [], ww=[('bank', bD)], inc=(c == 3))
                sc.op('act', lambda e: e.activation(out=sg[p][0][:], in_=bank(bA), func=AF.Sigmoid), reads=[('bank', bA)], writes=[('sg', p, 0)])
                sc.op('act', lambda e: e.activation(out=sg[p][1][:], in_=bank(bB), func=AF.Sigmoid), reads=[('bank', bB)], writes=[('sg', p, 1)])
                sc.op('dve', lambda e: e.tensor_tensor(out=sg[p][0][:], in0=sg[p][0][:], in1=bank(bC), op=ALU.mult),
                      reads=[('sg', p, 0), ('bank', bC)], writes=[('sg', p, 0)])
                sc.op('dve', lambda e: e.tensor_tensor(out=sg[p][1][:], in0=sg[p][1][:], in1=bank(bD), op=ALU.mult),
                      reads=[('sg', p, 1), ('bank', bD)], writes=[('sg', p, 1)])
                sc.op('pool', lambda e: e.tensor_tensor(out=mixT[:, fc, tg * 512:(tg + 1) * 512], in0=sg[p][0][:], in1=sg[p][1][:], op=ALU.add),
                      reads=[('sg', p, 0), ('sg', p, 1)], writes=[('mixT', fc, tg)])
        sc.barrier()
        phc.close()
        sbp = _sbp_outer

        wout = sbp("wout", [128, 8, D], BF16)
        for g in range(2):
            sc.dma('pool', wout[:, :, g * 512:(g + 1) * 512], wout_d.rearrange("(c p) n -> p c n", p=128)[:, :, g * 512:(g + 1) * 512], writes=[('wout', g)])
        g1B = sbp("g1B", [128, D], F32)
        b1B = sbp("b1B", [128, D], F32)
        wr = sbp("wr", [128, 8, 72], F32)
        brB = sbp("brB", [128, 72], F32)
        identf = sbp("identf", [128, 128], F32)
        strib = sbp("strib", [128, 128], BF16)
        onesb = sbp("onesb", [128, 128], BF16)
        iota64 = sbp("iota64s", [128, 64], F32)
        tokid = sbp("tokids", [128, NT], F32)
        sc.dma('sp', g1B[:], ln1g_d.partition_broadcast(128), writes=['g1B'])
        sc.dma('sp', b1B[:], ln1b_d.partition_broadcast(128), writes=['b1B'])
        sc.dma('sp', wr[:], wr_d.rearrange("(c p) n -> p c n", p=128), writes=['wr'])
        sc.dma('sp', brB[:], br_d.partition_broadcast(128), writes=['brB'])
        sc.dma('sp', identf[:], identf_d[:, :], writes=['identf'])
        sc.dma('sp', strib[:], stri_d[:, :], writes=['strib'])
        sc.dma('sp', onesb[:], onesb_d[:, :], writes=['onesb'])
        sc.dma('sp', iota64[:], iota64_d[:, :], writes=['iota64'])
        sc.dma('sp', tokid[:], tokid_d[:, :], writes=['tokid'])
        x0t = [sbp("x0t%d" % a, [128, D], F32) for a in range(4)]
        xp = [sbp("xp%d" % a, [128, D], F32) for a in range(2)]
        x1T = [sbp("x1T%d" % a, [128, 8, 128], F32) for a in range(2)]
        run = sbp("run", [128, 64], F32)
        sc.op('dve', lambda e: e.memset(run[:], 0.0), writes=['run'])

        lgA = sbp("lgA", [128, NT, 72], F32)
        mx8A = sbp("mx8A", [128, NT, 8], F32)
        ohgA = sbp("ohgA", [128, NT, 8], F32)
        exgA = sbp("exgA", [128, NT, 8], F32)
        scA = sbp("scA", [128, 6, NT], F32)
        tmpA = sbp("tmpA", [128, NT, 64], F32)
        elA = sbp("elA", [128, NT, 8], F32)
        mxeA = sbp("mxeA", [128, NT, 8], F32)
        ohA = [sbp("ohA%d" % c, [128, NT, 8], F32) for c in range(2)]
        AcA = [sbp("AcA%d" % c, [128, NT, 64], F32) for c in range(2)]
        AbA = sbp("AbA", [128, NT, 64], BF16)
        prodA = sbp("prodA", [128, NT, 64], F32)
        ridfA = sbp("ridfA", [128, 2, NT], F32)
        ridxA = [sbp("ridxA%d" % c, [128, NT], I32) for c in range(2)]
        rowA = [sbp("rowA%d" % c, [128, NT, 4], F32) for c in range(2)]

        def ld_x0(i):
            sc.dma('sp', x0t[i % 4][:], x0_d[i * 128:(i + 1) * 128, :], reads=[('x0_d', i)], writes=[('x0t', i % 4)])
        ld_x0(0)
        ld_x0(1)
        for i in range(NT):
            p = i % 2
            p4 = i % 4
            if i + 2 < NT:
                ld_x0(i + 2)
            bp = 2 * p
            for half in range(2):
                for kc in range(8):
                    sc.op('pe', lambda e, kc=kc: e.matmul(bank(bp + half), lhsT=mixT[:, kc, i * 128:(i + 1) * 128],
                                                         rhs=wout[:, kc, half * 512:(half + 1) * 512], start=(kc == 0), stop=(kc == 7)),
                          reads=[('mixT', kc, i // 4), ('wout', half)], writes=[('bank', bp + half)] if kc == 7 else [],
                          ww=[('bank', bp + half)], inc=(kc == 7))
            sc.op('dve', lambda e: e.scalar_tensor_tensor(out=xp[p][:], in0=x0t[p4][:], scalar=ALPHA, in1=ps[:, bp * 512:(bp + 2) * 512],
                                                          op0=ALU.mult, op1=ALU.add),
                  reads=[('x0t', p4), ('bank', bp), ('bank', bp + 1)], writes=[('xp', p)])
            layernorm_stats(xp[p], p, ('xp', p))
            sc.op('dve', lambda e: e.tensor_scalar(out=xp[p][:], in0=xp[p][:], scalar1=mv[p][:, 0:1], scalar2=rs[p][:, 1:2],
                                                   op0=ALU.subtract, op1=ALU.mult),
                  reads=[('xp', p), ('mv', p), ('rs', p)], writes=[('xp', p)])
            sc.op('pool', lambda e: e.tensor_tensor(out=xp[p][:], in0=xp[p][:], in1=g1B[:], op=ALU.mult),
                  reads=[('xp', p), 'g1B'], writes=[('xp', p)])
            sc.op('pool', lambda e: e.tensor_tensor(out=xp[p][:], in0=xp[p][:], in1=b1B[:], op=ALU.add),
                  reads=[('xp', p), 'b1B'], writes=[('xp', p)])
            sc.dma('sp', x1_d[i * 128:(i + 1) * 128, :], xp[p][:], reads=[('xp', p)], writes=[('x1_d', i)])
            bt = 4
            for kc in range(8):
                sc.op('pe', lambda e, kc=kc: e.transpose(out=ps[:, bt * 512 + kc * 128: bt * 512 + (kc + 1) * 128],
                                                         in_=xp[p][:, kc * 128:(kc + 1) * 128], identity=identf[:]),
                      reads=[('xp', p), 'identf'], writes=[('bank', bt), ('bank', bt + 1)] if kc == 7 else [],
                      ww=[('bank', bt), ('bank', bt + 1)], inc=(kc == 7))
            sc.op('act', lambda e: e.copy(out=x1T[p][:].rearrange("p c t -> p (c t)"), in_=ps[:, bt * 512:(bt + 2) * 512]),
                  reads=[('bank', bt), ('bank', bt + 1)], writes=[('x1T', p)])
            bl = 6
            for kc in range(8):
                sc.op('pe', lambda e, kc=kc: e.matmul(bank(bl, 72), lhsT=x1T[p][:, kc, :], rhs=wr[:, kc, :], start=(kc == 0), stop=(kc == 7)),
                      reads=[('x1T', p), 'wr'], writes=[('bank', bl)] if kc == 7 else [], ww=[('bank', bl)], inc=(kc == 7))
            sc.op('dve', lambda e: e.tensor_tensor(out=lgA[:, i, :], in0=bank(bl, 72), in1=brB[:], op=ALU.add),
                  reads=[('bank', bl), 'brB'], writes=[('lgA', i)])
            sc.op('dve', lambda e: e.max(out=mx8A[:, i, :], in_=lgA[:, i, 0:8]), reads=[('lgA', i)], writes=[('mx8A', i)])

        T = NT
        lgs = [('lgA', i) for i in range(NT)]
        mxs = [('mx8A', i) for i in range(NT)]
        sc.op('dve', lambda e: e.tensor_tensor(out=ohgA[:], in0=lgA[:, :, 0:8], in1=mx8A[:, :, 0:1].broadcast_to([128, T, 8]), op=ALU.is_equal),
              reads=lgs + mxs, writes=['ohgA'])
        sc.op('dve', lambda e: e.tensor_tensor(out=exgA[:], in0=lgA[:, :, 0:8], in1=mx8A[:, :, 0:1].broadcast_to([128, T, 8]), op=ALU.subtract),
              reads=lgs + mxs, writes=['exgA'])
        sc.op('act', lambda e: e.activation(out=exgA[:], in_=exgA[:], func=AF.Exp), reads=['exgA'], writes=['exgA'])
        sc.op('dve', lambda e: e.tensor_reduce(out=scA[:, 0, :], in_=exgA[:], axis=AX.X, op=ALU.add), reads=['exgA'], writes=['ssum'])
        sc.op('dve', lambda e: e.reciprocal(out=scA[:, 1, :], in_=scA[:, 0, :]), reads=['ssum'], writes=['gw'])
        sc.op('dve', lambda e: e.tensor_tensor(out=tmpA[:].rearrange("p t (g e) -> p t g e", e=8),
                                               in0=lgA[:, :, 8:72].rearrange("p t (g e) -> p t g e", e=8),
                                               in1=ohgA[:].unsqueeze(3).broadcast_to([128, T, 8, 8]), op=ALU.mult),
              reads=lgs + ['ohgA'], writes=['tmpA'])
        sc.op('dve', lambda e: e.tensor_reduce(out=elA[:], in_=tmpA[:].rearrange("p t (g e) -> p t e g", e=8), axis=AX.X, op=ALU.add),
              reads=['tmpA'], writes=['elA'])
        for i in range(NT):
            sc.op('dve', lambda e: e.max(out=mxeA[:, i, :], in_=elA[:, i, :]), reads=['elA'], writes=[('mxeA', i)])
        mes = [('mxeA', i) for i in range(NT)]
        for c in range(2):
            sc.op('dve', lambda e, c=c: e.tensor_tensor(out=ohA[c][:], in0=elA[:], in1=mxeA[:, :, c:c + 1].broadcast_to([128, T, 8]), op=ALU.is_equal),
                  reads=['elA'] + mes, writes=[('ohA', c)])
        sc.op('dve', lambda e: e.tensor_tensor(out=scA[:, 2, :], in0=mxeA[:, :, 1], in1=mxeA[:, :, 0], op=ALU.subtract), reads=mes, writes=['dv'])
        sc.op('act', lambda e: e.activation(out=scA[:, 2, :], in_=scA[:, 2, :], func=AF.Exp), reads=['dv'], writes=['dv'])
        sc.op('dve', lambda e: e.tensor_scalar(out=scA[:, 3, :], in0=scA[:, 2, :], scalar1=1.0, scalar2=None, op0=ALU.add), reads=['dv'], writes=['dv1'])
        sc.op('dve', lambda e: e.reciprocal(out=scA[:, 4, :], in_=scA[:, 3, :]), reads=['dv1'], writes=['p1'])
        sc.op('dve', lambda e: e.tensor_scalar(out=scA[:, 5, :], in0=scA[:, 4, :], scalar1=-1.0, scalar2=1.0, op0=ALU.mult, op1=ALU.add),
              reads=['p1'], writes=['p2'])
        for c in range(2):
            sc.op('dve', lambda e, c=c: e.tensor_tensor(out=AcA[c][:].rearrange("p t (g e) -> p t g e", e=8),
                                                        in0=ohgA[:].unsqueeze(3).broadcast_to([128, T, 8, 8]),
                                                        in1=ohA[c][:].unsqueeze(2).broadcast_to([128, T, 8, 8]), op=ALU.mult),
                  reads=['ohgA', ('ohA', c)], writes=[('AcA', c)])
        sc.op('dve', lambda e: e.tensor_tensor(out=AbA[:], in0=AcA[0][:], in1=AcA[1][:], op=ALU.add),
              reads=[('AcA', 0), ('AcA', 1)], writes=['AbA'])
        for i in range(NT):
            bq = i // 8
            col = (i % 8) * 64
            for j in range(i + 1):
                lastm = (j == i)
                lhs = strib if j == 0 else onesb
                src_t = i if j == 0 else j - 1
                sc.op('pe', lambda e, lhs=lhs, src_t=src_t: e.matmul(bank(bq, 64, col), lhsT=lhs[:], rhs=AbA[:, src_t, :], start=(j == 0), stop=lastm),
                      reads=['strib', 'onesb', 'AbA'], writes=[('bank', bq)] if lastm else [], ww=[('bank', bq)], inc=lastm)
        posv = ps[:, 0:1024].rearrange("p (t e) -> p t e", e=64)
        for c in range(2):
            sc.op('dve', lambda e, c=c: e.tensor_tensor(out=prodA[:], in0=AcA[c][:], in1=posv, op=ALU.mult),
                  reads=[('AcA', c), ('bank', 0), ('bank', 1)], writes=['prodA'])
            sc.op('dve', lambda e: e.tensor_reduce(out=ridfA[:, 0, :], in_=prodA[:], axis=AX.X, op=ALU.add), reads=['prodA'], writes=['rid0'])
            sc.op('dve', lambda e, c=c: e.tensor_tensor(out=prodA[:], in0=AcA[c][:], in1=iota64[:].unsqueeze(1).broadcast_to([128, T, 64]), op=ALU.mult),
                  reads=[('AcA', c), 'iota64', 'rid0'], writes=['prodA'])
            sc.op('dve', lambda e: e.tensor_reduce(out=ridfA[:, 1, :], in_=prodA[:], axis=AX.X, op=ALU.add), reads=['prodA'], writes=['rid1'])
            sc.op('dve', lambda e: e.scalar_tensor_tensor(out=ridfA[:, 0, :], in0=ridfA[:, 1, :], scalar=float(CAP), in1=ridfA[:, 0, :],
                                                          op0=ALU.mult, op1=ALU.add),
                  reads=['rid0', 'rid1'], writes=['rid0'])
            sc.op('dve', lambda e, c=c: e.tensor_copy(out=ridxA[c][:], in_=ridfA[:, 0, :]), reads=['rid0'], writes=[('ridxA', c)])
            sc.op('dve', lambda e, c=c: e.tensor_copy(out=rowA[c][:, :, 0], in_=tokid[:]), reads=['tokid'], writes=[('rowA', c)])
            sc.op('dve', lambda e, c=c: e.tensor_scalar(out=rowA[c][:, :, 1], in0=tokid[:], scalar1=float(c * S), scalar2=None, op0=ALU.add),
                  reads=['tokid', ('rowA', c)], writes=[('rowA', c)])
            sc.op('dve', lambda e, c=c: e.tensor_tensor(out=rowA[c][:, :, 2], in0=scA[:, 1, :], in1=scA[:, 4 + c, :], op=ALU.mult),
                  reads=['gw', 'p1', 'p2', ('rowA', c)], writes=[('rowA', c)])
            sc.op('dve', lambda e, c=c: e.memset(rowA[c][:, :, 3], 0.0), reads=[('rowA', c)], writes=[('rowA', c)])
            for i in range(NT):
                sc.dma('pool', tab_d[:, :], rowA[c][:, i, :], reads=[('rowA', c), ('ridxA', c), 'tab_d'], writes=[('tabw', i, c)],
                       indirect=dict(out_offset=bass.IndirectOffsetOnAxis(ap=ridxA[c][:, i:i + 1], axis=0), in_offset=None))
    sc.barrier()
    if 'x1' in dbg:
        d = dout("dbg_x1", [S, D])
        sc.dma('sp', d[:, :], x1_d[:, :], reads=[('x1_d', i) for i in range(NT)])
        d2 = dout("dbg_tab", [64 * CAP, 4])
        sc.dma('sp', d2[:, :], tab_d[:, :], reads=[('tabw', i, c) for i in range(NT) for c in range(2)])
    if 'stopC' in dbg:
        sc.barrier()
        sc.finish()
        return

    wgate_d = din("w_gate", [64, D, 512])
    wup_d = din("w_up", [64, D, 512])
    wdown_d = din("w_down", [64, 512, D])
    ln2g_d = din("ln2_g", [D])
    ln2b_d = din("ln2_b", [D])
    with contextlib.ExitStack() as ph:
        def sbp(name, shape, dt):
            return ph.enter_context(nc.sbuf_tensor(name, list(shape), dt))
        tabS = sbp("tabS", [128, 64, 4], F32)
        gidx = sbp("gidx", [128, 64], I32)
        sidx = sbp("sidx", [128, 64], I32)
        tabw = [('tabw', i, c) for i in range(NT) for c in range(2)]
        tab_v = tab_d.rearrange("(e s) c -> s e c", s=CAP)
        for q in range(4):
            sc.dma('sp', tabS[:, q * 16:(q + 1) * 16, :], tab_v[:, q * 16:(q + 1) * 16, :], reads=tabw, writes=[('tabS', q)])
        tq = [('tabS', q) for q in range(4)]
        sc.op('dve', lambda e: e.tensor_copy(out=gidx[:], in_=tabS[:, :, 0]), reads=tq, writes=['gidx'])
        sc.op('dve', lambda e: e.tensor_copy(out=sidx[:], in_=tabS[:, :, 1]), reads=tq, writes=['sidx'])
        wge = [sbp("wge%d" % a, [128, 8, 512], BF16) for a in range(2)]
        wue = [sbp("wue%d" % a, [128, 8, 512], BF16) for a in range(2)]
        wde = [sbp("wde%d" % a, [128, 4, D], BF16) for a in range(2)]
        xg = [sbp("xg%d" % a, [128, D], BF16) for a in range(2)]
        xgT = [sbp("xgT%d" % a, [128, 8, 128], BF16) for a in range(2)]
        sil = [sbp("sil%d" % a, [128, 512], F32) for a in range(2)]
        hT = [sbp("hT%d" % a, [128, 4, 128], BF16) for a in range(2)]
        yw = [sbp("yw%d" % a, [128, D], F32) for a in range(2)]

        def load_expert(ex):
            p = ex % 2
            sc.dma('pool', xg[p][:], x1_d[:, :], reads=[('x1_d', i) for i in range(NT)] + ['gidx'], writes=[('xg', p)],
                   indirect=dict(out_offset=None, in_offset=bass.IndirectOffsetOnAxis(ap=gidx[:, ex:ex + 1], axis=0)))
            sc.dma('pool', wge[p][:], wgate_d[ex].rearrange("(c p) n -> p c n", p=128), writes=[('wge', p)])
            sc.dma('pool', wue[p][:], wup_d[ex].rearrange("(c p) n -> p c n", p=128), writes=[('wue', p)])
            for half in range(2):
                sc.dma('pool', wde[p][:, :, half * 512:(half + 1) * 512],
                       wdown_d[ex].rearrange("(c p) n -> p c n", p=128)[:, :, half * 512:(half + 1) * 512], writes=[('wde', p, half)])

        load_expert(0)
        for ex in range(64):
            p = ex % 2
            if ex + 1 < 64:
                load_expert(ex + 1)
            bt = p
            for kc in range(8):
                sc.op('pe', lambda e, kc=kc: e.transpose(out=bankbf(bt)[:, kc * 128:(kc + 1) * 128], in_=xg[p][:, kc * 128:(kc + 1) * 128],
                                                         identity=identb[:]),
                      reads=[('xg', p), 'identb'], writes=[('bank', bt)] if kc == 7 else [], ww=[('bank', bt)], inc=(kc == 7))
            sc.op('act', lambda e: e.copy(out=xgT[p][:].rearrange("p c t -> p (c t)"), in_=bankbf(bt)), reads=[('bank', bt)], writes=[('xgT', p)])
            bG, bU = 2 + p, 4 + p
            for (bk, wt, wk) in ((bG, wge, 'wge'), (bU, wue, 'wue')):
                for fcn in range(4):
                    for kc in range(8):
                        last = (fcn == 3 and kc == 7)
                        sc.op('pe', lambda e, kc=kc, fcn=fcn, wt=wt, bk=bk: e.matmul(bank(bk, 128, fcn * 128), lhsT=wt[p][:, kc, fcn * 128:(fcn + 1) * 128],
                                                                                   rhs=xgT[p][:, kc, :], start=(kc == 0), stop=(kc == 7)),
                              reads=[('xgT', p), (wk, p)], writes=[('bank', bk)] if last else [], ww=[('bank', bk)], inc=last)
            sc.op('act', lambda e: e.activation(out=sil[p][:], in_=bank(bG), func=AF.Silu), reads=[('bank', bG)], writes=[('sil', p)])
            sc.op('dve', lambda e: e.tensor_tensor(out=hT[p][:].rearrange("p c t -> p (c t)"), in0=sil[p][:], in1=bank(bU), op=ALU.mult),
                  reads=[('sil', p), ('bank', bU)], writes=[('hT', p)])
            for half in range(2):
                for fcn in range(4):
                    sc.op('pe', lambda e, fcn=fcn: e.matmul(bank(6 + half), lhsT=hT[p][:, fcn, :], rhs=wde[p][:, fcn, half * 512:(half + 1) * 512],
                                                           start=(fcn == 0), stop=(fcn == 3)),
                          reads=[('hT', p), ('wde', p, half)], writes=[('bank', 6 + half)] if fcn == 3 else [], ww=[('bank', 6 + half)], inc=(fcn == 3))
            sc.op('act', lambda e: e.activation(out=yw[p][:], in_=ps[:, 6 * 512:8 * 512], func=AF.Copy, scale=tabS[:, ex, 2:3]),
                  reads=[('bank', 6), ('bank', 7)] + tq, writes=[('yw', p)])
            sc.dma('pool', ybuf_d[:, :], yw[p][:], reads=[('yw', p), 'sidx'], writes=[('ybuf', ex)],
                   indirect=dict(out_offset=bass.IndirectOffsetOnAxis(ap=sidx[:, ex:ex + 1], axis=0), in_offset=None))
        sc.barrier()

        g2B = sbp("g2B", [128, D], F32)
        b2B = sbp("b2B", [128, D], F32)
        sc.dma('sp', g2B[:], ln2g_d.partition_broadcast(128), writes=['g2B'])
        sc.dma('sp', b2B[:], ln2b_d.partition_broadcast(128), writes=['b2B'])
        xa = [sbp("xa%d" % a, [128, D], F32) for a in range(4)]
        ya_ = [sbp("yA%d" % a, [128, D], F32) for a in range(4)]
        yb_ = [sbp("yB%d" % a, [128, D], F32) for a in range(4)]

        def ld_fin(i):
            q = i % 4
            sc.dma('sp', xa[q][:], x1_d[i * 128:(i + 1) * 128, :], writes=[('xa', q)])
            sc.dma('sp', ya_[q][:], ybuf_d[i * 128:(i + 1) * 128, :], writes=[('yA', q)])
            sc.dma('sp', yb_[q][:], ybuf_d[S + i * 128: S + (i + 1) * 128, :], writes=[('yB', q)])
        ld_fin(0)
        ld_fin(1)
        for i in range(NT):
            p = i % 4
            pl = i % 2
            if i + 2 < NT:
                ld_fin(i + 2)
            sc.op('pool', lambda e: e.tensor_tensor(out=ya_[p][:], in0=ya_[p][:], in1=yb_[p][:], op=ALU.add),
                  reads=[('yA', p), ('yB', p)], writes=[('yA', p)])
            sc.op('dve', lambda e: e.scalar_tensor_tensor(out=xa[p][:], in0=xa[p][:], scalar=ALPHA, in1=ya_[p][:], op0=ALU.mult, op1=ALU.add),
                  reads=[('xa', p), ('yA', p)], writes=[('xa', p)])
            layernorm_stats(xa[p], pl, ('xa', p))
            sc.op('dve', lambda e: e.tensor_scalar(out=xa[p][:], in0=xa[p][:], scalar1=mv[pl][:, 0:1], scalar2=rs[pl][:, 1:2],
                                                   op0=ALU.subtract, op1=ALU.mult),
                  reads=[('xa', p), ('mv', pl), ('rs', pl)], writes=[('xa', p)])
            sc.op('pool', lambda e: e.tensor_tensor(out=xa[p][:], in0=xa[p][:], in1=g2B[:], op=ALU.mult),
                  reads=[('xa', p), 'g2B'], writes=[('xa', p)])
            sc.op('pool', lambda e: e.tensor_tensor(out=xa[p][:], in0=xa[p][:], in1=b2B[:], op=ALU.add),
                  reads=[('xa', p), 'b2B'], writes=[('xa', p)])
            sc.dma('sp', out_d[i * 128:(i + 1) * 128, :], xa[p][:], reads=[('xa', p)], writes=[('out', i)])

    sc.finish()


def host_consts():
    c = {}
    c["ident_bf"] = np.eye(128, dtype=np.float32).astype(ml_dtypes.bfloat16)
    half = 32
    inv_freq = (10000.0 ** (-np.arange(half, dtype=np.float32) / half)).astype(np.float32)
    ang = np.arange(S, dtype=np.float32)[:, None] * inv_freq[None, :]
    c["cos_t"] = np.cos(ang).astype(np.float32)
    c["sin_t"] = np.sin(ang).astype(np.float32)
    pp = np.arange(128)
    c["tri_bf"] = (pp[None, :] >= pp[:, None]).astype(np.float32).astype(ml_dtypes.bfloat16)
    c["blkind"] = (np.arange(S)[None, :] // 256 == np.arange(8)[:, None]).astype(np.float32).astype(ml_dtypes.bfloat16)
    c["tri_f32"] = (pp[None, :] >= pp[:, None]).astype(np.float32)
    c["ones_f32"] = np.ones((128, 128), np.float32)
    c["ident_f32"] = np.eye(128, dtype=np.float32)
    c["stri_bf"] = (pp[None, :] > pp[:, None]).astype(np.float32).astype(ml_dtypes.bfloat16)
    c["ones_bf"] = np.ones((128, 128), np.float32).astype(ml_dtypes.bfloat16)
    c["iota64"] = np.tile(np.arange(64, dtype=np.float32)[None, :], (128, 1))
    c["tokid"] = (np.arange(NT, dtype=np.float32)[None, :] * 128 + pp[:, None]).astype(np.float32)
    ti = np.zeros((64 * 128, 4), np.float32)
    ti[:, 1] = 2 * S + np.tile(np.arange(128, dtype=np.float32), 64)
    c["tab_init"] = ti
    return c


def make_in_maps(inputs, n_cores=8):
    c = host_consts()
    maps = []
    for b in range(n_cores):
        m = dict(c)
        m["x"] = np.ascontiguousarray(inputs["x"][b])
        m["ln0_g"] = np.ascontiguousarray(inputs["ln0_g"])
        m["ln0_b"] = np.ascontiguousarray(inputs["ln0_b"])
        m["w_in"] = np.ascontiguousarray(inputs["w_in"][0])
        cv = np.concatenate([inputs["conv_w"][0], inputs["conv_b"][0][None], inputs["gn_g"][0][None], inputs["skip"][0][None]], axis=0)
        m["chanv"] = np.ascontiguousarray(cv.reshape(7, 4, 128).transpose(2, 1, 0))
        m["wqk_m"] = np.ascontiguousarray(np.concatenate([inputs["w_mq"][0], inputs["w_mk"][0]], axis=2))
        m["b_if"] = np.ascontiguousarray(np.concatenate([inputs["b_i"][0], inputs["b_f"][0]]))
        m["w_attn_up"] = np.ascontiguousarray(inputs["w_attn_up"][0])
        m["w_mlstm_up"] = np.ascontiguousarray(inputs["w_mlstm_up"][0])
        m["w_out"] = np.ascontiguousarray(inputs["w_out"][0])
        m["ln1_g"] = np.ascontiguousarray(inputs["ln1_g"][0])
        m["w_gate"] = np.ascontiguousarray(inputs["w_gate"][0])
        m["w_up"] = np.ascontiguousarray(inputs["w_up"][0])
        m["w_down"] = np.ascontiguousarray(inputs["w_down"][0])
        m["ln2_g"] = np.ascontiguousarray(inputs["ln2_g"][0])
        m["ln2_b"] = np.ascontiguousarray(inputs["ln2_b"][0])
        m["ln1_b"] = np.ascontiguousarray(inputs["ln1_b"][0])
        m["w_router"] = np.ascontiguousarray(np.concatenate([inputs["w_router_group"][0], inputs["w_router_expert"][0]], axis=1))
        m["b_router"] = np.ascontiguousarray(np.concatenate([inputs["b_router_group"][0], inputs["b_router_expert"][0]]))
        maps.append(m)
    return maps


def kernel(**inputs):
    inputs = {k: np.asarray(v) for k, v in inputs.items()}
    nc = build_program()
    maps = make_in_maps(inputs, 8)
    res = run_bass_kernel_spmd(nc, maps, core_ids=list(range(8)))
    return np.stack([r["out"] for r in res.results], axis=0).astype(np.float32)
```
